# Optimizing a Trainium2 kernel written in Bass

```python
import jax, jax.numpy as jnp
from jax import lax
import numpy as np

D_MODEL = 1024
BATCH = 2
SEQ = 8192
DEPTH = 1

CHUNK = 64
HEAD_SIZE = 64
D_RWKV = D_MODEL // 2
N_RWKV_HEADS = D_RWKV // HEAD_SIZE
D_SGU = D_MODEL - D_RWKV
N_SGU_HEADS = D_SGU // HEAD_SIZE
SGU_BLOCK = 128
D_DECAY_LORA = 64
D_AAA_LORA = 64
D_GATE_LORA = 128
N_SHIFT = 3 * D_RWKV + D_DECAY_LORA + D_AAA_LORA + D_GATE_LORA
D_IN = N_SHIFT + 2 * D_SGU
N_EXPERTS = 64
TOP_K = 8
N_GROUPS = 8
TOPK_GROUPS = 4
D_EXPERT = 256
ROUTED_SCALE = 2.5
MOE_BLOCK = 128
RMS_EPS = 1e-6
LN_EPS = 1e-5
LN_X_EPS = 64e-5

kernel_name = 'hymba_rwkv7_sgu_moe_adaln_block'


def rmsnorm(x, g):
    xf = x.astype(jnp.float32)
    y = xf * lax.rsqrt(jnp.mean(xf * xf, axis=-1, keepdims=True) + RMS_EPS)
    return (y * g.astype(jnp.float32)).astype(x.dtype)


def layernorm(x, g, b):
    xf = x.astype(jnp.float32)
    mu = jnp.mean(xf, axis=-1, keepdims=True)
    var = jnp.mean((xf - mu) ** 2, axis=-1, keepdims=True)
    y = (xf - mu) * lax.rsqrt(var + LN_EPS)
    return (y * g.astype(jnp.float32) + b.astype(jnp.float32)).astype(x.dtype)


def swiglu(h, w1, w3, w2):
    return (jax.nn.silu(h @ w1) * (h @ w3)) @ w2


def rwkv7_mix(ps, w0, w_decay_up, a0, w_a_up, w_g_up, k_k, k_a, r_k, lnx_g, lnx_b):
    B, S, _ = ps.shape
    f32 = jnp.float32
    c1, c2, c3 = D_RWKV, 2 * D_RWKV, 3 * D_RWKV
    c4, c5 = c3 + D_DECAY_LORA, c3 + D_DECAY_LORA + D_AAA_LORA
    r, k, v = ps[..., :c1], ps[..., c1:c2], ps[..., c2:c3]
    xw, xa, xg = ps[..., c3:c4], ps[..., c4:c5], ps[..., c5:]
    w = -jax.nn.softplus(-(w0 + jnp.tanh(xw) @ w_decay_up).astype(f32)) - 0.5
    decay = jnp.exp(-jnp.exp(w))
    a = jax.nn.sigmoid((a0 + xa @ w_a_up).astype(f32))
    g = jax.nn.sigmoid(xg) @ w_g_up
    heads = lambda t: t.reshape(B, S, N_RWKV_HEADS, HEAD_SIZE)
    kk = heads((k * k_k).astype(f32))
    kk = kk / jnp.maximum(jnp.sqrt(jnp.sum(kk * kk, axis=-1, keepdims=True)), 1e-12)
    a_h = heads(a)
    k_h = heads(k.astype(f32) * (1.0 + (a - 1.0) * k_a.astype(f32)))
    r_h = heads(r.astype(f32))
    v_h = heads(v.astype(f32))
    tm = lambda t: jnp.swapaxes(t, 0, 1)

    def step(state, inp):
        r_t, w_t, k_t, v_t, kk_t, a_t = inp
        sa = jnp.einsum('bhij,bhj->bhi', state, -kk_t)
        state = (state * w_t[:, :, None, :]
                 + sa[..., None] * (kk_t * a_t)[:, :, None, :]
                 + v_t[..., None] * k_t[:, :, None, :])
        y_t = jnp.einsum('bhij,bhj->bhi', state, r_t)
        return state, y_t

    s0 = jnp.zeros((B, N_RWKV_HEADS, HEAD_SIZE, HEAD_SIZE), f32)
    _, y = lax.scan(step, s0, (tm(r_h), tm(heads(decay)), tm(k_h), tm(v_h), tm(kk), tm(a_h)))
    y = tm(y)
    mu = jnp.mean(y, axis=-1, keepdims=True)
    var = jnp.mean((y - mu) ** 2, axis=-1, keepdims=True)
    y = ((y - mu) * lax.rsqrt(var + LN_X_EPS)).reshape(B, S, D_RWKV)
    y = y * lnx_g.astype(f32) + lnx_b.astype(f32)
    bonus = jnp.sum(r_h * k_h * r_k.astype(f32), axis=-1, keepdims=True) * v_h
    y = y + bonus.reshape(B, S, D_RWKV)
    return (y * g.astype(f32)).astype(ps.dtype)


def sgu_mix(pb, ln_g, ln_b, w_spatial, b_spatial):
    B, S, _ = pb.shape
    z = jax.nn.gelu(pb)
    u, v = z[..., :D_SGU], z[..., D_SGU:]
    v = layernorm(v, ln_g, ln_b)
    nb = S // SGU_BLOCK
    v = v.reshape(B, nb, SGU_BLOCK, N_SGU_HEADS, HEAD_SIZE)
    pos_chunk = jnp.arange(SGU_BLOCK) // CHUNK
    mask = pos_chunk[None, :] <= pos_chunk[:, None]
    ws = jnp.where(mask[None], w_spatial, 0.0).astype(v.dtype)
    sp = jnp.einsum('hts,bnshc->bnthc', ws, v) + b_spatial.T[:, :, None].astype(v.dtype)
    return u * sp.reshape(B, S, D_SGU)


def routed_moe(h, w_router, e_bias, w1_e, w3_e, w2_e):
    T, D = h.shape
    f32 = jnp.float32
    scores = jax.nn.sigmoid((h @ w_router).astype(f32))
    biased = scores + e_bias.astype(f32)
    per_group = N_EXPERTS // N_GROUPS
    grp_score = jnp.sum(lax.top_k(biased.reshape(T, N_GROUPS, per_group), 2)[0], axis=-1)
    _, gidx = lax.top_k(grp_score, TOPK_GROUPS)
    gmask = jnp.sum(jax.nn.one_hot(gidx, N_GROUPS, dtype=f32), axis=-2) > 0
    emask = jnp.repeat(gmask, per_group, axis=-1)
    _, eidx = lax.top_k(jnp.where(emask, biased, -jnp.inf), TOP_K)
    wts = jnp.take_along_axis(scores, eidx, axis=-1)
    wts = wts / jnp.sum(wts, axis=-1, keepdims=True) * ROUTED_SCALE

    A = T * TOP_K
    flat_e = eidx.reshape(A).astype(jnp.int32)
    flat_t = jnp.repeat(jnp.arange(T, dtype=jnp.int32), TOP_K)
    flat_w = wts.reshape(A)
    order = jnp.argsort(flat_e)
    se = flat_e[order]
    counts = jnp.bincount(flat_e, length=N_EXPERTS).astype(jnp.int32)
    padded = (counts + MOE_BLOCK - 1) // MOE_BLOCK * MOE_BLOCK
    start = jnp.cumsum(counts) - counts
    pend = jnp.cumsum(padded)
    pstart = pend - padded
    dest = pstart[se] + jnp.arange(A, dtype=jnp.int32) - start[se]
    n_blocks = (A + N_EXPERTS * (MOE_BLOCK - 1) + MOE_BLOCK - 1) // MOE_BLOCK
    P = n_blocks * MOE_BLOCK
    buf_t = jnp.zeros((P,), jnp.int32).at[dest].set(flat_t[order])
    buf_w = jnp.zeros((P,), f32).at[dest].set(flat_w[order])
    block_e = jnp.searchsorted(pend, jnp.arange(n_blocks, dtype=jnp.int32) * MOE_BLOCK, side='right')
    block_e = jnp.minimum(block_e, N_EXPERTS - 1)

    def block_fn(args):
        tok, e = args
        xb = h[tok]
        return swiglu(xb, w1_e[e], w3_e[e], w2_e[e])

    yb = lax.map(block_fn, (buf_t.reshape(n_blocks, MOE_BLOCK), block_e))
    yb = yb.reshape(P, D) * buf_w[:, None].astype(h.dtype)
    return jnp.zeros_like(h).at[buf_t].add(yb)


def setup_inputs(seed: int = 0) -> dict:
    key = jax.random.key(seed)
    ks = jax.random.split(key, 40)
    f32 = jnp.float32
    nrm = lambda k, shape, s: jax.random.normal(k, shape, f32) * s
    L, D = DEPTH, D_MODEL
    return {
        'x': nrm(ks[0], (BATCH, SEQ, D), 1.0),
        'c': nrm(ks[1], (BATCH, D), 1.0),
        'w_ada': nrm(ks[2], (L, D, 6 * D), 0.5 * D ** -0.5),
        'b_ada': nrm(ks[3], (L, 6 * D), 0.02),
        'norm1_g': 1.0 + nrm(ks[4], (L, D), 0.02),
        'w_in': nrm(ks[5], (L, D, D_IN), D ** -0.5),
        'mu_shift': jax.random.uniform(ks[6], (L, N_SHIFT), f32),
        'w0': jax.random.uniform(ks[7], (L, D_RWKV), f32, -3.0, 0.5),
        'w_decay_up': nrm(ks[8], (L, D_DECAY_LORA, D_RWKV), 0.1),
        'a0': nrm(ks[9], (L, D_RWKV), 0.1),
        'w_a_up': nrm(ks[10], (L, D_AAA_LORA, D_RWKV), D_AAA_LORA ** -0.5),
        'w_g_up': nrm(ks[11], (L, D_GATE_LORA, D_RWKV), D_GATE_LORA ** -0.5),
        'k_k': 0.85 + nrm(ks[12], (L, D_RWKV), 0.02),
        'k_a': 1.0 + nrm(ks[13], (L, D_RWKV), 0.02),
        'r_k': nrm(ks[14], (L, N_RWKV_HEADS, HEAD_SIZE), 0.1),
        'lnx_g': 1.0 + nrm(ks[15], (L, D_RWKV), 0.02),
        'lnx_b': nrm(ks[16], (L, D_RWKV), 0.02),
        'sgu_ln_g': 1.0 + nrm(ks[17], (L, D_SGU), 0.02),
        'sgu_ln_b': nrm(ks[18], (L, D_SGU), 0.02),
        'w_spatial': nrm(ks[19], (L, N_SGU_HEADS, SGU_BLOCK, SGU_BLOCK), SGU_BLOCK ** -0.5),
        'b_spatial': 1.0 + nrm(ks[20], (L, N_SGU_HEADS, SGU_BLOCK), 0.01),
        'w_out': nrm(ks[21], (L, D, D), D ** -0.5),
        'norm2_g': 1.0 + nrm(ks[22], (L, D), 0.02),
        'w_router': nrm(ks[23], (L, D, N_EXPERTS), D ** -0.5),
        'e_bias': nrm(ks[24], (L, N_EXPERTS), 0.01),
        'w1_e': nrm(ks[25], (L, N_EXPERTS, D, D_EXPERT), D ** -0.5),
        'w3_e': nrm(ks[26], (L, N_EXPERTS, D, D_EXPERT), D ** -0.5),
        'w2_e': nrm(ks[27], (L, N_EXPERTS, D_EXPERT, D), D_EXPERT ** -0.5),
        'w1_s': nrm(ks[28], (L, D, D_EXPERT), D ** -0.5),
        'w3_s': nrm(ks[29], (L, D, D_EXPERT), D ** -0.5),
        'w2_s': nrm(ks[30], (L, D_EXPERT, D), D_EXPERT ** -0.5),
        'norm_f_g': 1.0 + nrm(ks[31], (D,), 0.02),
    }


def reference(x, c, w_ada, b_ada, norm1_g, w_in, mu_shift, w0, w_decay_up, a0, w_a_up, w_g_up,
              k_k, k_a, r_k, lnx_g, lnx_b, sgu_ln_g, sgu_ln_b, w_spatial, b_spatial, w_out,
              norm2_g, w_router, e_bias, w1_e, w3_e, w2_e, w1_s, w3_s, w2_s, norm_f_g):
    B, S, D = x.shape
    for l in range(DEPTH):
        mod = (jax.nn.silu(c) @ w_ada[l] + b_ada[l])[:, None, :]
        sh1, sc1, g1, sh2, sc2, g2 = jnp.split(mod, 6, axis=-1)

        h = rmsnorm(x, norm1_g[l]) * (1.0 + sc1) + sh1
        proj = h @ w_in[l]
        ps = proj[..., :N_SHIFT]
        prev = jnp.pad(ps, ((0, 0), (1, 0), (0, 0)))[:, :-1]
        ps = ps + (prev - ps) * mu_shift[l]
        y_a = rwkv7_mix(ps, w0[l], w_decay_up[l], a0[l], w_a_up[l], w_g_up[l],
                        k_k[l], k_a[l], r_k[l], lnx_g[l], lnx_b[l])
        y_b = sgu_mix(proj[..., N_SHIFT:], sgu_ln_g[l], sgu_ln_b[l],
                      w_spatial[l], b_spatial[l])
        y = jnp.concatenate([y_a, y_b], axis=-1) @ w_out[l]
        x = x + g1 * y

        h2 = (rmsnorm(x, norm2_g[l]) * (1.0 + sc2) + sh2).reshape(B * S, D)
        m = swiglu(h2, w1_s[l], w3_s[l], w2_s[l]) + routed_moe(h2, w_router[l], e_bias[l],
                                                               w1_e[l], w3_e[l], w2_e[l])
        x = x + g2 * m.reshape(B, S, D)
    return rmsnorm(x, norm_f_g)
```

```python
import numpy as np
import ml_dtypes
from contextlib import ExitStack

import concourse.bass as bass
import concourse.mybir as mybir
from concourse.bass_utils import run_bass_kernel_spmd

F32 = mybir.dt.float32
BF16 = mybir.dt.bfloat16
AF = mybir.ActivationFunctionType
ALU = mybir.AluOpType
AX = mybir.AxisListType

D = 1024
SEG = 2048
NSEG = 4
GRP = 512
NGRP = NSEG * SEG // GRP
OWN_G0 = (NSEG - 1) * SEG // GRP
C0 = 0.6065306597126334
NE = 64
DE = 256
SAME_SYNC = True


class Buf:
    def __init__(self, name):
        self.name = name
        self.psum = False
        self.w = None
        self.r = {}


class Tile(Buf):
    def __init__(self, ctx, name, shape, dtype, psum=False):
        super().__init__(name)
        self.psum = psum
        ctx.all_bufs.append(self)
        alloc = ctx.nc.psum_tensor if psum else ctx.nc.sbuf_tensor
        self.t = ctx.es.enter_context(alloc(name, list(shape), dtype))
        self.subs = {}

    def __getitem__(self, idx):
        return self.t[idx]

    def sub(self, key):
        if key not in self.subs:
            self.subs[key] = Buf(f"{self.name}.{key}")
        return self.subs[key]


class Ctx:
    NDS = 24

    def __init__(self, nc, es):
        self.nc = nc
        self.es = es
        self.eng = {"pe": nc.tensor, "act": nc.scalar, "dve": nc.vector, "pool": nc.gpsimd, "sp": nc.sync}
        self.sem = {k: es.enter_context(nc.semaphore("s_" + k)) for k in ("pe", "act", "dve", "pool")}
        self.cnt = {k: 0 for k in self.sem}
        self.seen = {k: {} for k in self.eng}
        self.dsem = [es.enter_context(nc.semaphore(f"s_dma{i}")) for i in range(self.NDS)]
        self.dcnt = [0] * self.NDS
        self.dnext = 0
        self.pending = {k: [] for k in self.sem}
        self.n_inst = 0
        self.all_bufs = []

    def _wait(self, e, h):
        kind, key, val = h
        if kind == "e" and key == e and (e == "pe" or not SAME_SYNC):
            return
        sk = (kind, key)
        if self.seen[e].get(sk, 0) >= val:
            return
        sem = self.sem[key] if kind == "e" else self.dsem[key]
        self.eng[e].wait_ge(sem, val)
        self.seen[e][sk] = val

    def _deps(self, e, reads, writes):
        hs = {}
        for b in reads:
            if b.w is not None:
                k = b.w[:2]
                hs[k] = max(hs.get(k, 0), b.w[2])
        for b in writes:
            if b.w is not None:
                k = b.w[:2]
                hs[k] = max(hs.get(k, 0), b.w[2])
            for k, v in b.r.items():
                hs[k] = max(hs.get(k, 0), v)
        for k, v in hs.items():
            self._wait(e, (k[0], k[1], v))

    def _mark(self, h, reads, writes):
        k = h[:2]
        for b in reads:
            b.r[k] = max(b.r.get(k, 0), h[2])
        for b in writes:
            b.w = h
            b.r = {}

    def op(self, e, fn, reads=(), writes=(), inc=True):
        pr = [b for b in reads if b.psum]
        if pr:
            reads = [b for b in reads if not b.psum]
            writes = list(writes) + [b for b in pr if b not in writes]
        self._deps(e, reads, writes)
        inst = fn(self.eng[e])
        self.n_inst += 1
        if inc:
            self.cnt[e] += 1
            inst.then_inc(self.sem[e], 1)
            h = ("e", e, self.cnt[e])
        else:
            h = ("e", e, self.cnt[e] + 1)
        self._mark(h, reads, writes)
        return h

    def dma(self, q, out, in_, reads=(), writes=(), **kw):
        i = self.dnext
        self.dnext = (self.dnext + 1) % self.NDS
        if self.dcnt[i]:
            self._wait(q, ("d", i, self.dcnt[i]))
        self._deps(q, reads, writes)
        inst = self.eng[q].dma_start(out=out, in_=in_, **kw)
        self.n_inst += 1
        self.dcnt[i] += 16
        inst.then_inc(self.dsem[i], 16)
        h = ("d", i, self.dcnt[i])
        self._mark(h, reads, writes)
        return h

    def wait_all(self, e, bufs):
        self._deps(e, (), bufs)

    def mm(self, out, lhsT, rhs, reads, writes, start=True, stop=True, inc=True):
        return self.op("pe", lambda E: E.matmul(out, lhsT, rhs, start=start, stop=stop), reads, writes, inc=inc)

    def tr(self, out, in_, ident, reads, writes, inc=True):
        return self.op("pe", lambda E: E.transpose(out, in_, ident), reads, writes, inc=inc)

    def act(self, out, in_, func, reads, writes, bias=None, scale=None, e="act", accum_out=None):
        kw = {}
        if bias is not None:
            kw["bias"] = bias
        if scale is not None:
            kw["scale"] = scale
        if accum_out is not None:
            kw["accum_out"] = accum_out
        return self.op(e, lambda E: E.activation(out=out, in_=in_, func=func, **kw), reads, writes)

    def tt(self, out, in0, in1, op, reads, writes, e="dve"):
        return self.op(e, lambda E: E.tensor_tensor(out=out, in0=in0, in1=in1, op=op), reads, writes)

    def ts(self, out, in0, s1, s2, op0, op1, reads, writes, e="dve"):
        if op1 is None:
            return self.op(e, lambda E: E.tensor_scalar(out=out, in0=in0, scalar1=s1, scalar2=None, op0=op0), reads, writes)
        return self.op(e, lambda E: E.tensor_scalar(out=out, in0=in0, scalar1=s1, scalar2=s2, op0=op0, op1=op1), reads, writes)

    def stt(self, out, in0, scalar, in1, op0, op1, reads, writes):
        return self.op("dve", lambda E: E.scalar_tensor_tensor(out=out, in0=in0, scalar=scalar, in1=in1, op0=op0, op1=op1), reads, writes)

    def cp(self, out, in_, reads, writes, e="dve"):
        if e == "act":
            return self.op("act", lambda E: E.copy(out=out, in_=in_), reads, writes)
        return self.op(e, lambda E: E.tensor_copy(out=out, in_=in_), reads, writes)

    def memset(self, ap, val, writes, e="dve"):
        return self.op(e, lambda E: E.memset(ap, val), (), writes)


def _consts():
    c = {}
    ident = np.eye(128, dtype=np.float32)
    c["ident"] = ident
    s = np.arange(128)[:, None] % 64
    t = np.arange(64)[None, :]
    su = (t > s).astype(np.float32)
    ui = (t >= s).astype(np.float32)
    sl = (t < s).astype(np.float32)
    half = (np.arange(128)[:, None] // 64 == np.arange(128)[None, :] // 64).astype(np.float32)
    def bd(m64):
        return np.concatenate([m64, m64], axis=1) * half
    c["maskX"] = np.concatenate([bd(su), bd(sl), bd(su), bd(sl)], axis=1)
    c["maskY"] = np.concatenate([bd(su), bd(ui), bd(ui)], axis=1)
    c["blk"] = half.copy()
    restart = np.ones((128, GRP), np.float32)
    restart[:, ::64] = 0.0
    c["restart"] = restart
    hsel = np.zeros((128, 4, 8), np.float32)
    for p_ in range(128):
        for hp in range(4):
            hsel[p_, hp, 2 * hp + p_ // 64] = 1.0
    c["hsel"] = hsel
    c["hselT"] = np.ascontiguousarray(hsel.transpose(2, 1, 0))
    return c


CONST_SHAPES = {"ident": [128, 128], "maskX": [128, 512], "maskY": [128, 384], "blk": [128, 128], "restart": [128, GRP], "hsel": [128, 4, 8], "hselT": [8, 4, 128]}


def build_nc(stage=99, dbg=False):
    nc = bass.Bass("TRN2", target_bir_lowering=False)

    def din(name, shape, dt=F32):
        return nc.dram_tensor(name, list(shape), dt, kind="ExternalInput").ap()

    xw = din("xw", [NSEG * SEG, D])
    flags = din("flags", [128, 5])
    c_fm = din("c_fm", [128, 8])
    w_ada = din("w_ada", [D, 6 * D])
    b_ada = din("b_ada", [128, 48])
    n1g = din("n1g", [128, 8])
    w_in = din("w_in", [D, 2816])
    mu = din("mu", [128, 14])
    vec4 = din("vec4", [128, 7, 4])
    w_dec = din("w_dec", [64, 512])
    w_aup = din("w_aup", [64, 512])
    w_gup = din("w_gup", [128, 512])
    sgu_g = din("sgu_g", [1, 512])
    sgu_b = din("sgu_b", [1, 512])
    w_sp = din("w_sp", [8, 128, 128])
    b_sp = din("b_sp", [8, 128])
    w_out = din("w_out", [D, D])
    n2g = din("n2g", [128, 8])
    w_rt = din("w_rt", [D, NE])
    e_bias = din("e_bias", [1, NE])
    nexp = NE + 1 if stage >= 3 else 1
    w1e = din("w1e", [nexp, D, DE])
    w3e = din("w3e", [nexp, D, DE])
    w2e = din("w2e", [nexp, DE, D])
    nfg = din("nfg", [1, D])
    cst = {k: din("c_" + k, shp) for k, shp in CONST_SHAPES.items()}
    out = nc.dram_tensor("out", [SEG, D], F32, kind="ExternalOutput").ap()
    x2d = nc.dram_tensor("x2_scratch", [SEG, D], F32, kind="Internal").ap()
    dbg_out = {}
    x2dbuf = Buf("x2dbuf")

    def dbg_tensor(name, shape):
        dbg_out[name] = nc.dram_tensor("dbg_" + name, list(shape), F32, kind="ExternalOutput").ap()
        return dbg_out[name]

    es = ExitStack()
    with es:
        K = Ctx(nc, es)
        T = lambda name, shape, dt=F32, psum=False: Tile(K, name, shape, dt, psum)
        def sb_used():
            return 0

        ident = T("ident", [128, 128])
        identb = T("identb", [128, 128], BF16)
        maskX = T("maskX", [128, 512])
        maskY = T("maskY", [128, 384])
        blk = T("blk", [128, 128])
        blkb = T("blkb", [128, 128], BF16)
        blk64 = T("blk64", [128, 128])
        restart = T("restart", [128, GRP])
        ones1 = T("ones1", [1, 128])
        flg = T("flg", [128, 5])
        K.dma("sp", ident[:], cst["ident"], (), [ident])
        K.dma("sp", maskX[:], cst["maskX"], (), [maskX])
        K.dma("sp", maskY[:], cst["maskY"], (), [maskY])
        K.dma("sp", blk[:], cst["blk"], (), [blk])
        K.dma("sp", restart[:], cst["restart"], (), [restart])
        K.dma("sp", flg[:], flags, (), [flg])
        K.cp(identb[:], ident[:], [ident], [identb])
        K.cp(blkb[:], blk[:], [blk], [blkb])
        K.ts(blk64[:], blk[:], 1.0 / 64.0, None, ALU.mult, None, [blk], [blk64])
        K.memset(ones1[:], 1.0, [ones1])

        banks = [T(f"bank{i}", [128, 512], F32, psum=True) for i in range(8)]

        n1g_t = T("n1g_t", [128, 8]); n2g_t = T("n2g_t", [128, 8]); mu_t = T("mu_t", [128, 14]); omm_t = T("omm_t", [128, 14])
        v4 = T("v4", [128, 7, 4]); cfm = T("cfm", [128, 8])
        K.dma("sp", n1g_t[:], n1g, (), [n1g_t]); K.dma("sp", n2g_t[:], n2g, (), [n2g_t])
        K.dma("sp", mu_t[:], mu, (), [mu_t]); K.dma("sp", v4[:], vec4, (), [v4]); K.dma("sp", cfm[:], c_fm, (), [cfm])
        K.ts(omm_t[:], mu_t[:], -1.0, 1.0, ALU.mult, ALU.add, [mu_t], [omm_t])
        omka = T("omka", [128, 4])
        K.ts(omka[:], v4[:, 3, :], -1.0, 1.0, ALU.mult, ALU.add, [v4], [omka])

        silc = T("silc", [128, 8])
        K.act(silc[:], cfm[:], AF.Silu, [cfm], [silc])
        modT = T("modT", [128, 48])
        K.dma("sp", modT[:], b_ada, (), [modT])
        g1rep = T("g1rep", [128, D]); g2rep = T("g2rep", [128, D])
        with ExitStack() as es2:
            K.es = es2
            wab = [T(f"wab{i}", [128, 3072]) for i in range(2)]
            dg = T("dg", [128, 128])
            it = 0
            for kc in range(8):
                for half in range(2):
                    wt = wab[it % 2]; it += 1
                    K.dma("sp", wt[:], w_ada[kc * 128:(kc + 1) * 128, half * 3072:(half + 1) * 3072], (), [wt])
                    for jc in range(24):
                        K.mm(banks[0][:, half * 24 + jc: half * 24 + jc + 1], wt[:, jc * 128:(jc + 1) * 128], silc[:, kc:kc + 1], [wt, silc], [banks[0]],
                             inc=(jc == 23))
                K.tt(modT[:], banks[0][:, 0:48], modT[:], ALU.add, [banks[0], modT], [modT])
            onesf = T("onesf", [128, 128])
            K.memset(onesf[:], 1.0, [onesf])
            for (idx, dst) in ((2, g1rep), (5, g2rep)):
                for kc in range(8):
                    K.ts(dg[:], ident[:], modT[:, idx * 8 + kc: idx * 8 + kc + 1], None, ALU.mult, None, [ident, modT], [dg])
                    K.mm(banks[1][:, 0:128], onesf[:], dg[:], [onesf, dg], [banks[1]])
                    K.cp(dst[:, kc * 128:(kc + 1) * 128], banks[1][:, 0:128], [banks[1]], [dst])
            K.wait_all("dve", wab + [dg, onesf]); K.wait_all("pe", wab + [dg, onesf]); K.wait_all("sp", wab)
            K.wait_all("act", wab + [dg, onesf]); K.wait_all("pool", wab + [dg, onesf])
        K.es = es
        A1 = T("A1", [128, 8]); A2 = T("A2", [128, 8])
        K.stt(A1[:], modT[:, 8:16], 1.0, n1g_t[:], ALU.add, ALU.mult, [modT, n1g_t], [A1])
        K.stt(A2[:], modT[:, 32:40], 1.0, n2g_t[:], ALU.add, ALU.mult, [modT, n2g_t], [A2])
        A1f = T("A1f", [128, 5, 8]); B1f = T("B1f", [128, 5, 8])
        for f in range(5):
            K.ts(A1f[:, f, :], A1[:], flg[:, f:f + 1], None, ALU.mult, None, [A1, flg], [A1f])
            K.ts(B1f[:, f, :], modT[:, 0:8], flg[:, f:f + 1], None, ALU.mult, None, [modT, flg], [B1f])

        def finish():
            for i in range(K.NDS):
                if K.dcnt[i]:
                    K._wait("sp", ("d", i, K.dcnt[i]))
            for e in ("pe", "act", "dve", "pool"):
                if K.cnt[e]:
                    K._wait("sp", ("e", e, K.cnt[e]))

        if dbg and stage == 0:
            d = dbg_tensor("modT", [128, 48])
            K.dma("sp", d, modT[:], [modT], ())
            d = dbg_tensor("A1", [128, 8])
            K.dma("sp", d, A1[:], [A1], ())
        if stage == 0:
            zt = T("zt", [128, D])
            K.memset(zt[:], 0.0, [zt])
            for i in range(16):
                K.dma("sp", out[i * 128:(i + 1) * 128, :], zt[:], [zt], ())
            finish()
            return nc, dbg_out

        es_scan = ExitStack()
        es_scan.__enter__()
        K.es = es_scan
        own_extra = stage >= 2
        winb = T("winb", [128, 8, 2816], BF16)
        for kc in range(8):
            for c0 in range(0, 2816, 1408):
                K.dma("pool", winb[:, kc, c0:c0 + 1408], w_in[kc * 128:(kc + 1) * 128, c0:c0 + 1408], (), [winb])
        wlo = T("wlo", [128, 512], BF16)
        wgb = T("wgb", [128, 512], BF16)
        K.dma("pool", wlo[0:64, :], w_dec, (), [wlo])
        K.dma("pool", wlo[64:128, :], w_aup, (), [wlo])
        K.dma("pool", wgb[:], w_gup, (), [wgb])
        hsel = T("hsel", [128, 4, 8], BF16); hselT = T("hselT", [8, 4, 128], BF16)
        hsel_f = T("hsel_f", [128, 4, 8]); hselT_f = T("hselT_f", [8, 4, 128])
        K.dma("sp", hsel_f[:], cst["hsel"], (), [hsel_f]); K.dma("sp", hselT_f[:], cst["hselT"], (), [hselT_f])
        K.cp(hsel[:], hsel_f[:], [hsel_f], [hsel]); K.cp(hselT[:], hselT_f[:], [hselT_f], [hselT])
        lrk = T("lrk", [128, 4, 128], BF16)
        for hp in range(4):
            K.ts(lrk[:, hp, :], blk[:], v4[:, 6, hp:hp + 1], None, ALU.mult, None, [blk, v4], [lrk])
        epsln = T("epsln", [128, 1])
        K.memset(epsln[:], 64e-5, [epsln])
        for bk in banks:
            K.memset(bk[:], 0.0, [bk])

        xt = T("xt", [128, 4, D])
        x2t = T("x2t", [128, D])
        if own_extra:
            woutb = T("woutb", [128, 8, D], BF16)
            for kc in range(8):
                K.dma("sp", x2t[:], w_out[kc * 128:(kc + 1) * 128, :], (), [x2t])
                K.tt(woutb[:, kc, :], x2t[:], g1rep[:], ALU.mult, [x2t, g1rep], [woutb])
            wsTb = T("wsTb", [128, 8, 128], BF16)
            for h in range(8):
                K.dma("sp", x2t[:, 0:128], w_sp[h], (), [x2t])
                K.tr(banks[0][:, 0:128], x2t[:, 0:128], ident[:], [x2t, ident], [banks[0]])
                K.cp(wsTb[:, h, :], banks[0][:, 0:128], [banks[0]], [wsTb])
                K.memset(wsTb[64:128, h, 0:64], 0.0, [wsTb])
            bsp = T("bsp", [128, 4, 128])
            for hp in range(4):
                for hh in range(2):
                    K.dma("sp", bsp[hh * 64:(hh + 1) * 64, hp, :], b_sp[2 * hp + hh, :].partition_broadcast(64), (), [bsp])
            lngr = T("lngr", [128, 512]); lnbr = T("lnbr", [128, 512])
            K.dma("sp", lngr[:], sgu_g[0, :].partition_broadcast(128), (), [lngr])
            K.dma("sp", lnbr[:], sgu_b[0, :].partition_broadcast(128), (), [lnbr])

        xy = T("xy", [128, 4096], BF16)
        hTt = T("hTt", [128, 8, GRP], BF16)
        uT = T("uT", [128, 4, GRP], BF16)
        ssq = T("ssq", [128, 4]); rstd = T("rstd", [128, 4])
        carry = T("carry", [128, 14])
        K.memset(carry[:], 0.0, [carry])
        shtmp = T("shtmp", [128, GRP + 1])
        sh12 = T("sh12", [128, GRP])
        twxa = T("twxa", [128, GRP], BF16); sgb = T("sgb", [128, GRP], BF16)
        shv = T("shv", [128, GRP])
        shk4 = [T(f"shk{i}", [128, GRP]) for i in range(4)]
        ss8 = T("ss8", [8, GRP]); rn8 = T("rn8", [8, GRP], BF16)
        lwp = T("lwp", [128, GRP]); av = T("av", [128, GRP]); gT = T("gT", [128, GRP])
        kkn = T("kkn", [128, GRP]); kmod = T("kmod", [128, GRP]); bb = T("bb", [128, GRP])
        cum = T("cum", [128, GRP])
        ep = T("ep", [128, GRP]); em = T("em", [128, GRP]); eq = T("eq", [128, GRP])
        tmpa = T("tmpa", [128, GRP]); tmpb = T("tmpb", [128, GRP])
        pc = T("pc", [128, 8])
        atb = T("atb", [128, GRP], BF16); rtb = T("rtb", [128, GRP], BF16); rtf = T("rtf", [128, GRP])
        btb = T("btb", [128, GRP], BF16); ktb = T("ktb", [128, GRP], BF16)
        bhb = T("bhb", [128, GRP], BF16); khb = T("khb", [128, GRP], BF16); vbb = T("vbb", [128, GRP], BF16)
        sqb = [atb, rtb, btb, ktb]
        rkb = T("rkb", [128, GRP], BF16); bv = T("bv", [128, GRP])
        TM = [T(f"TM{i}", [128, 3, 128], BF16) for i in range(2)]
        ZB = [[T(f"ZB{i}_{h}", [128, 2, 64], BF16) for h in range(2)] for i in range(2)]
        XQ = [[T(f"XQ{h}_{i}", [128, 3, 128], BF16) for i in range(2)] for h in range(2)]
        YS = [T(f"YS{h}", [128, 384], BF16) for h in range(2)]
        UW = [T(f"UW{h}", [128, 2, 64], BF16) for h in range(2)]
        MT = [T(f"MT{i}", [128, 128], BF16) for i in range(2)]
        RhT = [T(f"RhT{i}", [128, 64], BF16) for i in range(2)]
        Hb = [[T(f"H{hp}_{i}", [128, 128], BF16) for i in range(2)] for hp in range(4)]
        hpar = [0, 0, 0, 0]
        for hp in range(4):
            K.memset(Hb[hp][0][:], 0.0, [Hb[hp][0]])
        YT = T("YT", [128, GRP])
        sh13 = YT; shr = cum; vtm = tmpb
        if own_extra:
            vnb = T("vnb", [128, 4, 512], BF16)
            bnst = T("bnst", [128, 6]); bnag = T("bnag", [128, 2]); lnr = T("lnr", [128, 2])
        bS = [banks[0], banks[1]]; bAh = [banks[2], banks[3]]; bEh = [banks[4], banks[5]]; bH = banks[6]; bYg = banks[7]
        ptb = [banks[4][:, 0:256].bitcast(BF16), banks[5][:, 0:256].bitcast(BF16)]
        ptm = banks[7][:, 0:256].bitcast(BF16)

        dbg_el = dbg_tensor("el", [10, 128, GRP]) if (dbg and stage == 1) else None
        dbg_yt = dbg_tensor("yt", [4, 4, 128, GRP]) if (dbg and stage in (1, 2)) else None
        dbg_H = dbg_tensor("H", [4, 128, 128]) if (dbg and stage in (1, 2)) else None
        dbg_ya = dbg_tensor("ya", [4, 8, 128, GRP]) if (dbg and stage == 2) else None
        pj_cnt = [0]
        dump_n = [0]

        def dump(name, ap, shape, deps):
            if not (dbg and stage == 1):
                return
            dt_ = dbg_tensor(name, shape)
            st_ = T(f"dump{dump_n[0]}", shape); dump_n[0] += 1
            K.cp(st_[:], ap, deps, [st_])
            K.dma("sp", dt_, st_[:], [st_], ())


        def proj_shift(cc, dst, e2="dve"):
            bk = banks[pj_cnt[0] % 2]; pj_cnt[0] += 1
            for kc in range(8):
                K.mm(bk[:, :], winb[:, kc, cc * 128:(cc + 1) * 128], hTt[:, kc, :], [winb, hTt], [bk], start=(kc == 0), stop=(kc == 7), inc=(kc == 7))
            K.cp(shtmp[:, 0:1], carry[:, cc:cc + 1], [carry], [shtmp], e="pool")
            K.act(shtmp[:, 1:GRP + 1], bk[:, :], AF.Copy, [bk, mu_t], [shtmp], scale=mu_t[:, cc:cc + 1])
            K.cp(carry[:, cc:cc + 1], shtmp[:, GRP:GRP + 1], [shtmp], [carry], e="pool")
            K.stt(dst[:], bk[:, :], omm_t[:, cc:cc + 1], shtmp[:, 0:GRP], ALU.mult, ALU.add, [bk, omm_t, shtmp], [dst])

        g_first = 0 if stage != 1 else 0
        for g in range(g_first, NGRP):
            seg = g // 4
            own = g >= OWN_G0
            for tt in range(4):
                r0 = g * GRP + tt * 128
                K.dma("sp", xt[:, tt, :], xw[r0:r0 + 128, :], (), [xt])
            for tt in range(4):
                K.act(uT[:, 0:2, :].rearrange("p a b -> p (a b)"), xt[:, tt, :], AF.Square, [xt], [uT, ssq], accum_out=ssq[:, tt:tt + 1])
            K.ts(rstd[:], ssq[:], 1.0 / D, 1e-6, ALU.mult, ALU.add, [ssq], [rstd])
            K.act(rstd[:], rstd[:], AF.Sqrt, [rstd], [rstd])
            K.op("dve", lambda E: E.reciprocal(out=rstd[:], in_=rstd[:]), [rstd], [rstd])
            for tt in range(4):
                K.act(xy[:, tt * D:(tt + 1) * D], xt[:, tt, :], AF.Copy, [xt, rstd], [xy], scale=rstd[:, tt:tt + 1])
            for kc in range(8):
                pbk = banks[4 + kc % 2]
                pz = ptb[kc % 2]
                for tt in range(4):
                    K.tr(pz[:, tt * 128:(tt + 1) * 128], xy[:, tt * D + kc * 128: tt * D + (kc + 1) * 128], identb[:], [xy, identb], [pbk], inc=(tt == 3))
                K.act(hTt[:, kc, :], pz, AF.Identity, [pbk, A1f, B1f], [hTt],
                      scale=A1f[:, seg + 1, kc:kc + 1], bias=B1f[:, seg + 1, kc:kc + 1])

            if g == OWN_G0 - 1:
                for cc_ in (0, 1, 2, 3, 13):
                    proj_shift(cc_, sh13)
            proj_shift(12, sh12)
            K.act(twxa[0:64, :], sh12[0:64, :], AF.Tanh, [sh12], [twxa])
            K.cp(twxa[64:128, :], sh12[64:128, :], [sh12], [twxa], e="pool")
            if own:
                proj_shift(13, sh13)
                K.act(sgb[:], sh13[:], AF.Sigmoid, [sh13], [sgb])
            for hp in range(4):
                proj_shift(4 + hp, shk4[hp])
                K.act(sqb[hp][:], shk4[hp][:], AF.Square, [shk4[hp], v4], [sqb[hp]], scale=v4[:, 2, hp:hp + 1])
            for hp in range(4):
                K.mm(banks[2][0:8, :], hsel[:, hp, :], sqb[hp][:], [hsel, sqb[hp]], [banks[2]], start=(hp == 0), stop=(hp == 3), inc=(hp == 3))
            K.ts(ss8[:], banks[2][0:8, :], 1e-18, None, ALU.max, None, [banks[2]], [ss8])
            K.act(ss8[:], ss8[:], AF.Ln, [ss8], [ss8])
            K.act(rn8[:], ss8[:], AF.Exp, [ss8], [rn8], scale=-0.5)

            for hp in range(4):
                shk = shk4[hp]
                w0c = v4[:, 0, hp:hp + 1]; a0c = v4[:, 1, hp:hp + 1]; kkc = v4[:, 2, hp:hp + 1]; kac = v4[:, 3, hp:hp + 1]
                K.mm(banks[2][:, :], wlo[0:64, hp * 128:(hp + 1) * 128], twxa[0:64, :], [wlo, twxa], [banks[2]])
                K.mm(banks[3][:, :], wlo[64:128, hp * 128:(hp + 1) * 128], twxa[64:128, :], [wlo, twxa], [banks[3]])
                K.act(lwp[:], banks[2][:, :], AF.Sigmoid, [banks[2], v4], [lwp], bias=w0c)
                K.act(av[:], banks[3][:, :], AF.Sigmoid, [banks[3], v4], [av], bias=a0c)
                if own:
                    K.mm(banks[0][:, :], wgb[:, hp * 128:(hp + 1) * 128], sgb[:], [wgb, sgb], [banks[0]])
                    K.cp(gT[:], banks[0][:, :], [banks[0]], [gT], e="act")
                proj_shift(8 + hp, shv)
                K.mm(banks[2][:, :], hselT[:, hp, :], rn8[:], [hselT, rn8], [banks[2]])
                K.stt(kkn[:], shk[:], kkc, banks[2][:, :], ALU.mult, ALU.mult, [shk, v4, banks[2]], [kkn])
                K.ts(tmpa[:], av[:], kac, omka[:, hp:hp + 1], ALU.mult, ALU.add, [av, v4, omka], [tmpa])
                K.tt(kmod[:], shk[:], tmpa[:], ALU.mult, [shk, tmpa], [kmod])
                K.tt(bb[:], kkn[:], av[:], ALU.mult, [kkn, av], [bb], e="pool")
                K.op("dve", lambda E: E.tensor_tensor_scan(out=cum[:], data0=restart[:], data1=lwp[:], initial=0.0, op0=ALU.mult, op1=ALU.add),
                     [restart, lwp], [cum])
                K.tt(tmpb[:], cum[:], lwp[:], ALU.subtract, [cum, lwp], [tmpb], e="pool")
                K.act(ep[:], cum[:], AF.Exp, [cum], [ep], scale=-C0)
                K.act(em[:], cum[:], AF.Exp, [cum], [em], scale=C0)
                K.act(eq[:], tmpb[:], AF.Exp, [tmpb], [eq], scale=-C0)
                epv = ep[:].rearrange("p (c t) -> p c t", t=64)
                K.cp(pc[:], epv[:, :, 63], [ep], [pc])
                pcb = epv[:, :, 63:64].broadcast_to([128, 8, 64])
                v3 = lambda t_: t_[:].rearrange("p (c t) -> p c t", t=64)
                K.tt(tmpa[:], bb[:], em[:], ALU.mult, [bb, em], [tmpa])
                K.cp(btb[:], tmpa[:], [tmpa], [btb], e="pool")
                K.tt(v3(bhb), v3(tmpa), pcb, ALU.mult, [tmpa, ep], [bhb])
                K.tt(tmpb[:], kmod[:], em[:], ALU.mult, [kmod, em], [tmpb])
                K.cp(ktb[:], tmpb[:], [tmpb], [ktb], e="pool")
                K.tt(v3(khb), v3(tmpb), pcb, ALU.mult, [tmpb, ep], [khb])
                K.stt(atb[:], kkn[:], -1.0, eq[:], ALU.mult, ALU.mult, [kkn, eq], [atb])
                K.cp(vbb[:], shv[:], [shv], [vbb], e="act")
                if own:
                    proj_shift(hp, shr)
                    K.tt(rtf[:], shr[:], ep[:], ALU.mult, [shr, ep], [rtf])
                    K.cp(rtb[:], rtf[:], [rtf], [rtb], e="pool")
                    K.tt(rkb[:], shr[:], kmod[:], ALU.mult, [shr, kmod], [rkb])
                    K.mm(banks[3][:, :], lrk[:, hp, :], rkb[:], [lrk, rkb], [banks[3]])
                    K.tt(bv[:], banks[3][:, :], shv[:], ALU.mult, [banks[3], shv], [bv])
                if dbg_el is not None and g == NGRP - 1 and hp == 1:
                    for i, t_ in enumerate((lwp, av, kkn, kmod, bb, cum, shv, gT, shr, bv)):
                        K.dma("sp", dbg_el[i], t_[:], [t_], ())

                for cp_ in range(4):
                    tok = slice(cp_ * 128, (cp_ + 1) * 128)
                    TMt = TM[cp_ % 2]; ZBt = ZB[cp_ % 2]
                    for i, src in enumerate((atb, bhb, khb, vbb)):
                        K.tr(ptm[:, i * 128:(i + 1) * 128], src[:, tok], identb[:], [src, identb], [bYg], inc=(i == 3))
                    K.cp(TMt[:].rearrange("p a b -> p (a b)"), ptm[:, 128:512], [bYg], [TMt])
                    for h in range(2):
                        K.cp(ZBt[h][:, 1, :], ptm[:, h * 64:(h + 1) * 64], [bYg], [ZBt[h]], e="act")
                    for h in range(2):
                        hb = slice(h * 64, (h + 1) * 64)
                        for q in range(2):
                            qp = slice(q * 64, (q + 1) * 64)
                            tq = slice(cp_ * 128 + q * 64, cp_ * 128 + (q + 1) * 64)
                            cq = slice(q * 64, (q + 1) * 64)
                            K.mm(bS[h][qp, q * 64:(q + 1) * 64], btb[hb, tq], atb[hb, tq], [btb, atb], [bS[h]], inc=False)
                            K.mm(bS[h][qp, 128 + q * 64:128 + (q + 1) * 64], atb[hb, tq], btb[hb, tq], [btb, atb], [bS[h]], inc=False)
                            K.mm(bS[h][qp, 256 + q * 64:256 + (q + 1) * 64], ktb[hb, tq], atb[hb, tq], [ktb, atb], [bS[h]], inc=(q == 1 and not own))
                            if own:
                                K.mm(bS[h][qp, 384 + q * 64:384 + (q + 1) * 64], btb[hb, tq], rtb[hb, tq], [btb, rtb], [bS[h]], inc=(q == 1))
                        XQc = XQ[h][0]
                        K.tt(XQc[:, 1, :], bS[h][:, 0:128], maskX[:, 0:128], ALU.mult, [bS[h], maskX], [XQc])
                        K.tt(XQc[:, 0, :], bS[h][:, 128:256], maskX[:, 128:256], ALU.mult, [bS[h], maskX], [XQc])
                        K.tt(XQc[:, 2, :], XQc[:, 1, :], identb[:], ALU.add, [XQc, identb], [XQc], e="pool")
                        ny = 256 if own else 128
                        K.tt(YS[h][:, 0:ny], bS[h][:, 256:256 + ny], maskY[:, 0:ny], ALU.mult, [bS[h], maskY], [YS[h]])
                    if own:
                        for h in range(2):
                            hb = slice(h * 64, (h + 1) * 64)
                            for q in range(2):
                                qp = slice(q * 64, (q + 1) * 64)
                                tq = slice(cp_ * 128 + q * 64, cp_ * 128 + (q + 1) * 64)
                                K.mm(bEh[h][qp, 128 + q * 64:128 + (q + 1) * 64], ktb[hb, tq], rtb[hb, tq], [ktb, rtb], [bEh[h]], inc=(q == 1))
                            K.tt(YS[h][:, 256:384], bEh[h][:, 128:256], maskY[:, 256:384], ALU.mult, [bEh[h], maskY], [YS[h]])
                    for it in range(0, 6):
                        for h in range(2):
                            Xc = XQ[h][it % 2]; Xn = XQ[h][1 - it % 2]
                            A_ = bAh[h]
                            if it == 0:
                                K.mm(A_[:, 128:256], Xc[:, 0, :], Xc[:, 1, :], [Xc], [A_], inc=False)
                                K.mm(A_[:, 0:128], Xc[:, 1, :], Xc[:, 0, :], [Xc], [A_])
                                K.cp(Xn[:, 0:2, :].rearrange("p a t -> p (a t)"), A_[:, 0:256], [A_], [Xn], e="act")
                                K.cp(Xn[:, 2, :], Xc[:, 2, :], [Xc], [Xn], e="pool")
                            elif it < 5:
                                K.mm(A_[:, 128:384], Xc[:, 0, :], Xc[:, 1:3, :].rearrange("p a t -> p (a t)"), [Xc], [A_], inc=False)
                                K.mm(A_[:, 0:128], Xc[:, 1, :], Xc[:, 0, :], [Xc], [A_])
                                K.cp(Xn[:, 0:2, :].rearrange("p a t -> p (a t)"), A_[:, 0:256], [A_], [Xn], e="act")
                                K.tt(Xn[:, 2, :], A_[:, 256:384], Xc[:, 2, :], ALU.add, [A_, Xc], [Xn])
                            else:
                                K.mm(A_[:, 256:384], Xc[:, 0, :], Xc[:, 2, :], [Xc], [A_])
                                K.tt(Xn[:, 2, :], A_[:, 256:384], Xc[:, 2, :], ALU.add, [A_, Xc], [Xn])
                    for h in range(2):
                        Rh = XQ[h][0][:, 2, :]
                        K.mm(bAh[h][:, 384:448], YS[h][:, 0:128], TMt[:, 2, h * 64:(h + 1) * 64], [YS[h], TMt], [bAh[h]])
                        K.cp(ZBt[h][:, 0, :], bAh[h][:, 384:448], [bAh[h]], [ZBt[h]], e="act")
                        K.mm(bEh[h][:, 0:128], Rh, ZBt[h][:].rearrange("p a v -> p (a v)"), [XQ[h][0], ZBt[h]], [bEh[h]])
                        K.cp(UW[h][:].rearrange("p a v -> p (a v)"), bEh[h][:, 0:128], [bEh[h]], [UW[h]], e=("dve" if h == 0 else "act"))
                    for q in range(2):
                        ck = cp_ * 2 + q
                        qp = slice(q * 64, (q + 1) * 64)
                        tq = slice(cp_ * 128 + q * 64, cp_ * 128 + (q + 1) * 64)
                        MTt = MT[q]
                        Hc = Hb[hp][hpar[hp]]; Hn = Hb[hp][1 - hpar[hp]]
                        hreg = bH[:, q * 128:(q + 1) * 128]
                        if own:
                            RhTt = RhT[q]
                            yreg = bYg[:, 256 + q * 64:256 + (q + 1) * 64]
                            rreg = bYg[:, 384 + q * 64:384 + (q + 1) * 64]
                            for h in range(2):
                                hs_ = slice(h * 64, (h + 1) * 64)
                                K.mm(rreg[hs_, :], UW[h][qp, 1, :], YS[h][qp, 128 + q * 64:128 + (q + 1) * 64], [UW[h], YS[h]], [bYg], inc=(h == 1))
                            K.tt(RhTt[:], rreg, rtf[:, tq], ALU.add, [bYg, rtf], [RhTt])
                        for h in range(2):
                            hs_ = slice(h * 64, (h + 1) * 64)
                            K.mm(bH[hs_, 384 + h * 64:384 + (h + 1) * 64], UW[h][qp, 1, :], TMt[qp, 0, hs_], [UW[h], TMt], [bH], inc=(h == 1))
                        K.stt(MTt[:], ident[:], pc[:, ck:ck + 1], bH[:, 384:512], ALU.mult, ALU.add, [ident, pc, bH], [MTt])
                        for h in range(2):
                            hs_ = slice(h * 64, (h + 1) * 64)
                            K.mm(bH[hs_, q * 128 + h * 64:q * 128 + (h + 1) * 64], TMt[qp, 0, hs_], UW[h][qp, 0, :], [TMt, UW[h]], [bH], start=True, stop=False, inc=False)
                            K.mm(bH[hs_, q * 128 + h * 64:q * 128 + (h + 1) * 64], TMt[qp, 1, hs_], TMt[qp, 2, hs_], [TMt], [bH], start=False, stop=False, inc=False)
                        if own:
                            for h in range(2):
                                hs_ = slice(h * 64, (h + 1) * 64)
                                K.mm(yreg[hs_, :], UW[h][qp, 0, :], YS[h][qp, 128 + q * 64:128 + (q + 1) * 64], [UW[h], YS[h]], [bYg], start=True, stop=False, inc=False)
                                K.mm(yreg[hs_, :], TMt[qp, 2, hs_], YS[h][qp, 256 + q * 64:256 + (q + 1) * 64], [TMt, YS[h]], [bYg], start=False, stop=False, inc=False)
                            K.mm(yreg, Hc[:], RhTt[:], [Hc, RhTt], [bYg], start=False, stop=True)
                            K.cp(YT[:, tq], yreg, [bYg], [YT], e="act")
                        K.mm(hreg, MTt[:], Hc[:], [MTt, Hc], [bH], start=False, stop=True)
                        K.cp(Hn[:], hreg, [bH], [Hn])
                        hpar[hp] = 1 - hpar[hp]
                        if g == 0 and hp == 0 and cp_ == 0:
                            dump(f"MT{q}", MTt[:], [128, 128], [MTt]); dump(f"Hn{q}", Hn[:], [128, 128], [Hn])
                if dbg_yt is not None and own:
                    K.dma("sp", dbg_yt[g - OWN_G0, hp], YT[:], [YT], ())
                if dbg and stage == 1 and hp == 0:
                    dump(f"Hg{g}", Hb[0][hpar[0]][:], [128, 128], [Hb[0][hpar[0]]])
                if own_extra and own:
                    lgc = v4[:, 4, hp:hp + 1]; lbc = v4[:, 5, hp:hp + 1]
                    K.mm(banks[2][:, :], blk64[:], YT[:], [blk64, YT], [banks[2]])
                    K.tt(tmpa[:], YT[:], banks[2][:, :], ALU.subtract, [YT, banks[2]], [tmpa])
                    K.act(tmpb[:], tmpa[:], AF.Square, [tmpa], [tmpb])
                    K.mm(banks[3][:, :], blk64[:], tmpb[:], [blk64, tmpb], [banks[3]])
                    K.act(tmpb[:], banks[3][:, :], AF.Ln, [banks[3], epsln], [tmpb], bias=epsln[:, 0:1])
                    K.act(tmpb[:], tmpb[:], AF.Exp, [tmpb], [tmpb], scale=-0.5)
                    K.tt(tmpa[:], tmpa[:], tmpb[:], ALU.mult, [tmpa, tmpb], [tmpa])
                    K.ts(tmpa[:], tmpa[:], lgc, lbc, ALU.mult, ALU.add, [tmpa, v4], [tmpa])
                    K.tt(tmpa[:], tmpa[:], bv[:], ALU.add, [tmpa, bv], [tmpa], e="pool")
                    K.tt(xy[:, hp * GRP:(hp + 1) * GRP], tmpa[:], gT[:], ALU.mult, [tmpa, gT], [xy])
            if own_extra and own:
                for i in range(4):
                    bk = banks[i % 2]
                    for kc in range(8):
                        K.mm(bk[:, :], winb[:, kc, 1792 + i * 128:1792 + (i + 1) * 128], hTt[:, kc, :], [winb, hTt], [bk], start=(kc == 0), stop=(kc == 7), inc=(kc == 7))
                    K.act(uT[:, i, :], bk[:, :], AF.Gelu_apprx_tanh, [bk], [uT])
                spb = [banks[2], banks[3], banks[4], banks[5]]
                for tt in range(4):
                    for kc in range(8):
                        K.mm(banks[0][:, :], hTt[:, kc, tt * 128:(tt + 1) * 128], winb[:, kc, 2304:2816], [winb, hTt], [banks[0]], start=(kc == 0), stop=(kc == 7), inc=(kc == 7))
                    K.act(vtm[:], banks[0][:, :], AF.Gelu_apprx_tanh, [banks[0]], [vtm])
                    K.op("dve", lambda E: E.bn_stats(out=bnst[:], in_=vtm[:]), [vtm], [bnst])
                    K.op("dve", lambda E: E.bn_aggr(out=bnag[:], in_=bnst[:]), [bnst], [bnag])
                    K.ts(lnr[:, 0:1], bnag[:, 1:2], 1e-5, None, ALU.add, None, [bnag], [lnr])
                    K.act(lnr[:, 0:1], lnr[:, 0:1], AF.Sqrt, [lnr], [lnr])
                    K.op("dve", lambda E: E.reciprocal(out=lnr[:, 1:2], in_=lnr[:, 0:1]), [lnr], [lnr])
                    K.ts(vtm[:], vtm[:], bnag[:, 0:1], lnr[:, 1:2], ALU.subtract, ALU.mult, [vtm, bnag, lnr], [vtm])
                    K.tt(vtm[:], vtm[:], lngr[:], ALU.mult, [vtm, lngr], [vtm], e="pool")
                    K.tt(vnb[:, tt, :], vtm[:], lnbr[:], ALU.add, [vtm, lnbr], [vnb], e="pool")
                    for h in range(8):
                        K.mm(spb[h // 2][(h % 2) * 64:(h % 2) * 64 + 64, tt * 128:(tt + 1) * 128], vnb[:, tt, h * 64:(h + 1) * 64], wsTb[:, h, :],
                             [vnb, wsTb], [spb[h // 2]], inc=(h % 2 == 1))
                for hp in range(4):
                    K.tt(tmpa[:].rearrange("p (r t) -> p r t", r=4), spb[hp][:, :].rearrange("p (r t) -> p r t", r=4),
                         bsp[:, hp:hp + 1, :].broadcast_to([128, 4, 128]), ALU.add, [spb[hp], bsp], [tmpa])
                    K.tt(xy[:, (4 + hp) * GRP:(5 + hp) * GRP], tmpa[:], uT[:, hp, :], ALU.mult, [tmpa, uT], [xy])
                if dbg_ya is not None:
                    for i in range(8):
                        K.cp(tmpa[:], xy[:, i * GRP:(i + 1) * GRP], [xy], [tmpa])
                        K.dma("sp", dbg_ya[g - OWN_G0, i], tmpa[:], [tmpa], ())
                for tt in range(4):
                    for half in range(2):
                        bk = banks[half]
                        for kc in range(8):
                            K.mm(bk[:, :], xy[:, kc * GRP + tt * 128: kc * GRP + (tt + 1) * 128], woutb[:, kc, half * 512:(half + 1) * 512], [xy, woutb], [bk],
                                 start=(kc == 0), stop=(kc == 7), inc=(kc == 7))
                        K.tt(x2t[:, half * 512:(half + 1) * 512], bk[:, :], xt[:, tt, half * 512:(half + 1) * 512], ALU.add, [bk, xt], [x2t])
                    r0 = (g - OWN_G0) * GRP + tt * 128
                    K.dma("sp", x2d[r0:r0 + 128, :], x2t[:], [x2t], [x2dbuf])
        if dbg_H is not None:
            for hp in range(4):
                Hf = T(f"Hf{hp}", [128, 128])
                K.cp(Hf[:], Hb[hp][hpar[hp]][:], [Hb[hp][hpar[hp]]], [Hf])
                K.dma("sp", dbg_H[hp], Hf[:], [Hf], ())
        if stage <= 2:
            if stage == 2:
                for i in range(16):
                    K.dma("sp", x2t[:], x2d[i * 128:(i + 1) * 128, :], [x2dbuf], [x2t])
                    K.dma("sp", out[i * 128:(i + 1) * 128, :], x2t[:], [x2t], ())
            else:
                K.memset(x2t[:], 0.0, [x2t])
                for i in range(16):
                    K.dma("sp", out[i * 128:(i + 1) * 128, :], x2t[:], [x2t], ())
            finish()
            es_scan.__exit__(None, None, None)
            return nc, dbg_out
        allb = list(K.all_bufs)
        for e in ("pe", "act", "dve", "pool", "sp"):
            K._deps(e, (), allb)
        es_scan.__exit__(None, None, None)
        K.es = es

        acc = T("acc", [128, 16, D])
        h2T = T("h2T", [128, 8, SEG], BF16)
        gates = T("gates", [128, 16, NE + 1])
        ebr = T("ebr", [128, NE]); nfr = T("nfr", [128, D]); wrt = T("wrt", [128, 8, NE])
        K.dma("sp", ebr[:], e_bias[0, :].partition_broadcast(128), (), [ebr])
        K.dma("sp", nfr[:], nfg[0, :].partition_broadcast(128), (), [nfr])
        K.dma("sp", wrt[:], w_rt.rearrange("(kc p) n -> p kc n", p=128), (), [wrt])
        xnf = T("xnf", [128, D]); h2f = T("h2f", [128, 8, 128])
        sc = T("sc", [128, NE]); bi = T("bi", [128, NE]); mk = T("mk", [128, NE]); m8 = T("m8", [128, 8, 8])
        gs = T("gs", [128, 8]); gs8 = T("gs8", [128, 8]); gm = T("gm", [128, 8]); pen = T("pen", [128, 8]); t8 = T("t8", [128, 8])
        ssq2 = T("ssq2", [128, 1]); rs2 = T("rs2", [128, 1]); rsum = T("rsum", [128, 1]); junk2 = T("junk2", [128, D], BF16)
        dbg_g = dbg_tensor("gates", [16, 128, NE + 1]) if dbg else None

        def rms_rstd(t):
            K.act(junk2[:], acc[:, t, :], AF.Square, [acc.sub(t)], [junk2, ssq2], accum_out=ssq2[:, 0:1])
            K.ts(rs2[:], ssq2[:], 1.0 / D, 1e-6, ALU.mult, ALU.add, [ssq2], [rs2])
            K.act(rs2[:], rs2[:], AF.Sqrt, [rs2], [rs2])
            K.op("dve", lambda E: E.reciprocal(out=rs2[:], in_=rs2[:]), [rs2], [rs2])

        for t in range(16):
            K.dma("sp", acc[:, t, :], x2d[t * 128:(t + 1) * 128, :], [x2dbuf], [acc.sub(t)])
            rms_rstd(t)
            K.act(xnf[:], acc[:, t, :], AF.Copy, [acc.sub(t), rs2], [xnf], scale=rs2[:, 0:1])
            for kc in range(8):
                bk = banks[kc // 4]
                K.tr(bk[:, (kc % 4) * 128:(kc % 4 + 1) * 128], xnf[:, kc * 128:(kc + 1) * 128], ident[:], [xnf, ident], [bk], inc=(kc % 4 == 3))
            for kc in range(8):
                bk = banks[kc // 4]; reg = bk[:, (kc % 4) * 128:(kc % 4 + 1) * 128]
                K.act(h2T[:, kc, t * 128:(t + 1) * 128], reg, AF.Identity, [bk, A2, modT], [h2T.sub(t)], scale=A2[:, kc:kc + 1], bias=modT[:, 24 + kc:25 + kc])
                K.ts(h2f[:, kc, :], reg, A2[:, kc:kc + 1], modT[:, 24 + kc:25 + kc], ALU.mult, ALU.add, [bk, A2, modT], [h2f])
            for kc in range(8):
                K.mm(banks[2][:, 0:NE], h2f[:, kc, :], wrt[:, kc, :], [h2f, wrt], [banks[2]], start=(kc == 0), stop=(kc == 7), inc=(kc == 7))
            K.act(sc[:], banks[2][:, 0:NE], AF.Sigmoid, [banks[2]], [sc])
            K.tt(bi[:], sc[:], ebr[:], ALU.add, [sc, ebr], [bi])
            for gi in range(8):
                K.op("dve", lambda E: E.max(out=m8[:, gi, :], in_=bi[:, gi * 8:(gi + 1) * 8]), [bi], [m8])
            K.tt(gs[:], m8[:, :, 0], m8[:, :, 1], ALU.add, [m8], [gs])
            K.op("dve", lambda E: E.max(out=gs8[:], in_=gs[:]), [gs], [gs8])
            K.ts(gm[:], gs[:], gs8[:, 3:4], None, ALU.is_ge, None, [gs, gs8], [gm])
            K.ts(pen[:], gm[:], 1e9, -1e9, ALU.mult, ALU.add, [gm], [pen])
            mkv = mk[:].rearrange("p (g e) -> p g e", e=8); biv = bi[:].rearrange("p (g e) -> p g e", e=8)
            K.tt(mkv, biv, gm[:].unsqueeze(2).broadcast_to([128, 8, 8]), ALU.mult, [bi, gm], [mk])
            K.tt(mkv, mkv, pen[:].unsqueeze(2).broadcast_to([128, 8, 8]), ALU.add, [mk, pen], [mk])
            K.op("dve", lambda E: E.max(out=t8[:], in_=mk[:]), [mk], [t8])
            K.ts(mk[:], mk[:], t8[:, 7:8], None, ALU.is_ge, None, [mk, t8], [mk])
            K.tt(mk[:], mk[:], sc[:], ALU.mult, [mk, sc], [mk])
            K.op("dve", lambda E: E.tensor_reduce(out=rsum[:], in_=mk[:], axis=AX.X, op=ALU.add), [mk], [rsum])
            K.op("dve", lambda E: E.reciprocal(out=rsum[:], in_=rsum[:]), [rsum], [rsum])
            K.ts(gates[:, t, 0:NE], mk[:], rsum[:, 0:1], 2.5, ALU.mult, ALU.mult, [mk, rsum], [gates])
            K.memset(gates[:, t, NE:NE + 1], 1.0, [gates])
            if dbg_g is not None:
                K.dma("sp", dbg_g[t], gates[:, t, :], [gates], ())

        w1b = [T(f"w1b{i}", [128, 8, DE], BF16) for i in range(2)]
        w3b = [T(f"w3b{i}", [128, 8, DE], BF16) for i in range(2)]
        w2b = [T(f"w2b{i}", [128, 2, D], BF16) for i in range(2)]
        actb = [T(f"actb{i}", [128, 2, GRP], BF16) for i in range(2)]
        sgt = [T(f"sgt{i}", [128, GRP]) for i in range(2)]
        n_exp = NE + 1
        for e in range(n_exp):
            i = e % 2
            for hf in range(2):
                K.dma("pool", w1b[i][:, hf * 4:(hf + 1) * 4, :], w1e[e, hf * 512:(hf + 1) * 512, :].rearrange("(kc p) n -> p kc n", p=128), (), [w1b[i]])
                K.dma("pool", w3b[i][:, hf * 4:(hf + 1) * 4, :], w3e[e, hf * 512:(hf + 1) * 512, :].rearrange("(kc p) n -> p kc n", p=128), (), [w3b[i]])
                K.dma("pool", w2b[i][:, hf, :], w2e[e, hf * 128:(hf + 1) * 128, :], (), [w2b[i]])
            for hf in range(2):
                K.tt(w2b[i][:, hf, :], w2b[i][:, hf, :], g2rep[:], ALU.mult, [w2b[i], g2rep], [w2b[i]], e="pool")
            for tg in range(4):
                ab = actb[tg % 2]
                h2r = [h2T.sub(4 * tg + j) for j in range(4)]
                for cc in range(2):
                    bG = banks[cc * 2]; bU = banks[cc * 2 + 1]
                    for kc in range(8):
                        K.mm(bG[:, :], w1b[i][:, kc, cc * 128:(cc + 1) * 128], h2T[:, kc, tg * GRP:(tg + 1) * GRP], [w1b[i]] + h2r, [bG],
                             start=(kc == 0), stop=(kc == 7), inc=(kc == 7))
                    for kc in range(8):
                        K.mm(bU[:, :], w3b[i][:, kc, cc * 128:(cc + 1) * 128], h2T[:, kc, tg * GRP:(tg + 1) * GRP], [w3b[i]] + h2r, [bU],
                             start=(kc == 0), stop=(kc == 7), inc=(kc == 7))
                    K.act(sgt[cc][:], bG[:, :], AF.Silu, [bG], [sgt[cc]])
                    K.tt(ab[:, cc, :], sgt[cc][:], bU[:, :], ALU.mult, [sgt[cc], bU], [ab])
                for tt in range(4):
                    t = tg * 4 + tt
                    for half in range(2):
                        bO = banks[4 + (tt % 2) * 2 + half]
                        for cc in range(2):
                            K.mm(bO[:, :], ab[:, cc, tt * 128:(tt + 1) * 128], w2b[i][:, cc, half * 512:(half + 1) * 512], [ab, w2b[i]], [bO],
                                 start=(cc == 0), stop=(cc == 1), inc=(cc == 1))
                        K.stt(acc[:, t, half * 512:(half + 1) * 512], bO[:, :], gates[:, t, e:e + 1], acc[:, t, half * 512:(half + 1) * 512],
                              ALU.mult, ALU.add, [bO, gates, acc.sub(t)], [acc.sub(t)])
        for t in range(16):
            rms_rstd(t)
            K.stt(xnf[:], acc[:, t, :], rs2[:, 0:1], nfr[:], ALU.mult, ALU.mult, [acc.sub(t), rs2, nfr], [xnf])
            K.dma("sp", out[t * 128:(t + 1) * 128, :], xnf[:], [xnf], ())
        finish()
    return nc, dbg_out


def _fm(v, n):
    return np.ascontiguousarray(np.asarray(v, np.float32).reshape(n, 128).T)


def make_in_maps(inputs, small=False):
    g = lambda k: np.asarray(inputs[k], np.float32)
    x = g("x"); c = g("c")
    consts = _consts()
    shared = {
        "w_ada": g("w_ada")[0], "b_ada": _fm(g("b_ada")[0], 48), "n1g": _fm(g("norm1_g")[0], 8),
        "w_in": g("w_in")[0], "mu": _fm(g("mu_shift")[0], 14),
        "vec4": np.ascontiguousarray(np.stack([_fm(g("w0")[0], 4), _fm(g("a0")[0], 4), _fm(g("k_k")[0], 4), _fm(g("k_a")[0], 4),
                                               _fm(g("lnx_g")[0], 4), _fm(g("lnx_b")[0], 4), _fm(g("r_k")[0].reshape(-1), 4)], axis=1)),
        "w_dec": g("w_decay_up")[0], "w_aup": g("w_a_up")[0], "w_gup": g("w_g_up")[0],
        "sgu_g": g("sgu_ln_g")[0][None, :], "sgu_b": g("sgu_ln_b")[0][None, :],
        "w_sp": g("w_spatial")[0], "b_sp": g("b_spatial")[0], "w_out": g("w_out")[0], "n2g": _fm(g("norm2_g")[0], 8),
        "w_rt": g("w_router")[0], "e_bias": g("e_bias")[0][None, :],
        "nfg": g("norm_f_g")[None, :],
    }
    if small:
        shared["w1e"] = np.zeros((1, D, DE), np.float32); shared["w3e"] = np.zeros((1, D, DE), np.float32); shared["w2e"] = np.zeros((1, DE, D), np.float32)
    else:
        shared["w1e"] = np.concatenate([g("w1_e")[0], g("w1_s")], axis=0)
        shared["w3e"] = np.concatenate([g("w3_e")[0], g("w3_s")], axis=0)
        shared["w2e"] = np.concatenate([g("w2_e")[0], g("w2_s")], axis=0)
    for k, v in consts.items():
        shared["c_" + k] = v
    maps = []
    for core in range(8):
        b, j = core // 4, core % 4
        win = np.zeros((NSEG * SEG, D), np.float32)
        lo = (j - 3) * SEG
        src0 = max(lo, 0)
        win[src0 - lo:] = x[b, src0:(j + 1) * SEG]
        fl = np.zeros((128, 5), np.float32)
        for s in range(5):
            seg_global = j - 4 + s
            fl[:, s] = 1.0 if seg_global >= 0 else 0.0
        m = dict(shared)
        m["xw"] = win
        m["flags"] = fl
        m["c_fm"] = _fm(c[b], 8)
        maps.append(m)
    return maps


_NC_CACHE = {}


def kernel(**inputs):
    if "nc" not in _NC_CACHE:
        _NC_CACHE["nc"] = build_nc()[0]
    nc = _NC_CACHE["nc"]
    maps = make_in_maps(inputs)
    res = run_bass_kernel_spmd(nc, maps, core_ids=list(range(8)))
    outp = np.zeros((2, 4 * SEG, D), np.float32)
    for core in range(8):
        b, j = core // 4, core % 4
        outp[b, j * SEG:(j + 1) * SEG] = res.results[core]["out"]
    return outp
```

```python
import numpy as np
import ml_dtypes
from contextlib import ExitStack

import concourse.bass as bass
import concourse.mybir as mybir
from concourse.bass_utils import run_bass_kernel_spmd

F32 = mybir.dt.float32
BF16 = mybir.dt.bfloat16
AF = mybir.ActivationFunctionType
ALU = mybir.AluOpType
AX = mybir.AxisListType

D = 1024
SEG = 2048
NSEG = 4
GRP = 512
NGRP = NSEG * SEG // GRP
OWN_G0 = (NSEG - 1) * SEG // GRP
C0 = 0.6065306597126334
NE = 64
DE = 256
SAME_SYNC = True


class Buf:
    def __init__(self, name):
        self.name = name
        self.psum = False
        self.w = None
        self.r = {}


class Tile(Buf):
    def __init__(self, ctx, name, shape, dtype, psum=False):
        super().__init__(name)
        self.psum = psum
        ctx.all_bufs.append(self)
        alloc = ctx.nc.psum_tensor if psum else ctx.nc.sbuf_tensor
        self.t = ctx.es.enter_context(alloc(name, list(shape), dtype))
        self.subs = {}

    def __getitem__(self, idx):
        return self.t[idx]

    def sub(self, key):
        if key not in self.subs:
            self.subs[key] = Buf(f"{self.name}.{key}")
        return self.subs[key]


class Ctx:
    NDS = 24

    def __init__(self, nc, es):
        self.nc = nc
        self.es = es
        self.eng = {"pe": nc.tensor, "act": nc.scalar, "dve": nc.vector, "pool": nc.gpsimd, "sp": nc.sync}
        self.sem = {k: es.enter_context(nc.semaphore("s_" + k)) for k in ("pe", "act", "dve", "pool")}
        self.cnt = {k: 0 for k in self.sem}
        self.seen = {k: {} for k in self.eng}
        self.dsem = [es.enter_context(nc.semaphore(f"s_dma{i}")) for i in range(self.NDS)]
        self.dcnt = [0] * self.NDS
        self.dnext = 0
        self.pending = {k: [] for k in self.sem}
        self.n_inst = 0
        self.all_bufs = []

    def _wait(self, e, h):
        kind, key, val = h
        if kind == "e" and key == e and (e == "pe" or not SAME_SYNC):
            return
        sk = (kind, key)
        if self.seen[e].get(sk, 0) >= val:
            return
        sem = self.sem[key] if kind == "e" else self.dsem[key]
        self.eng[e].wait_ge(sem, val)
        self.seen[e][sk] = val

    def _deps(self, e, reads, writes):
        hs = {}
        for b in reads:
            if b.w is not None:
                k = b.w[:2]
                hs[k] = max(hs.get(k, 0), b.w[2])
        for b in writes:
            if b.w is not None:
                k = b.w[:2]
                hs[k] = max(hs.get(k, 0), b.w[2])
            for k, v in b.r.items():
                hs[k] = max(hs.get(k, 0), v)
        for k, v in hs.items():
            self._wait(e, (k[0], k[1], v))

    def _mark(self, h, reads, writes):
        k = h[:2]
        for b in reads:
            b.r[k] = max(b.r.get(k, 0), h[2])
        for b in writes:
            b.w = h
            b.r = {}

    def op(self, e, fn, reads=(), writes=(), inc=True):
        pr = [b for b in reads if b.psum]
        if pr:
            reads = [b for b in reads if not b.psum]
            writes = list(writes) + [b for b in pr if b not in writes]
        self._deps(e, reads, writes)
        inst = fn(self.eng[e])
        self.n_inst += 1
        if inc:
            self.cnt[e] += 1
            inst.then_inc(self.sem[e], 1)
            h = ("e", e, self.cnt[e])
        else:
            h = ("e", e, self.cnt[e] + 1)
        self._mark(h, reads, writes)
        return h

    def dma(self, q, out, in_, reads=(), writes=(), **kw):
        i = self.dnext
        self.dnext = (self.dnext + 1) % self.NDS
        if self.dcnt[i]:
            self._wait(q, ("d", i, self.dcnt[i]))
        self._deps(q, reads, writes)
        inst = self.eng[q].dma_start(out=out, in_=in_, **kw)
        self.n_inst += 1
        self.dcnt[i] += 16
        inst.then_inc(self.dsem[i], 16)
        h = ("d", i, self.dcnt[i])
        self._mark(h, reads, writes)
        return h

    def wait_all(self, e, bufs):
        self._deps(e, (), bufs)

    def mm(self, out, lhsT, rhs, reads, writes, start=True, stop=True, inc=True):
        return self.op("pe", lambda E: E.matmul(out, lhsT, rhs, start=start, stop=stop), reads, writes, inc=inc)

    def tr(self, out, in_, ident, reads, writes, inc=True):
        return self.op("pe", lambda E: E.transpose(out, in_, ident), reads, writes, inc=inc)

    def act(self, out, in_, func, reads, writes, bias=None, scale=None, e="act", accum_out=None):
        kw = {}
        if bias is not None:
            kw["bias"] = bias
        if scale is not None:
            kw["scale"] = scale
        if accum_out is not None:
            kw["accum_out"] = accum_out
        return self.op(e, lambda E: E.activation(out=out, in_=in_, func=func, **kw), reads, writes)

    def tt(self, out, in0, in1, op, reads, writes, e="dve"):
        return self.op(e, lambda E: E.tensor_tensor(out=out, in0=in0, in1=in1, op=op), reads, writes)

    def ts(self, out, in0, s1, s2, op0, op1, reads, writes, e="dve"):
        if op1 is None:
            return self.op(e, lambda E: E.tensor_scalar(out=out, in0=in0, scalar1=s1, scalar2=None, op0=op0), reads, writes)
        return self.op(e, lambda E: E.tensor_scalar(out=out, in0=in0, scalar1=s1, scalar2=s2, op0=op0, op1=op1), reads, writes)

    def stt(self, out, in0, scalar, in1, op0, op1, reads, writes):
        return self.op("dve", lambda E: E.scalar_tensor_tensor(out=out, in0=in0, scalar=scalar, in1=in1, op0=op0, op1=op1), reads, writes)

    def cp(self, out, in_, reads, writes, e="dve"):
        if e == "act":
            return self.op("act", lambda E: E.copy(out=out, in_=in_), reads, writes)
        return self.op(e, lambda E: E.tensor_copy(out=out, in_=in_), reads, writes)

    def memset(self, ap, val, writes, e="dve"):
        return self.op(e, lambda E: E.memset(ap, val), (), writes)


def _consts():
    c = {}
    ident = np.eye(128, dtype=np.float32)
    c["ident"] = ident
    s = np.arange(128)[:, None] % 64
    t = np.arange(64)[None, :]
    su = (t > s).astype(np.float32)
    ui = (t >= s).astype(np.float32)
    sl = (t < s).astype(np.float32)
    half = (np.arange(128)[:, None] // 64 == np.arange(128)[None, :] // 64).astype(np.float32)
    def bd(m64):
        return np.concatenate([m64, m64], axis=1) * half
    c["maskX"] = np.concatenate([bd(sl), bd(su), bd(sl), bd(su)], axis=1)
    c["maskY"] = np.concatenate([bd(su), bd(ui), bd(ui)], axis=1)
    c["blk"] = half.copy()
    restart = np.ones((128, GRP), np.float32)
    restart[:, ::64] = 0.0
    c["restart"] = restart
    hsel = np.zeros((128, 4, 8), np.float32)
    for p_ in range(128):
        for hp in range(4):
            hsel[p_, hp, 2 * hp + p_ // 64] = 1.0
    c["hsel"] = hsel
    c["hselT"] = np.ascontiguousarray(hsel.transpose(2, 1, 0))
    return c


CONST_SHAPES = {"ident": [128, 128], "maskX": [128, 512], "maskY": [128, 384], "blk": [128, 128], "restart": [128, GRP], "hsel": [128, 4, 8], "hselT": [8, 4, 128]}


def build_nc(stage=99, dbg=False):
    nc = bass.Bass("TRN2", target_bir_lowering=False)

    def din(name, shape, dt=F32):
        return nc.dram_tensor(name, list(shape), dt, kind="ExternalInput").ap()

    xw = din("xw", [NSEG * SEG, D])
    flags = din("flags", [128, 5])
    c_fm = din("c_fm", [128, 8])
    w_ada = din("w_ada", [D, 6 * D])
    b_ada = din("b_ada", [128, 48])
    n1g = din("n1g", [128, 8])
    w_in = din("w_in", [D, 2816])
    mu = din("mu", [128, 14])
    vec4 = din("vec4", [128, 7, 4])
    w_dec = din("w_dec", [64, 512])
    w_aup = din("w_aup", [64, 512])
    w_gup = din("w_gup", [128, 512])
    sgu_g = din("sgu_g", [1, 512])
    sgu_b = din("sgu_b", [1, 512])
    w_sp = din("w_sp", [8, 128, 128])
    b_sp = din("b_sp", [8, 128])
    w_out = din("w_out", [D, D])
    n2g = din("n2g", [128, 8])
    w_rt = din("w_rt", [D, NE])
    e_bias = din("e_bias", [1, NE])
    nexp = NE + 1 if stage >= 3 else 1
    w1e = din("w1e", [nexp, D, DE])
    w3e = din("w3e", [nexp, D, DE])
    w2e = din("w2e", [nexp, DE, D])
    nfg = din("nfg", [1, D])
    cst = {k: din("c_" + k, shp) for k, shp in CONST_SHAPES.items()}
    out = nc.dram_tensor("out", [SEG, D], F32, kind="ExternalOutput").ap()
    x2d = nc.dram_tensor("x2_scratch", [SEG, D], F32, kind="Internal").ap()
    dbg_out = {}
    x2dbuf = Buf("x2dbuf")

    def dbg_tensor(name, shape):
        dbg_out[name] = nc.dram_tensor("dbg_" + name, list(shape), F32, kind="ExternalOutput").ap()
        return dbg_out[name]

    es = ExitStack()
    with es:
        K = Ctx(nc, es)
        T = lambda name, shape, dt=F32, psum=False: Tile(K, name, shape, dt, psum)
        def sb_used():
            return 0

        ident = T("ident", [128, 128])
        identb = T("identb", [128, 128], BF16)
        maskX = T("maskX", [128, 512])
        maskY = T("maskY", [128, 384])
        blk = T("blk", [128, 128])
        blkb = T("blkb", [128, 128], BF16)
        blk64 = T("blk64", [128, 128])
        restart = T("restart", [128, GRP])
        ones1 = T("ones1", [1, 128])
        flg = T("flg", [128, 5])
        K.dma("sp", ident[:], cst["ident"], (), [ident])
        K.dma("sp", maskX[:], cst["maskX"], (), [maskX])
        K.dma("sp", maskY[:], cst["maskY"], (), [maskY])
        K.dma("sp", blk[:], cst["blk"], (), [blk])
        K.dma("sp", restart[:], cst["restart"], (), [restart])
        K.dma("sp", flg[:], flags, (), [flg])
        K.cp(identb[:], ident[:], [ident], [identb])
        K.cp(blkb[:], blk[:], [blk], [blkb])
        K.ts(blk64[:], blk[:], 1.0 / 64.0, None, ALU.mult, None, [blk], [blk64])
        K.memset(ones1[:], 1.0, [ones1])

        banks = [T(f"bank{i}", [128, 512], F32, psum=True) for i in range(8)]

        n1g_t = T("n1g_t", [128, 8]); n2g_t = T("n2g_t", [128, 8]); mu_t = T("mu_t", [128, 14]); omm_t = T("omm_t", [128, 14])
        v4 = T("v4", [128, 7, 4]); cfm = T("cfm", [128, 8])
        K.dma("sp", n1g_t[:], n1g, (), [n1g_t]); K.dma("sp", n2g_t[:], n2g, (), [n2g_t])
        K.dma("sp", mu_t[:], mu, (), [mu_t]); K.dma("sp", v4[:], vec4, (), [v4]); K.dma("sp", cfm[:], c_fm, (), [cfm])
        K.ts(omm_t[:], mu_t[:], -1.0, 1.0, ALU.mult, ALU.add, [mu_t], [omm_t])
        omka = T("omka", [128, 4])
        K.ts(omka[:], v4[:, 3, :], -1.0, 1.0, ALU.mult, ALU.add, [v4], [omka])

        silc = T("silc", [128, 8])
        K.act(silc[:], cfm[:], AF.Silu, [cfm], [silc])
        modT = T("modT", [128, 48])
        K.dma("sp", modT[:], b_ada, (), [modT])
        g1rep = T("g1rep", [128, D]); g2rep = T("g2rep", [128, D])
        with ExitStack() as es2:
            K.es = es2
            wab = [T(f"wab{i}", [128, 3072]) for i in range(2)]
            dg = T("dg", [128, 128])
            it = 0
            for kc in range(8):
                for half in range(2):
                    wt = wab[it % 2]; it += 1
                    K.dma("sp", wt[:], w_ada[kc * 128:(kc + 1) * 128, half * 3072:(half + 1) * 3072], (), [wt])
                    for jc in range(24):
                        K.mm(banks[0][:, half * 24 + jc: half * 24 + jc + 1], wt[:, jc * 128:(jc + 1) * 128], silc[:, kc:kc + 1], [wt, silc], [banks[0]],
                             inc=(jc == 23))
                K.tt(modT[:], banks[0][:, 0:48], modT[:], ALU.add, [banks[0], modT], [modT])
            onesf = T("onesf", [128, 128])
            K.memset(onesf[:], 1.0, [onesf])
            for (idx, dst) in ((2, g1rep), (5, g2rep)):
                for kc in range(8):
                    K.ts(dg[:], ident[:], modT[:, idx * 8 + kc: idx * 8 + kc + 1], None, ALU.mult, None, [ident, modT], [dg])
                    K.mm(banks[1][:, 0:128], onesf[:], dg[:], [onesf, dg], [banks[1]])
                    K.cp(dst[:, kc * 128:(kc + 1) * 128], banks[1][:, 0:128], [banks[1]], [dst])
            K.wait_all("dve", wab + [dg, onesf]); K.wait_all("pe", wab + [dg, onesf]); K.wait_all("sp", wab)
            K.wait_all("act", wab + [dg, onesf]); K.wait_all("pool", wab + [dg, onesf])
        K.es = es
        A1 = T("A1", [128, 8]); A2 = T("A2", [128, 8])
        K.stt(A1[:], modT[:, 8:16], 1.0, n1g_t[:], ALU.add, ALU.mult, [modT, n1g_t], [A1])
        K.stt(A2[:], modT[:, 32:40], 1.0, n2g_t[:], ALU.add, ALU.mult, [modT, n2g_t], [A2])
        A1f = T("A1f", [128, 5, 8]); B1f = T("B1f", [128, 5, 8])
        for f in range(5):
            K.ts(A1f[:, f, :], A1[:], flg[:, f:f + 1], None, ALU.mult, None, [A1, flg], [A1f])
            K.ts(B1f[:, f, :], modT[:, 0:8], flg[:, f:f + 1], None, ALU.mult, None, [modT, flg], [B1f])

        def finish():
            for i in range(K.NDS):
                if K.dcnt[i]:
                    K._wait("sp", ("d", i, K.dcnt[i]))
            for e in ("pe", "act", "dve", "pool"):
                if K.cnt[e]:
                    K._wait("sp", ("e", e, K.cnt[e]))

        if dbg and stage == 0:
            d = dbg_tensor("modT", [128, 48])
            K.dma("sp", d, modT[:], [modT], ())
            d = dbg_tensor("A1", [128, 8])
            K.dma("sp", d, A1[:], [A1], ())
        if stage == 0:
            zt = T("zt", [128, D])
            K.memset(zt[:], 0.0, [zt])
            for i in range(16):
                K.dma("sp", out[i * 128:(i + 1) * 128, :], zt[:], [zt], ())
            finish()
            return nc, dbg_out

        es_scan = ExitStack()
        es_scan.__enter__()
        K.es = es_scan
        own_extra = stage >= 2
        winb = T("winb", [128, 8, 2816], BF16)
        for kc in range(8):
            for c0 in range(0, 2816, 1408):
                K.dma("pool", winb[:, kc, c0:c0 + 1408], w_in[kc * 128:(kc + 1) * 128, c0:c0 + 1408], (), [winb])
        wlo = T("wlo", [128, 512], BF16)
        wgb = T("wgb", [128, 512], BF16)
        K.dma("pool", wlo[0:64, :], w_dec, (), [wlo])
        K.dma("pool", wlo[64:128, :], w_aup, (), [wlo])
        K.dma("pool", wgb[:], w_gup, (), [wgb])
        hsel = T("hsel", [128, 4, 8], BF16); hselT = T("hselT", [8, 4, 128], BF16)
        hsel_f = T("hsel_f", [128, 4, 8]); hselT_f = T("hselT_f", [8, 4, 128])
        K.dma("sp", hsel_f[:], cst["hsel"], (), [hsel_f]); K.dma("sp", hselT_f[:], cst["hselT"], (), [hselT_f])
        K.cp(hsel[:], hsel_f[:], [hsel_f], [hsel]); K.cp(hselT[:], hselT_f[:], [hselT_f], [hselT])
        lrk = T("lrk", [128, 4, 128], BF16)
        for hp in range(4):
            K.ts(lrk[:, hp, :], blk[:], v4[:, 6, hp:hp + 1], None, ALU.mult, None, [blk, v4], [lrk])
        epsln = T("epsln", [128, 1])
        K.memset(epsln[:], 64e-5, [epsln])
        for bk in banks:
            K.memset(bk[:], 0.0, [bk])

        xt = T("xt", [128, 4, D])
        x2t = T("x2t", [128, D])
        if own_extra:
            woutb = T("woutb", [128, 8, D], BF16)
            for kc in range(8):
                K.dma("sp", x2t[:], w_out[kc * 128:(kc + 1) * 128, :], (), [x2t])
                K.tt(woutb[:, kc, :], x2t[:], g1rep[:], ALU.mult, [x2t, g1rep], [woutb])
            wsTb = T("wsTb", [128, 8, 128], BF16)
            for h in range(8):
                K.dma("sp", x2t[:, 0:128], w_sp[h], (), [x2t])
                K.tr(banks[0][:, 0:128], x2t[:, 0:128], ident[:], [x2t, ident], [banks[0]])
                K.cp(wsTb[:, h, :], banks[0][:, 0:128], [banks[0]], [wsTb])
                K.memset(wsTb[64:128, h, 0:64], 0.0, [wsTb])
            bsp = T("bsp", [128, 4, 128])
            for hp in range(4):
                for hh in range(2):
                    K.dma("sp", bsp[hh * 64:(hh + 1) * 64, hp, :], b_sp[2 * hp + hh, :].partition_broadcast(64), (), [bsp])
            lngr = T("lngr", [128, 512]); lnbr = T("lnbr", [128, 512])
            K.dma("sp", lngr[:], sgu_g[0, :].partition_broadcast(128), (), [lngr])
            K.dma("sp", lnbr[:], sgu_b[0, :].partition_broadcast(128), (), [lnbr])

        xy = T("xy", [128, 4096], BF16)
        hTt = T("hTt", [128, 8, GRP], BF16)
        uT = T("uT", [128, 4, GRP], BF16)
        ssq = T("ssq", [128, 4]); rstd = T("rstd", [128, 4])
        carry = T("carry", [128, 14])
        K.memset(carry[:], 0.0, [carry])
        shtmp = T("shtmp", [128, GRP + 1])
        sh12 = T("sh12", [128, GRP])
        twxa = T("twxa", [128, GRP], BF16); sgb = T("sgb", [128, GRP], BF16)
        shv = T("shv", [128, GRP])
        shk4 = [T(f"shk{i}", [128, GRP]) for i in range(4)]
        ss8 = T("ss8", [8, GRP]); rn8 = T("rn8", [8, GRP], BF16)
        lwp = T("lwp", [128, GRP]); av = T("av", [128, GRP]); gT = T("gT", [128, GRP])
        kkn = T("kkn", [128, GRP]); kmod = T("kmod", [128, GRP]); bb = T("bb", [128, GRP])
        cum = T("cum", [128, GRP])
        ep = T("ep", [128, GRP]); em = T("em", [128, GRP]); eq = T("eq", [128, GRP])
        tmpa = T("tmpa", [128, GRP]); tmpb = T("tmpb", [128, GRP])
        pc = T("pc", [128, 8])
        atb = T("atb", [128, GRP], BF16); rtb = T("rtb", [128, GRP], BF16); rtf = T("rtf", [128, GRP])
        btb = T("btb", [128, GRP], BF16); ktb = T("ktb", [128, GRP], BF16)
        bhb = T("bhb", [128, GRP], BF16); khb = T("khb", [128, GRP], BF16); vbb = T("vbb", [128, GRP], BF16)
        sqb = [atb, rtb, btb, ktb]
        rkb = T("rkb", [128, GRP], BF16); bv = T("bv", [128, GRP])
        TM = [T(f"TM{i}", [128, 3, 128], BF16) for i in range(2)]
        ZB = [T(f"ZB{i}", [128, 2, 2, 64], BF16) for i in range(2)]
        XQ = [[T(f"XQ{h}_{i}", [128, 3, 128], BF16) for i in range(2)] for h in range(2)]
        YS = T("YS", [128, 2, 384], BF16)
        UW = T("UW", [128, 2, 2, 64], BF16)
        MT = [T(f"MT{i}", [128, 128], BF16) for i in range(2)]
        RhT = [T(f"RhT{i}", [128, 64], BF16) for i in range(2)]
        Hb = [[T(f"H{hp}_{i}", [128, 128], BF16) for i in range(2)] for hp in range(4)]
        hpar = [0, 0, 0, 0]
        for hp in range(4):
            K.memset(Hb[hp][0][:], 0.0, [Hb[hp][0]])
        YT = T("YT", [128, GRP])
        sh13 = YT; shr = cum; vtm = tmpb
        if own_extra:
            vnb = T("vnb", [128, 4, 512], BF16)
            bnst = T("bnst", [128, 6]); bnag = T("bnag", [128, 2]); lnr = T("lnr", [128, 2])
        bX, bY0, bY1, bA, bR, bE, bYg, bH = banks
        ptb = [banks[5][:, 0:256].bitcast(BF16), banks[6][:, 0:256].bitcast(BF16)]
        ptm = banks[6][:, 0:256].bitcast(BF16)

        dbg_el = dbg_tensor("el", [10, 128, GRP]) if (dbg and stage == 1) else None
        dbg_yt = dbg_tensor("yt", [4, 4, 128, GRP]) if (dbg and stage in (1, 2)) else None
        dbg_H = dbg_tensor("H", [4, 128, 128]) if (dbg and stage in (1, 2)) else None
        dbg_ya = dbg_tensor("ya", [4, 8, 128, GRP]) if (dbg and stage == 2) else None
        pj_cnt = [0]
        dump_n = [0]

        def dump(name, ap, shape, deps):
            if not (dbg and stage == 1):
                return
            dt_ = dbg_tensor(name, shape)
            st_ = T(f"dump{dump_n[0]}", shape); dump_n[0] += 1
            K.cp(st_[:], ap, deps, [st_])
            K.dma("sp", dt_, st_[:], [st_], ())


        def proj_shift(cc, dst, e2="dve"):
            bk = banks[pj_cnt[0] % 2]; pj_cnt[0] += 1
            for kc in range(8):
                K.mm(bk[:, :], winb[:, kc, cc * 128:(cc + 1) * 128], hTt[:, kc, :], [winb, hTt], [bk], start=(kc == 0), stop=(kc == 7), inc=(kc == 7))
            K.cp(shtmp[:, 0:1], carry[:, cc:cc + 1], [carry], [shtmp], e="pool")
            K.act(shtmp[:, 1:GRP + 1], bk[:, :], AF.Copy, [bk, mu_t], [shtmp], scale=mu_t[:, cc:cc + 1])
            K.cp(carry[:, cc:cc + 1], shtmp[:, GRP:GRP + 1], [shtmp], [carry], e="pool")
            K.stt(dst[:], bk[:, :], omm_t[:, cc:cc + 1], shtmp[:, 0:GRP], ALU.mult, ALU.add, [bk, omm_t, shtmp], [dst])

        g_first = 0 if stage != 1 else 0
        for g in range(g_first, NGRP):
            seg = g // 4
            own = g >= OWN_G0
            for tt in range(4):
                r0 = g * GRP + tt * 128
                K.dma("sp", xt[:, tt, :], xw[r0:r0 + 128, :], (), [xt])
            for tt in range(4):
                K.act(uT[:, 0:2, :].rearrange("p a b -> p (a b)"), xt[:, tt, :], AF.Square, [xt], [uT, ssq], accum_out=ssq[:, tt:tt + 1])
            K.ts(rstd[:], ssq[:], 1.0 / D, 1e-6, ALU.mult, ALU.add, [ssq], [rstd])
            K.act(rstd[:], rstd[:], AF.Sqrt, [rstd], [rstd])
            K.op("dve", lambda E: E.reciprocal(out=rstd[:], in_=rstd[:]), [rstd], [rstd])
            for tt in range(4):
                K.act(xy[:, tt * D:(tt + 1) * D], xt[:, tt, :], AF.Copy, [xt, rstd], [xy], scale=rstd[:, tt:tt + 1])
            for kc in range(8):
                pbk = banks[5 + kc % 2]
                pz = ptb[kc % 2]
                for tt in range(4):
                    K.tr(pz[:, tt * 128:(tt + 1) * 128], xy[:, tt * D + kc * 128: tt * D + (kc + 1) * 128], identb[:], [xy, identb], [pbk], inc=(tt == 3))
                K.act(hTt[:, kc, :], pz, AF.Identity, [pbk, A1f, B1f], [hTt],
                      scale=A1f[:, seg + 1, kc:kc + 1], bias=B1f[:, seg + 1, kc:kc + 1])

            if g == OWN_G0 - 1:
                for cc_ in (0, 1, 2, 3, 13):
                    proj_shift(cc_, sh13)
            proj_shift(12, sh12)
            K.act(twxa[0:64, :], sh12[0:64, :], AF.Tanh, [sh12], [twxa])
            K.cp(twxa[64:128, :], sh12[64:128, :], [sh12], [twxa], e="pool")
            if own:
                proj_shift(13, sh13)
                K.act(sgb[:], sh13[:], AF.Sigmoid, [sh13], [sgb])
            for hp in range(4):
                proj_shift(4 + hp, shk4[hp])
                K.act(sqb[hp][:], shk4[hp][:], AF.Square, [shk4[hp], v4], [sqb[hp]], scale=v4[:, 2, hp:hp + 1])
            for hp in range(4):
                K.mm(banks[2][0:8, :], hsel[:, hp, :], sqb[hp][:], [hsel, sqb[hp]], [banks[2]], start=(hp == 0), stop=(hp == 3), inc=(hp == 3))
            K.ts(ss8[:], banks[2][0:8, :], 1e-18, None, ALU.max, None, [banks[2]], [ss8])
            K.act(ss8[:], ss8[:], AF.Ln, [ss8], [ss8])
            K.act(rn8[:], ss8[:], AF.Exp, [ss8], [rn8], scale=-0.5)

            for hp in range(4):
                shk = shk4[hp]
                w0c = v4[:, 0, hp:hp + 1]; a0c = v4[:, 1, hp:hp + 1]; kkc = v4[:, 2, hp:hp + 1]; kac = v4[:, 3, hp:hp + 1]
                K.mm(banks[2][:, :], wlo[0:64, hp * 128:(hp + 1) * 128], twxa[0:64, :], [wlo, twxa], [banks[2]])
                K.mm(banks[3][:, :], wlo[64:128, hp * 128:(hp + 1) * 128], twxa[64:128, :], [wlo, twxa], [banks[3]])
                K.act(lwp[:], banks[2][:, :], AF.Sigmoid, [banks[2], v4], [lwp], bias=w0c)
                K.act(av[:], banks[3][:, :], AF.Sigmoid, [banks[3], v4], [av], bias=a0c)
                if own:
                    K.mm(banks[0][:, :], wgb[:, hp * 128:(hp + 1) * 128], sgb[:], [wgb, sgb], [banks[0]])
                    K.cp(gT[:], banks[0][:, :], [banks[0]], [gT], e="act")
                proj_shift(8 + hp, shv)
                K.mm(banks[2][:, :], hselT[:, hp, :], rn8[:], [hselT, rn8], [banks[2]])
                K.stt(kkn[:], shk[:], kkc, banks[2][:, :], ALU.mult, ALU.mult, [shk, v4, banks[2]], [kkn])
                K.ts(tmpa[:], av[:], kac, omka[:, hp:hp + 1], ALU.mult, ALU.add, [av, v4, omka], [tmpa])
                K.tt(kmod[:], shk[:], tmpa[:], ALU.mult, [shk, tmpa], [kmod])
                K.tt(bb[:], kkn[:], av[:], ALU.mult, [kkn, av], [bb], e="pool")
                K.op("dve", lambda E: E.tensor_tensor_scan(out=cum[:], data0=restart[:], data1=lwp[:], initial=0.0, op0=ALU.mult, op1=ALU.add),
                     [restart, lwp], [cum])
                K.tt(tmpb[:], cum[:], lwp[:], ALU.subtract, [cum, lwp], [tmpb], e="pool")
                K.act(ep[:], cum[:], AF.Exp, [cum], [ep], scale=-C0)
                K.act(em[:], cum[:], AF.Exp, [cum], [em], scale=C0)
                K.act(eq[:], tmpb[:], AF.Exp, [tmpb], [eq], scale=-C0)
                epv = ep[:].rearrange("p (c t) -> p c t", t=64)
                K.cp(pc[:], epv[:, :, 63], [ep], [pc])
                pcb = epv[:, :, 63:64].broadcast_to([128, 8, 64])
                v3 = lambda t_: t_[:].rearrange("p (c t) -> p c t", t=64)
                K.tt(tmpa[:], bb[:], em[:], ALU.mult, [bb, em], [tmpa])
                K.cp(btb[:], tmpa[:], [tmpa], [btb], e="pool")
                K.tt(v3(bhb), v3(tmpa), pcb, ALU.mult, [tmpa, ep], [bhb])
                K.tt(tmpb[:], kmod[:], em[:], ALU.mult, [kmod, em], [tmpb])
                K.cp(ktb[:], tmpb[:], [tmpb], [ktb], e="pool")
                K.tt(v3(khb), v3(tmpb), pcb, ALU.mult, [tmpb, ep], [khb])
                K.stt(atb[:], kkn[:], -1.0, eq[:], ALU.mult, ALU.mult, [kkn, eq], [atb])
                K.cp(vbb[:], shv[:], [shv], [vbb], e="act")
                if own:
                    proj_shift(hp, shr)
                    K.tt(rtf[:], shr[:], ep[:], ALU.mult, [shr, ep], [rtf])
                    K.cp(rtb[:], rtf[:], [rtf], [rtb], e="pool")
                    K.tt(rkb[:], shr[:], kmod[:], ALU.mult, [shr, kmod], [rkb])
                    K.mm(banks[3][:, :], lrk[:, hp, :], rkb[:], [lrk, rkb], [banks[3]])
                    K.tt(bv[:], banks[3][:, :], shv[:], ALU.mult, [banks[3], shv], [bv])
                if dbg_el is not None and g == NGRP - 1 and hp == 1:
                    for i, t_ in enumerate((lwp, av, kkn, kmod, bb, cum, shv, gT, shr, bv)):
                        K.dma("sp", dbg_el[i], t_[:], [t_], ())

                for cp_ in range(4):
                    tok = slice(cp_ * 128, (cp_ + 1) * 128)
                    TMt = TM[cp_ % 2]; ZBt = ZB[cp_ % 2]
                    for i, src in enumerate((atb, bhb, khb, vbb)):
                        K.tr(ptm[:, i * 128:(i + 1) * 128], src[:, tok], identb[:], [src, identb], [bYg], inc=(i == 3))
                    K.cp(TMt[:].rearrange("p a b -> p (a b)"), ptm[:, 128:512], [bYg], [TMt])
                    K.cp(ZBt[:, :, 1, :], ptm[:, 0:128].rearrange("p (h v) -> p h v", h=2), [bYg], [ZBt], e="act")
                    for h in range(2):
                        hb = slice(h * 64, (h + 1) * 64)
                        bY = bY0 if h == 0 else bY1
                        bXh = bX if h == 0 else bA
                        for q in range(2):
                            qp = slice(q * 64, (q + 1) * 64)
                            tq = slice(cp_ * 128 + q * 64, cp_ * 128 + (q + 1) * 64)
                            K.mm(bXh[qp, h * 256 + q * 64: h * 256 + (q + 1) * 64], atb[hb, tq], btb[hb, tq], [btb, atb], [bXh], inc=False)
                            K.mm(bXh[qp, h * 256 + 128 + q * 64: h * 256 + 128 + (q + 1) * 64], btb[hb, tq], atb[hb, tq], [btb, atb], [bXh], inc=False)
                            K.mm(bY[qp, q * 64:(q + 1) * 64], ktb[hb, tq], atb[hb, tq], [ktb, atb], [bY], inc=((q == 1) and not own))
                            if own:
                                K.mm(bY[qp, 128 + q * 64:128 + (q + 1) * 64], ktb[hb, tq], rtb[hb, tq], [ktb, rtb], [bY], inc=False)
                                K.mm(bY[qp, 256 + q * 64:256 + (q + 1) * 64], btb[hb, tq], rtb[hb, tq], [btb, rtb], [bY], inc=(q == 1))
                    ny = 384 if own else 128
                    for h in range(2):
                        bXh = bX if h == 0 else bA
                        bY = bY0 if h == 0 else bY1
                        K.tt(XQ[h][0][:, 0:2, :].rearrange("p a t -> p (a t)"), bXh[:, h * 256:(h + 1) * 256], maskX[:, h * 256:(h + 1) * 256], ALU.mult,
                             [bXh, maskX], [XQ[h][0]])
                        K.cp(XQ[h][0][:, 2, :], identb[:], [identb], [XQ[h][0]], e="pool")
                        K.tt(YS[:, h, 0:ny], bY[:, 0:ny], maskY[:, 0:ny], ALU.mult, [bY, maskY], [YS])
                    for h in range(2):
                        K.mm(bR[:, 256 + h * 64:256 + (h + 1) * 64], YS[:, h, 0:128], TMt[:, 2, h * 64:(h + 1) * 64], [YS, TMt], [bR], inc=(h == 1))
                    K.cp(ZBt[:, :, 0, :], bR[:, 256:384].rearrange("p (h v) -> p h v", h=2), [bR], [ZBt], e="act")
                    bN = [bX, bA]
                    for it in range(6):
                        for h in range(2):
                            Xc = XQ[h][it % 2]
                            if it < 5:
                                K.mm(bN[h][:, 128:384], Xc[:, 0, :], Xc[:, 1:3, :].rearrange("p a t -> p (a t)"), [Xc], [bN[h]], inc=False)
                                K.mm(bN[h][:, 0:128], Xc[:, 1, :], Xc[:, 0, :], [Xc], [bN[h]])
                            else:
                                K.mm(bN[h][:, 256:384], Xc[:, 0, :], Xc[:, 2, :], [Xc], [bN[h]])
                        for h in range(2):
                            Xc = XQ[h][it % 2]; Xn = XQ[h][1 - it % 2]
                            if it < 5:
                                K.cp(Xn[:, 0:2, :].rearrange("p a t -> p (a t)"), bN[h][:, 0:256], [bN[h]], [Xn], e="act")
                            K.tt(Xn[:, 2, :], bN[h][:, 256:384], Xc[:, 2, :], ALU.add, [bN[h], Xc], [Xn])
                    for h in range(2):
                        K.mm(bE[:, h * 128:(h + 1) * 128], XQ[h][0][:, 2, :], ZBt[:, h, :, :].rearrange("p a v -> p (a v)"), [XQ[h][0], ZBt], [bE], inc=(h == 1))
                    K.cp(UW[:].rearrange("p k h v -> p h k v"), bE[:, 0:256].rearrange("p (h k v) -> p h k v", h=2, k=2), [bE], [UW])
                    mtreg = [bR[:, 384:512], bE[:, 384:512]]; mtbank = [bR, bE]
                    for q in range(2):
                        qp = slice(q * 64, (q + 1) * 64)
                        for h in range(2):
                            hs_ = slice(h * 64, (h + 1) * 64)
                            K.mm(mtbank[q][hs_, 384 + h * 64:384 + (h + 1) * 64], UW[qp, 1, h, :], TMt[qp, 0, hs_], [UW, TMt], [mtbank[q]], inc=(h == 1))
                    for q in range(2):
                        ck = cp_ * 2 + q
                        K.stt(MT[q][:], ident[:], pc[:, ck:ck + 1], mtreg[q], ALU.mult, ALU.add, [ident, pc, mtbank[q]], [MT[q]])
                    hbank = [bH, bX]
                    for q in range(2):
                        qp = slice(q * 64, (q + 1) * 64)
                        for h in range(2):
                            hs_ = slice(h * 64, (h + 1) * 64)
                            K.mm(hbank[q][hs_, h * 64:(h + 1) * 64], TMt[qp, 0, hs_], UW[qp, 0, h, :], [TMt, UW], [hbank[q]], start=True, stop=False, inc=False)
                            K.mm(hbank[q][hs_, h * 64:(h + 1) * 64], TMt[qp, 1, hs_], TMt[qp, 2, hs_], [TMt], [hbank[q]], start=False, stop=False, inc=(h == 1))
                    for q in range(2):
                        qp = slice(q * 64, (q + 1) * 64)
                        tq = slice(cp_ * 128 + q * 64, cp_ * 128 + (q + 1) * 64)
                        Hc = Hb[hp][hpar[hp]]; Hn = Hb[hp][1 - hpar[hp]]
                        hreg = hbank[q][:, 0:128]
                        if own:
                            RhTt = RhT[q]
                            yreg = bYg[:, 256 + q * 64:256 + (q + 1) * 64]
                            for h in range(2):
                                hs_ = slice(h * 64, (h + 1) * 64)
                                K.mm(bE[hs_, 256 + q * 64:256 + (q + 1) * 64], UW[qp, 1, h, :], YS[qp, h, 256 + q * 64:256 + (q + 1) * 64], [UW, YS], [bE], inc=(h == 1))
                            K.tt(RhTt[:], bE[:, 256 + q * 64:256 + (q + 1) * 64], rtf[:, tq], ALU.add, [bE, rtf], [RhTt])
                            for h in range(2):
                                hs_ = slice(h * 64, (h + 1) * 64)
                                K.mm(bYg[hs_, 256 + q * 64:256 + (q + 1) * 64], UW[qp, 0, h, :], YS[qp, h, 256 + q * 64:256 + (q + 1) * 64], [UW, YS], [bYg], start=True, stop=False, inc=False)
                                K.mm(bYg[hs_, 256 + q * 64:256 + (q + 1) * 64], TMt[qp, 2, hs_], YS[qp, h, 128 + q * 64:128 + (q + 1) * 64], [TMt, YS], [bYg], start=False, stop=False, inc=False)
                            K.mm(yreg, Hc[:], RhTt[:], [Hc, RhTt], [bYg], start=False, stop=True)
                            K.cp(YT[:, tq], yreg, [bYg], [YT], e="act")
                        K.mm(hreg, MT[q][:], Hc[:], [MT[q], Hc], [hbank[q]], start=False, stop=True)
                        K.cp(Hn[:], hreg, [hbank[q]], [Hn])
                        hpar[hp] = 1 - hpar[hp]
                if dbg_yt is not None and own:
                    K.dma("sp", dbg_yt[g - OWN_G0, hp], YT[:], [YT], ())
                if dbg and stage == 1 and hp == 0:
                    dump(f"Hg{g}", Hb[0][hpar[0]][:], [128, 128], [Hb[0][hpar[0]]])
                if own_extra and own:
                    lgc = v4[:, 4, hp:hp + 1]; lbc = v4[:, 5, hp:hp + 1]
                    K.mm(banks[2][:, :], blk64[:], YT[:], [blk64, YT], [banks[2]])
                    K.tt(tmpa[:], YT[:], banks[2][:, :], ALU.subtract, [YT, banks[2]], [tmpa])
                    K.act(tmpb[:], tmpa[:], AF.Square, [tmpa], [tmpb])
                    K.mm(banks[3][:, :], blk64[:], tmpb[:], [blk64, tmpb], [banks[3]])
                    K.act(tmpb[:], banks[3][:, :], AF.Ln, [banks[3], epsln], [tmpb], bias=epsln[:, 0:1])
                    K.act(tmpb[:], tmpb[:], AF.Exp, [tmpb], [tmpb], scale=-0.5)
                    K.tt(tmpa[:], tmpa[:], tmpb[:], ALU.mult, [tmpa, tmpb], [tmpa])
                    K.ts(tmpa[:], tmpa[:], lgc, lbc, ALU.mult, ALU.add, [tmpa, v4], [tmpa])
                    K.tt(tmpa[:], tmpa[:], bv[:], ALU.add, [tmpa, bv], [tmpa], e="pool")
                    K.tt(xy[:, hp * GRP:(hp + 1) * GRP], tmpa[:], gT[:], ALU.mult, [tmpa, gT], [xy])
            if own_extra and own:
                for i in range(4):
                    bk = banks[i % 2]
                    for kc in range(8):
                        K.mm(bk[:, :], winb[:, kc, 1792 + i * 128:1792 + (i + 1) * 128], hTt[:, kc, :], [winb, hTt], [bk], start=(kc == 0), stop=(kc == 7), inc=(kc == 7))
                    K.act(uT[:, i, :], bk[:, :], AF.Gelu_apprx_tanh, [bk], [uT])
                spb = [banks[2], banks[3], banks[6], banks[7]]
                for tt in range(4):
                    for kc in range(8):
                        K.mm(banks[0][:, :], hTt[:, kc, tt * 128:(tt + 1) * 128], winb[:, kc, 2304:2816], [winb, hTt], [banks[0]], start=(kc == 0), stop=(kc == 7), inc=(kc == 7))
                    K.act(vtm[:], banks[0][:, :], AF.Gelu_apprx_tanh, [banks[0]], [vtm])
                    K.op("dve", lambda E: E.bn_stats(out=bnst[:], in_=vtm[:]), [vtm], [bnst])
                    K.op("dve", lambda E: E.bn_aggr(out=bnag[:], in_=bnst[:]), [bnst], [bnag])
                    K.ts(lnr[:, 0:1], bnag[:, 1:2], 1e-5, None, ALU.add, None, [bnag], [lnr])
                    K.act(lnr[:, 0:1], lnr[:, 0:1], AF.Sqrt, [lnr], [lnr])
                    K.op("dve", lambda E: E.reciprocal(out=lnr[:, 1:2], in_=lnr[:, 0:1]), [lnr], [lnr])
                    K.ts(vtm[:], vtm[:], bnag[:, 0:1], lnr[:, 1:2], ALU.subtract, ALU.mult, [vtm, bnag, lnr], [vtm])
                    K.tt(vtm[:], vtm[:], lngr[:], ALU.mult, [vtm, lngr], [vtm], e="pool")
                    K.tt(vnb[:, tt, :], vtm[:], lnbr[:], ALU.add, [vtm, lnbr], [vnb], e="pool")
                    for h in range(8):
                        K.mm(spb[h // 2][(h % 2) * 64:(h % 2) * 64 + 64, tt * 128:(tt + 1) * 128], vnb[:, tt, h * 64:(h + 1) * 64], wsTb[:, h, :],
                             [vnb, wsTb], [spb[h // 2]], inc=(h % 2 == 1))
                for hp in range(4):
                    K.tt(tmpa[:].rearrange("p (r t) -> p r t", r=4), spb[hp][:, :].rearrange("p (r t) -> p r t", r=4),
                         bsp[:, hp:hp + 1, :].broadcast_to([128, 4, 128]), ALU.add, [spb[hp], bsp], [tmpa])
                    K.tt(xy[:, (4 + hp) * GRP:(5 + hp) * GRP], tmpa[:], uT[:, hp, :], ALU.mult, [tmpa, uT], [xy])
                if dbg_ya is not None:
                    for i in range(8):
                        K.cp(tmpa[:], xy[:, i * GRP:(i + 1) * GRP], [xy], [tmpa])
                        K.dma("sp", dbg_ya[g - OWN_G0, i], tmpa[:], [tmpa], ())
                for tt in range(4):
                    for half in range(2):
                        bk = banks[half]
                        for kc in range(8):
                            K.mm(bk[:, :], xy[:, kc * GRP + tt * 128: kc * GRP + (tt + 1) * 128], woutb[:, kc, half * 512:(half + 1) * 512], [xy, woutb], [bk],
                                 start=(kc == 0), stop=(kc == 7), inc=(kc == 7))
                        K.tt(x2t[:, half * 512:(half + 1) * 512], bk[:, :], xt[:, tt, half * 512:(half + 1) * 512], ALU.add, [bk, xt], [x2t])
                    r0 = (g - OWN_G0) * GRP + tt * 128
                    K.dma("sp", x2d[r0:r0 + 128, :], x2t[:], [x2t], [x2dbuf])
        if dbg_H is not None:
            for hp in range(4):
                Hf = T(f"Hf{hp}", [128, 128])
                K.cp(Hf[:], Hb[hp][hpar[hp]][:], [Hb[hp][hpar[hp]]], [Hf])
                K.dma("sp", dbg_H[hp], Hf[:], [Hf], ())
        if stage <= 2:
            if stage == 2:
                for i in range(16):
                    K.dma("sp", x2t[:], x2d[i * 128:(i + 1) * 128, :], [x2dbuf], [x2t])
                    K.dma("sp", out[i * 128:(i + 1) * 128, :], x2t[:], [x2t], ())
            else:
                K.memset(x2t[:], 0.0, [x2t])
                for i in range(16):
                    K.dma("sp", out[i * 128:(i + 1) * 128, :], x2t[:], [x2t], ())
            finish()
            es_scan.__exit__(None, None, None)
            return nc, dbg_out
        allb = list(K.all_bufs)
        for e in ("pe", "act", "dve", "pool", "sp"):
            K._deps(e, (), allb)
        es_scan.__exit__(None, None, None)
        K.es = es

        acc = T("acc", [128, 16, D])
        h2T = T("h2T", [128, 8, SEG], BF16)
        gates = T("gates", [128, 16, NE + 1])
        ebr = T("ebr", [128, NE]); nfr = T("nfr", [128, D]); wrt = T("wrt", [128, 8, NE])
        K.dma("sp", ebr[:], e_bias[0, :].partition_broadcast(128), (), [ebr])
        K.dma("sp", nfr[:], nfg[0, :].partition_broadcast(128), (), [nfr])
        K.dma("sp", wrt[:], w_rt.rearrange("(kc p) n -> p kc n", p=128), (), [wrt])
        xnf = T("xnf", [128, D]); h2f = T("h2f", [128, 8, 128])
        sc = T("sc", [128, NE]); bi = T("bi", [128, NE]); mk = T("mk", [128, NE]); m8 = T("m8", [128, 8, 8])
        gs = T("gs", [128, 8]); gs8 = T("gs8", [128, 8]); gm = T("gm", [128, 8]); pen = T("pen", [128, 8]); t8 = T("t8", [128, 8])
        ssq2 = T("ssq2", [128, 1]); rs2 = T("rs2", [128, 1]); rsum = T("rsum", [128, 1]); junk2 = T("junk2", [128, D], BF16)
        dbg_g = dbg_tensor("gates", [16, 128, NE + 1]) if dbg else None

        def rms_rstd(t):
            K.act(junk2[:], acc[:, t, :], AF.Square, [acc.sub(t)], [junk2, ssq2], accum_out=ssq2[:, 0:1])
            K.ts(rs2[:], ssq2[:], 1.0 / D, 1e-6, ALU.mult, ALU.add, [ssq2], [rs2])
            K.act(rs2[:], rs2[:], AF.Sqrt, [rs2], [rs2])
            K.op("dve", lambda E: E.reciprocal(out=rs2[:], in_=rs2[:]), [rs2], [rs2])

        for t in range(16):
            K.dma("sp", acc[:, t, :], x2d[t * 128:(t + 1) * 128, :], [x2dbuf], [acc.sub(t)])
            rms_rstd(t)
            K.act(xnf[:], acc[:, t, :], AF.Copy, [acc.sub(t), rs2], [xnf], scale=rs2[:, 0:1])
            for kc in range(8):
                bk = banks[kc // 4]
                K.tr(bk[:, (kc % 4) * 128:(kc % 4 + 1) * 128], xnf[:, kc * 128:(kc + 1) * 128], ident[:], [xnf, ident], [bk], inc=(kc % 4 == 3))
            for kc in range(8):
                bk = banks[kc // 4]; reg = bk[:, (kc % 4) * 128:(kc % 4 + 1) * 128]
                K.act(h2T[:, kc, t * 128:(t + 1) * 128], reg, AF.Identity, [bk, A2, modT], [h2T.sub(t)], scale=A2[:, kc:kc + 1], bias=modT[:, 24 + kc:25 + kc])
                K.ts(h2f[:, kc, :], reg, A2[:, kc:kc + 1], modT[:, 24 + kc:25 + kc], ALU.mult, ALU.add, [bk, A2, modT], [h2f])
            for kc in range(8):
                K.mm(banks[2][:, 0:NE], h2f[:, kc, :], wrt[:, kc, :], [h2f, wrt], [banks[2]], start=(kc == 0), stop=(kc == 7), inc=(kc == 7))
            K.act(sc[:], banks[2][:, 0:NE], AF.Sigmoid, [banks[2]], [sc])
            K.tt(bi[:], sc[:], ebr[:], ALU.add, [sc, ebr], [bi])
            for gi in range(8):
                K.op("dve", lambda E: E.max(out=m8[:, gi, :], in_=bi[:, gi * 8:(gi + 1) * 8]), [bi], [m8])
            K.tt(gs[:], m8[:, :, 0], m8[:, :, 1], ALU.add, [m8], [gs])
            K.op("dve", lambda E: E.max(out=gs8[:], in_=gs[:]), [gs], [gs8])
            K.ts(gm[:], gs[:], gs8[:, 3:4], None, ALU.is_ge, None, [gs, gs8], [gm])
            K.ts(pen[:], gm[:], 1e9, -1e9, ALU.mult, ALU.add, [gm], [pen])
            mkv = mk[:].rearrange("p (g e) -> p g e", e=8); biv = bi[:].rearrange("p (g e) -> p g e", e=8)
            K.tt(mkv, biv, gm[:].unsqueeze(2).broadcast_to([128, 8, 8]), ALU.mult, [bi, gm], [mk])
            K.tt(mkv, mkv, pen[:].unsqueeze(2).broadcast_to([128, 8, 8]), ALU.add, [mk, pen], [mk])
            K.op("dve", lambda E: E.max(out=t8[:], in_=mk[:]), [mk], [t8])
            K.ts(mk[:], mk[:], t8[:, 7:8], None, ALU.is_ge, None, [mk, t8], [mk])
            K.tt(mk[:], mk[:], sc[:], ALU.mult, [mk, sc], [mk])
            K.op("dve", lambda E: E.tensor_reduce(out=rsum[:], in_=mk[:], axis=AX.X, op=ALU.add), [mk], [rsum])
            K.op("dve", lambda E: E.reciprocal(out=rsum[:], in_=rsum[:]), [rsum], [rsum])
            K.ts(gates[:, t, 0:NE], mk[:], rsum[:, 0:1], 2.5, ALU.mult, ALU.mult, [mk, rsum], [gates])
            K.memset(gates[:, t, NE:NE + 1], 1.0, [gates])
            if dbg_g is not None:
                K.dma("sp", dbg_g[t], gates[:, t, :], [gates], ())

        w1b = [T(f"w1b{i}", [128, 8, DE], BF16) for i in range(2)]
        w3b = [T(f"w3b{i}", [128, 8, DE], BF16) for i in range(2)]
        w2b = [T(f"w2b{i}", [128, 2, D], BF16) for i in range(2)]
        actb = [T(f"actb{i}", [128, 2, GRP], BF16) for i in range(2)]
        sgt = [T(f"sgt{i}", [128, GRP]) for i in range(2)]
        n_exp = NE + 1
        for e in range(n_exp):
            i = e % 2
            for hf in range(2):
                K.dma("pool", w1b[i][:, hf * 4:(hf + 1) * 4, :], w1e[e, hf * 512:(hf + 1) * 512, :].rearrange("(kc p) n -> p kc n", p=128), (), [w1b[i]])
                K.dma("pool", w3b[i][:, hf * 4:(hf + 1) * 4, :], w3e[e, hf * 512:(hf + 1) * 512, :].rearrange("(kc p) n -> p kc n", p=128), (), [w3b[i]])
                K.dma("pool", w2b[i][:, hf, :], w2e[e, hf * 128:(hf + 1) * 128, :], (), [w2b[i]])
            for hf in range(2):
                K.tt(w2b[i][:, hf, :], w2b[i][:, hf, :], g2rep[:], ALU.mult, [w2b[i], g2rep], [w2b[i]], e="pool")
            for tg in range(4):
                ab = actb[tg % 2]
                h2r = [h2T.sub(4 * tg + j) for j in range(4)]
                for cc in range(2):
                    bG = banks[cc * 2]; bU = banks[cc * 2 + 1]
                    for kc in range(8):
                        K.mm(bG[:, :], w1b[i][:, kc, cc * 128:(cc + 1) * 128], h2T[:, kc, tg * GRP:(tg + 1) * GRP], [w1b[i]] + h2r, [bG],
                             start=(kc == 0), stop=(kc == 7), inc=(kc == 7))
                    for kc in range(8):
                        K.mm(bU[:, :], w3b[i][:, kc, cc * 128:(cc + 1) * 128], h2T[:, kc, tg * GRP:(tg + 1) * GRP], [w3b[i]] + h2r, [bU],
                             start=(kc == 0), stop=(kc == 7), inc=(kc == 7))
                    K.act(sgt[cc][:], bG[:, :], AF.Silu, [bG], [sgt[cc]])
                    K.tt(ab[:, cc, :], sgt[cc][:], bU[:, :], ALU.mult, [sgt[cc], bU], [ab])
                for tt in range(4):
                    t = tg * 4 + tt
                    for half in range(2):
                        bO = banks[4 + (tt % 2) * 2 + half]
                        for cc in range(2):
                            K.mm(bO[:, :], ab[:, cc, tt * 128:(tt + 1) * 128], w2b[i][:, cc, half * 512:(half + 1) * 512], [ab, w2b[i]], [bO],
                                 start=(cc == 0), stop=(cc == 1), inc=(cc == 1))
                        K.stt(acc[:, t, half * 512:(half + 1) * 512], bO[:, :], gates[:, t, e:e + 1], acc[:, t, half * 512:(half + 1) * 512],
                              ALU.mult, ALU.add, [bO, gates, acc.sub(t)], [acc.sub(t)])
        for t in range(16):
            rms_rstd(t)
            K.stt(xnf[:], acc[:, t, :], rs2[:, 0:1], nfr[:], ALU.mult, ALU.mult, [acc.sub(t), rs2, nfr], [xnf])
            K.dma("sp", out[t * 128:(t + 1) * 128, :], xnf[:], [xnf], ())
        finish()
    return nc, dbg_out


def _fm(v, n):
    return np.ascontiguousarray(np.asarray(v, np.float32).reshape(n, 128).T)


def make_in_maps(inputs, small=False):
    g = lambda k: np.asarray(inputs[k], np.float32)
    x = g("x"); c = g("c")
    consts = _consts()
    shared = {
        "w_ada": g("w_ada")[0], "b_ada": _fm(g("b_ada")[0], 48), "n1g": _fm(g("norm1_g")[0], 8),
        "w_in": g("w_in")[0], "mu": _fm(g("mu_shift")[0], 14),
        "vec4": np.ascontiguousarray(np.stack([_fm(g("w0")[0], 4), _fm(g("a0")[0], 4), _fm(g("k_k")[0], 4), _fm(g("k_a")[0], 4),
                                               _fm(g("lnx_g")[0], 4), _fm(g("lnx_b")[0], 4), _fm(g("r_k")[0].reshape(-1), 4)], axis=1)),
        "w_dec": g("w_decay_up")[0], "w_aup": g("w_a_up")[0], "w_gup": g("w_g_up")[0],
        "sgu_g": g("sgu_ln_g")[0][None, :], "sgu_b": g("sgu_ln_b")[0][None, :],
        "w_sp": g("w_spatial")[0], "b_sp": g("b_spatial")[0], "w_out": g("w_out")[0], "n2g": _fm(g("norm2_g")[0], 8),
        "w_rt": g("w_router")[0], "e_bias": g("e_bias")[0][None, :],
        "nfg": g("norm_f_g")[None, :],
    }
    if small:
        shared["w1e"] = np.zeros((1, D, DE), np.float32); shared["w3e"] = np.zeros((1, D, DE), np.float32); shared["w2e"] = np.zeros((1, DE, D), np.float32)
    else:
        shared["w1e"] = np.concatenate([g("w1_e")[0], g("w1_s")], axis=0)
        shared["w3e"] = np.concatenate([g("w3_e")[0], g("w3_s")], axis=0)
        shared["w2e"] = np.concatenate([g("w2_e")[0], g("w2_s")], axis=0)
    for k, v in consts.items():
        shared["c_" + k] = v
    maps = []
    for core in range(8):
        b, j = core // 4, core % 4
        win = np.zeros((NSEG * SEG, D), np.float32)
        lo = (j - 3) * SEG
        src0 = max(lo, 0)
        win[src0 - lo:] = x[b, src0:(j + 1) * SEG]
        fl = np.zeros((128, 5), np.float32)
        for s in range(5):
            seg_global = j - 4 + s
            fl[:, s] = 1.0 if seg_global >= 0 else 0.0
        m = dict(shared)
        m["xw"] = win
        m["flags"] = fl
        m["c_fm"] = _fm(c[b], 8)
        maps.append(m)
    return maps


_NC_CACHE = {}


def kernel(**inputs):
    if "nc" not in _NC_CACHE:
        _NC_CACHE["nc"] = build_nc()[0]
    nc = _NC_CACHE["nc"]
    maps = make_in_maps(inputs)
    res = run_bass_kernel_spmd(nc, maps, core_ids=list(range(8)))
    outp = np.zeros((2, 4 * SEG, D), np.float32)
    for core in range(8):
        b, j = core // 4, core % 4
        outp[b, j * SEG:(j + 1) * SEG] = res.results[core]["out"]
    return outp
```

```python
import numpy as np
import ml_dtypes
from contextlib import ExitStack

import concourse.bass as bass
import concourse.mybir as mybir
from concourse.bass_utils import run_bass_kernel_spmd

F32 = mybir.dt.float32
BF16 = mybir.dt.bfloat16
AF = mybir.ActivationFunctionType
ALU = mybir.AluOpType
AX = mybir.AxisListType

D = 1024
SEG = 2048
NSEG = 4
GRP = 512
NGRP = NSEG * SEG // GRP
OWN_G0 = (NSEG - 1) * SEG // GRP
C0 = 0.6065306597126334
NE = 64
DE = 256
SAME_SYNC = True


class Buf:
    def __init__(self, name):
        self.name = name
        self.psum = False
        self.w = None
        self.r = {}


class Tile(Buf):
    def __init__(self, ctx, name, shape, dtype, psum=False):
        super().__init__(name)
        self.psum = psum
        ctx.all_bufs.append(self)
        alloc = ctx.nc.psum_tensor if psum else ctx.nc.sbuf_tensor
        self.t = ctx.es.enter_context(alloc(name, list(shape), dtype))
        self.subs = {}

    def __getitem__(self, idx):
        return self.t[idx]

    def sub(self, key):
        if key not in self.subs:
            self.subs[key] = Buf(f"{self.name}.{key}")
        return self.subs[key]


class Ctx:
    NDS = 24

    def __init__(self, nc, es):
        self.nc = nc
        self.es = es
        self.eng = {"pe": nc.tensor, "act": nc.scalar, "dve": nc.vector, "pool": nc.gpsimd, "sp": nc.sync}
        self.sem = {k: es.enter_context(nc.semaphore("s_" + k)) for k in ("pe", "act", "dve", "pool")}
        self.cnt = {k: 0 for k in self.sem}
        self.seen = {k: {} for k in self.eng}
        self.dsem = [es.enter_context(nc.semaphore(f"s_dma{i}")) for i in range(self.NDS)]
        self.dcnt = [0] * self.NDS
        self.dnext = 0
        self.pending = {k: [] for k in self.sem}
        self.n_inst = 0
        self.all_bufs = []

    def _wait(self, e, h):
        kind, key, val = h
        if kind == "e" and key == e and (e == "pe" or not SAME_SYNC):
            return
        sk = (kind, key)
        if self.seen[e].get(sk, 0) >= val:
            return
        sem = self.sem[key] if kind == "e" else self.dsem[key]
        self.eng[e].wait_ge(sem, val)
        self.seen[e][sk] = val

    def _deps(self, e, reads, writes):
        hs = {}
        for b in reads:
            if b.w is not None:
                k = b.w[:2]
                hs[k] = max(hs.get(k, 0), b.w[2])
        for b in writes:
            if b.w is not None:
                k = b.w[:2]
                hs[k] = max(hs.get(k, 0), b.w[2])
            for k, v in b.r.items():
                hs[k] = max(hs.get(k, 0), v)
        for k, v in hs.items():
            self._wait(e, (k[0], k[1], v))

    def _mark(self, h, reads, writes):
        k = h[:2]
        for b in reads:
            b.r[k] = max(b.r.get(k, 0), h[2])
        for b in writes:
            b.w = h
            b.r = {}

    def op(self, e, fn, reads=(), writes=(), inc=True):
        pr = [b for b in reads if b.psum]
        if pr:
            reads = [b for b in reads if not b.psum]
            writes = list(writes) + [b for b in pr if b not in writes]
        self._deps(e, reads, writes)
        inst = fn(self.eng[e])
        self.n_inst += 1
        if inc:
            self.cnt[e] += 1
            inst.then_inc(self.sem[e], 1)
            h = ("e", e, self.cnt[e])
        else:
            h = ("e", e, self.cnt[e] + 1)
        self._mark(h, reads, writes)
        return h

    def dma(self, q, out, in_, reads=(), writes=(), **kw):
        i = self.dnext
        self.dnext = (self.dnext + 1) % self.NDS
        if self.dcnt[i]:
            self._wait(q, ("d", i, self.dcnt[i]))
        self._deps(q, reads, writes)
        inst = self.eng[q].dma_start(out=out, in_=in_, **kw)
        self.n_inst += 1
        self.dcnt[i] += 16
        inst.then_inc(self.dsem[i], 16)
        h = ("d", i, self.dcnt[i])
        self._mark(h, reads, writes)
        return h

    def wait_all(self, e, bufs):
        self._deps(e, (), bufs)

    def mm(self, out, lhsT, rhs, reads, writes, start=True, stop=True, inc=True):
        return self.op("pe", lambda E: E.matmul(out, lhsT, rhs, start=start, stop=stop), reads, writes, inc=inc)

    def tr(self, out, in_, ident, reads, writes, inc=True):
        return self.op("pe", lambda E: E.transpose(out, in_, ident), reads, writes, inc=inc)

    def act(self, out, in_, func, reads, writes, bias=None, scale=None, e="act", accum_out=None):
        kw = {}
        if bias is not None:
            kw["bias"] = bias
        if scale is not None:
            kw["scale"] = scale
        if accum_out is not None:
            kw["accum_out"] = accum_out
        return self.op(e, lambda E: E.activation(out=out, in_=in_, func=func, **kw), reads, writes)

    def tt(self, out, in0, in1, op, reads, writes, e="dve"):
        return self.op(e, lambda E: E.tensor_tensor(out=out, in0=in0, in1=in1, op=op), reads, writes)

    def ts(self, out, in0, s1, s2, op0, op1, reads, writes, e="dve"):
        if op1 is None:
            return self.op(e, lambda E: E.tensor_scalar(out=out, in0=in0, scalar1=s1, scalar2=None, op0=op0), reads, writes)
        return self.op(e, lambda E: E.tensor_scalar(out=out, in0=in0, scalar1=s1, scalar2=s2, op0=op0, op1=op1), reads, writes)

    def stt(self, out, in0, scalar, in1, op0, op1, reads, writes):
        return self.op("dve", lambda E: E.scalar_tensor_tensor(out=out, in0=in0, scalar=scalar, in1=in1, op0=op0, op1=op1), reads, writes)

    def cp(self, out, in_, reads, writes, e="dve"):
        if e == "act":
            return self.op("act", lambda E: E.copy(out=out, in_=in_), reads, writes)
        return self.op(e, lambda E: E.tensor_copy(out=out, in_=in_), reads, writes)

    def memset(self, ap, val, writes, e="dve"):
        return self.op(e, lambda E: E.memset(ap, val), (), writes)


def _consts():
    c = {}
    ident = np.eye(128, dtype=np.float32)
    c["ident"] = ident
    s = np.arange(128)[:, None] % 64
    t = np.arange(64)[None, :]
    su = (t > s).astype(np.float32)
    ui = (t >= s).astype(np.float32)
    sl = (t < s).astype(np.float32)
    half = (np.arange(128)[:, None] // 64 == np.arange(128)[None, :] // 64).astype(np.float32)
    def bd(m64):
        return np.concatenate([m64, m64], axis=1) * half
    c["maskX"] = np.concatenate([bd(sl), bd(su), bd(sl), bd(su)], axis=1)
    c["maskY"] = np.concatenate([bd(su), bd(ui), bd(ui)], axis=1)
    c["blk"] = half.copy()
    restart = np.ones((128, GRP), np.float32)
    restart[:, ::64] = 0.0
    c["restart"] = restart
    hsel = np.zeros((128, 4, 8), np.float32)
    for p_ in range(128):
        for hp in range(4):
            hsel[p_, hp, 2 * hp + p_ // 64] = 1.0
    c["hsel"] = hsel
    c["hselT"] = np.ascontiguousarray(hsel.transpose(2, 1, 0))
    return c


CONST_SHAPES = {"ident": [128, 128], "maskX": [128, 512], "maskY": [128, 384], "blk": [128, 128], "restart": [128, GRP], "hsel": [128, 4, 8], "hselT": [8, 4, 128]}


def build_nc(stage=99, dbg=False):
    nc = bass.Bass("TRN2", target_bir_lowering=False)

    def din(name, shape, dt=F32):
        return nc.dram_tensor(name, list(shape), dt, kind="ExternalInput").ap()

    xw = din("xw", [NSEG * SEG, D])
    flags = din("flags", [128, 5])
    c_fm = din("c_fm", [128, 8])
    w_ada = din("w_ada", [D, 6 * D])
    b_ada = din("b_ada", [128, 48])
    n1g = din("n1g", [128, 8])
    w_in = din("w_in", [D, 2816])
    mu = din("mu", [128, 14])
    vec4 = din("vec4", [128, 7, 4])
    w_dec = din("w_dec", [64, 512])
    w_aup = din("w_aup", [64, 512])
    w_gup = din("w_gup", [128, 512])
    sgu_g = din("sgu_g", [1, 512])
    sgu_b = din("sgu_b", [1, 512])
    w_sp = din("w_sp", [8, 128, 128])
    b_sp = din("b_sp", [8, 128])
    w_out = din("w_out", [D, D])
    n2g = din("n2g", [128, 8])
    w_rt = din("w_rt", [D, NE])
    e_bias = din("e_bias", [1, NE])
    nexp = NE + 1 if stage >= 3 else 1
    w1e = din("w1e", [nexp, D, DE])
    w3e = din("w3e", [nexp, D, DE])
    w2e = din("w2e", [nexp, DE, D])
    nfg = din("nfg", [1, D])
    cst = {k: din("c_" + k, shp) for k, shp in CONST_SHAPES.items()}
    out = nc.dram_tensor("out", [SEG, D], F32, kind="ExternalOutput").ap()
    x2d = nc.dram_tensor("x2_scratch", [SEG, D], F32, kind="Internal").ap()
    dbg_out = {}
    x2dbuf = Buf("x2dbuf")

    def dbg_tensor(name, shape):
        dbg_out[name] = nc.dram_tensor("dbg_" + name, list(shape), F32, kind="ExternalOutput").ap()
        return dbg_out[name]

    es = ExitStack()
    with es:
        K = Ctx(nc, es)
        T = lambda name, shape, dt=F32, psum=False: Tile(K, name, shape, dt, psum)
        def sb_used():
            return 0

        ident = T("ident", [128, 128])
        identb = T("identb", [128, 128], BF16)
        maskX = T("maskX", [128, 512])
        maskY = T("maskY", [128, 384])
        blk = T("blk", [128, 128])
        blkb = T("blkb", [128, 128], BF16)
        blk64 = T("blk64", [128, 128])
        restart = T("restart", [128, GRP])
        ones1 = T("ones1", [1, 128])
        flg = T("flg", [128, 5])
        K.dma("sp", ident[:], cst["ident"], (), [ident])
        K.dma("sp", maskX[:], cst["maskX"], (), [maskX])
        K.dma("sp", maskY[:], cst["maskY"], (), [maskY])
        K.dma("sp", blk[:], cst["blk"], (), [blk])
        K.dma("sp", restart[:], cst["restart"], (), [restart])
        K.dma("sp", flg[:], flags, (), [flg])
        K.cp(identb[:], ident[:], [ident], [identb])
        K.cp(blkb[:], blk[:], [blk], [blkb])
        K.ts(blk64[:], blk[:], 1.0 / 64.0, None, ALU.mult, None, [blk], [blk64])
        K.memset(ones1[:], 1.0, [ones1])

        banks = [T(f"bank{i}", [128, 512], F32, psum=True) for i in range(8)]

        n1g_t = T("n1g_t", [128, 8]); n2g_t = T("n2g_t", [128, 8]); mu_t = T("mu_t", [128, 14]); omm_t = T("omm_t", [128, 14])
        v4 = T("v4", [128, 7, 4]); cfm = T("cfm", [128, 8])
        K.dma("sp", n1g_t[:], n1g, (), [n1g_t]); K.dma("sp", n2g_t[:], n2g, (), [n2g_t])
        K.dma("sp", mu_t[:], mu, (), [mu_t]); K.dma("sp", v4[:], vec4, (), [v4]); K.dma("sp", cfm[:], c_fm, (), [cfm])
        K.ts(omm_t[:], mu_t[:], -1.0, 1.0, ALU.mult, ALU.add, [mu_t], [omm_t])
        omka = T("omka", [128, 4])
        K.ts(omka[:], v4[:, 3, :], -1.0, 1.0, ALU.mult, ALU.add, [v4], [omka])

        silc = T("silc", [128, 8])
        K.act(silc[:], cfm[:], AF.Silu, [cfm], [silc])
        modT = T("modT", [128, 48])
        K.dma("sp", modT[:], b_ada, (), [modT])
        g1rep = T("g1rep", [128, D]); g2rep = T("g2rep", [128, D])
        with ExitStack() as es2:
            K.es = es2
            wab = [T(f"wab{i}", [128, 3072]) for i in range(2)]
            dg = T("dg", [128, 128])
            it = 0
            for kc in range(8):
                for half in range(2):
                    wt = wab[it % 2]; it += 1
                    K.dma("sp", wt[:], w_ada[kc * 128:(kc + 1) * 128, half * 3072:(half + 1) * 3072], (), [wt])
                    for jc in range(24):
                        K.mm(banks[0][:, half * 24 + jc: half * 24 + jc + 1], wt[:, jc * 128:(jc + 1) * 128], silc[:, kc:kc + 1], [wt, silc], [banks[0]],
                             inc=(jc == 23))
                K.tt(modT[:], banks[0][:, 0:48], modT[:], ALU.add, [banks[0], modT], [modT])
            onesf = T("onesf", [128, 128])
            K.memset(onesf[:], 1.0, [onesf])
            for (idx, dst) in ((2, g1rep), (5, g2rep)):
                for kc in range(8):
                    K.ts(dg[:], ident[:], modT[:, idx * 8 + kc: idx * 8 + kc + 1], None, ALU.mult, None, [ident, modT], [dg])
                    K.mm(banks[1][:, 0:128], onesf[:], dg[:], [onesf, dg], [banks[1]])
                    K.cp(dst[:, kc * 128:(kc + 1) * 128], banks[1][:, 0:128], [banks[1]], [dst])
            K.wait_all("dve", wab + [dg, onesf]); K.wait_all("pe", wab + [dg, onesf]); K.wait_all("sp", wab)
            K.wait_all("act", wab + [dg, onesf]); K.wait_all("pool", wab + [dg, onesf])
        K.es = es
        A1 = T("A1", [128, 8]); A2 = T("A2", [128, 8])
        K.stt(A1[:], modT[:, 8:16], 1.0, n1g_t[:], ALU.add, ALU.mult, [modT, n1g_t], [A1])
        K.stt(A2[:], modT[:, 32:40], 1.0, n2g_t[:], ALU.add, ALU.mult, [modT, n2g_t], [A2])
        A1f = T("A1f", [128, 5, 8]); B1f = T("B1f", [128, 5, 8])
        for f in range(5):
            K.ts(A1f[:, f, :], A1[:], flg[:, f:f + 1], None, ALU.mult, None, [A1, flg], [A1f])
            K.ts(B1f[:, f, :], modT[:, 0:8], flg[:, f:f + 1], None, ALU.mult, None, [modT, flg], [B1f])

        def finish():
            for i in range(K.NDS):
                if K.dcnt[i]:
                    K._wait("sp", ("d", i, K.dcnt[i]))
            for e in ("pe", "act", "dve", "pool"):
                if K.cnt[e]:
                    K._wait("sp", ("e", e, K.cnt[e]))

        if dbg and stage == 0:
            d = dbg_tensor("modT", [128, 48])
            K.dma("sp", d, modT[:], [modT], ())
            d = dbg_tensor("A1", [128, 8])
            K.dma("sp", d, A1[:], [A1], ())
        if stage == 0:
            zt = T("zt", [128, D])
            K.memset(zt[:], 0.0, [zt])
            for i in range(16):
                K.dma("sp", out[i * 128:(i + 1) * 128, :], zt[:], [zt], ())
            finish()
            return nc, dbg_out

        es_scan = ExitStack()
        es_scan.__enter__()
        K.es = es_scan
        own_extra = stage >= 2
        winb = T("winb", [128, 8, 2816], BF16)
        for kc in range(8):
            for c0 in range(0, 2816, 1408):
                K.dma("pool", winb[:, kc, c0:c0 + 1408], w_in[kc * 128:(kc + 1) * 128, c0:c0 + 1408], (), [winb])
        wlo = T("wlo", [128, 512], BF16)
        wgb = T("wgb", [128, 512], BF16)
        K.dma("pool", wlo[0:64, :], w_dec, (), [wlo])
        K.dma("pool", wlo[64:128, :], w_aup, (), [wlo])
        K.dma("pool", wgb[:], w_gup, (), [wgb])
        hsel = T("hsel", [128, 4, 8], BF16); hselT = T("hselT", [8, 4, 128], BF16)
        K.dma("pool", hsel[:], cst["hsel"], (), [hsel]); K.dma("pool", hselT[:], cst["hselT"], (), [hselT])
        lrk = T("lrk", [128, 4, 128], BF16)
        for hp in range(4):
            K.ts(lrk[:, hp, :], blk[:], v4[:, 6, hp:hp + 1], None, ALU.mult, None, [blk, v4], [lrk])
        epsln = T("epsln", [128, 1])
        K.memset(epsln[:], 64e-5, [epsln])
        for bk in banks:
            K.memset(bk[:], 0.0, [bk])

        xt = T("xt", [128, 4, D])
        x2t = T("x2t", [128, D])
        if own_extra:
            woutb = T("woutb", [128, 8, D], BF16)
            for kc in range(8):
                K.dma("sp", x2t[:], w_out[kc * 128:(kc + 1) * 128, :], (), [x2t])
                K.tt(woutb[:, kc, :], x2t[:], g1rep[:], ALU.mult, [x2t, g1rep], [woutb])
            wsTb = T("wsTb", [128, 8, 128], BF16)
            for h in range(8):
                K.dma("sp", x2t[:, 0:128], w_sp[h], (), [x2t])
                K.tr(banks[0][:, 0:128], x2t[:, 0:128], ident[:], [x2t, ident], [banks[0]])
                K.cp(wsTb[:, h, :], banks[0][:, 0:128], [banks[0]], [wsTb])
                K.memset(wsTb[64:128, h, 0:64], 0.0, [wsTb])
            bsp = T("bsp", [128, 4, 128])
            for hp in range(4):
                for hh in range(2):
                    K.dma("sp", bsp[hh * 64:(hh + 1) * 64, hp, :], b_sp[2 * hp + hh, :].partition_broadcast(64), (), [bsp])
            lngr = T("lngr", [128, 512]); lnbr = T("lnbr", [128, 512])
            K.dma("sp", lngr[:], sgu_g[0, :].partition_broadcast(128), (), [lngr])
            K.dma("sp", lnbr[:], sgu_b[0, :].partition_broadcast(128), (), [lnbr])

        xy = T("xy", [128, 4096], BF16)
        hTt = T("hTt", [128, 8, GRP], BF16)
        uT = T("uT", [128, 4, GRP], BF16)
        ssq = T("ssq", [128, 4]); rstd = T("rstd", [128, 4])
        carry = T("carry", [128, 14])
        K.memset(carry[:], 0.0, [carry])
        shtmp = T("shtmp", [128, GRP + 1])
        sh12 = T("sh12", [128, GRP])
        twxa = T("twxa", [128, GRP], BF16); sgb = T("sgb", [128, GRP], BF16)
        shv = T("shv", [128, GRP])
        shk4 = [T(f"shk{i}", [128, GRP]) for i in range(4)]
        ss8 = T("ss8", [8, GRP]); rn8 = T("rn8", [8, GRP], BF16)
        lwp = T("lwp", [128, GRP]); av = T("av", [128, GRP]); gT = T("gT", [128, GRP])
        kkn = T("kkn", [128, GRP]); kmod = T("kmod", [128, GRP]); bb = T("bb", [128, GRP])
        cum = T("cum", [128, GRP])
        ep = T("ep", [128, GRP]); em = T("em", [128, GRP]); eq = T("eq", [128, GRP])
        tmpa = T("tmpa", [128, GRP]); tmpb = T("tmpb", [128, GRP])
        pc = T("pc", [128, 8])
        atb = T("atb", [128, GRP], BF16); rtb = T("rtb", [128, GRP], BF16); rtf = T("rtf", [128, GRP])
        btb = T("btb", [128, GRP], BF16); ktb = T("ktb", [128, GRP], BF16)
        bhb = T("bhb", [128, GRP], BF16); khb = T("khb", [128, GRP], BF16); vbb = T("vbb", [128, GRP], BF16)
        sqb = [atb, rtb, btb, ktb]
        rkb = T("rkb", [128, GRP], BF16); bv = T("bv", [128, GRP])
        TM = [T(f"TM{i}", [128, 3, 128], BF16) for i in range(2)]
        ZB = [T(f"ZB{i}", [128, 2, 2, 64], BF16) for i in range(2)]
        XQ = [[T(f"XQ{h}_{i}", [128, 3, 128], BF16) for i in range(2)] for h in range(2)]
        XQb = [[T(f"XQb{h}_{i}", [128, 3, 128], BF16) for i in range(2)] for h in range(2)]
        XQ2 = [XQ, XQb]
        mtmp = [[T(f"mtmp{p_}_{q}", [128, 128], BF16) for q in range(2)] for p_ in range(2)]
        YS = T("YS", [128, 2, 384], BF16)
        UW = T("UW", [128, 2, 2, 64], BF16)
        MT = [T(f"MT{i}", [128, 128], BF16) for i in range(2)]
        YSb = T("YSb", [128, 2, 128], BF16); UWb = T("UWb", [128, 2, 2, 64], BF16)
        MTb = [T(f"MTb{i}", [128, 128], BF16) for i in range(2)]
        YS2 = [YS, YSb]; UW2 = [UW, UWb]; MT2 = [MT, MTb]
        RhT = [T(f"RhT{i}", [128, 64], BF16) for i in range(2)]
        Hb = [[T(f"H{hp}_{i}", [128, 128], BF16) for i in range(2)] for hp in range(4)]
        hpar = [0, 0, 0, 0]
        for hp in range(4):
            K.memset(Hb[hp][0][:], 0.0, [Hb[hp][0]])
        YT = T("YT", [128, GRP])
        sh13 = YT; shr = cum; vtm = tmpb
        if own_extra:
            vnb = T("vnb", [128, 4, 512], BF16)
            bnst = T("bnst", [128, 6]); bnag = T("bnag", [128, 2]); lnr = T("lnr", [128, 2])
        bX, bY0, bY1, bA, bR, bE, bYg, bH = banks
        ptb = [banks[5][:, 0:256].bitcast(BF16), banks[6][:, 0:256].bitcast(BF16)]
        ptm = banks[6][:, 0:256].bitcast(BF16)

        dbg_el = dbg_tensor("el", [10, 128, GRP]) if (dbg and stage == 1) else None
        dbg_yt = dbg_tensor("yt", [4, 4, 128, GRP]) if (dbg and stage in (1, 2)) else None
        dbg_H = dbg_tensor("H", [4, 128, 128]) if (dbg and stage in (1, 2)) else None
        dbg_ya = dbg_tensor("ya", [4, 8, 128, GRP]) if (dbg and stage == 2) else None
        pj_cnt = [0]
        dump_n = [0]

        def dump(name, ap, shape, deps):
            if not (dbg and stage == 1):
                return
            dt_ = dbg_tensor(name, shape)
            st_ = T(f"dump{dump_n[0]}", shape); dump_n[0] += 1
            K.cp(st_[:], ap, deps, [st_])
            K.dma("sp", dt_, st_[:], [st_], ())


        def proj_shift(cc, dst, e2="dve"):
            bk = banks[pj_cnt[0] % 2]; pj_cnt[0] += 1
            for kc in range(8):
                K.mm(bk[:, :], winb[:, kc, cc * 128:(cc + 1) * 128], hTt[:, kc, :], [winb, hTt], [bk], start=(kc == 0), stop=(kc == 7), inc=(kc == 7))
            K.cp(shtmp[:, 0:1], carry[:, cc:cc + 1], [carry], [shtmp], e="pool")
            K.act(shtmp[:, 1:GRP + 1], bk[:, :], AF.Copy, [bk, mu_t], [shtmp], scale=mu_t[:, cc:cc + 1])
            K.cp(carry[:, cc:cc + 1], shtmp[:, GRP:GRP + 1], [shtmp], [carry], e="pool")
            K.stt(dst[:], bk[:, :], omm_t[:, cc:cc + 1], shtmp[:, 0:GRP], ALU.mult, ALU.add, [bk, omm_t, shtmp], [dst])

        g_first = 0 if stage != 1 else 0
        for g in range(g_first, NGRP):
            seg = g // 4
            own = g >= OWN_G0
            for tt in range(4):
                r0 = g * GRP + tt * 128
                K.dma("sp", xt[:, tt, :], xw[r0:r0 + 128, :], (), [xt])
            for tt in range(4):
                K.act(uT[:, 0:2, :].rearrange("p a b -> p (a b)"), xt[:, tt, :], AF.Square, [xt], [uT, ssq], accum_out=ssq[:, tt:tt + 1])
            K.ts(rstd[:], ssq[:], 1.0 / D, 1e-6, ALU.mult, ALU.add, [ssq], [rstd])
            K.act(rstd[:], rstd[:], AF.Sqrt, [rstd], [rstd])
            K.op("dve", lambda E: E.reciprocal(out=rstd[:], in_=rstd[:]), [rstd], [rstd])
            for tt in range(4):
                K.act(xy[:, tt * D:(tt + 1) * D], xt[:, tt, :], AF.Copy, [xt, rstd], [xy], scale=rstd[:, tt:tt + 1])
            for kc in range(8):
                pbk = banks[5 + kc % 2]
                pz = ptb[kc % 2]
                for tt in range(4):
                    K.tr(pz[:, tt * 128:(tt + 1) * 128], xy[:, tt * D + kc * 128: tt * D + (kc + 1) * 128], identb[:], [xy, identb], [pbk], inc=(tt == 3))
                K.act(hTt[:, kc, :], pz, AF.Identity, [pbk, A1f, B1f], [hTt],
                      scale=A1f[:, seg + 1, kc:kc + 1], bias=B1f[:, seg + 1, kc:kc + 1])

            if g == OWN_G0 - 1:
                for cc_ in (0, 1, 2, 3, 13):
                    proj_shift(cc_, sh13)
            proj_shift(12, sh12)
            K.act(twxa[0:64, :], sh12[0:64, :], AF.Tanh, [sh12], [twxa])
            K.cp(twxa[64:128, :], sh12[64:128, :], [sh12], [twxa], e="pool")
            if own:
                proj_shift(13, sh13)
                K.act(sgb[:], sh13[:], AF.Sigmoid, [sh13], [sgb])
            for hp in range(4):
                proj_shift(4 + hp, shk4[hp])
                K.act(sqb[hp][:], shk4[hp][:], AF.Square, [shk4[hp], v4], [sqb[hp]], scale=v4[:, 2, hp:hp + 1])
            for hp in range(4):
                K.mm(banks[2][0:8, :], hsel[:, hp, :], sqb[hp][:], [hsel, sqb[hp]], [banks[2]], start=(hp == 0), stop=(hp == 3), inc=(hp == 3))
            K.ts(ss8[:], banks[2][0:8, :], 1e-18, None, ALU.max, None, [banks[2]], [ss8])
            K.act(ss8[:], ss8[:], AF.Ln, [ss8], [ss8])
            K.act(rn8[:], ss8[:], AF.Exp, [ss8], [rn8], scale=-0.5)

            for hp in range(4):
                shk = shk4[hp]
                w0c = v4[:, 0, hp:hp + 1]; a0c = v4[:, 1, hp:hp + 1]; kkc = v4[:, 2, hp:hp + 1]; kac = v4[:, 3, hp:hp + 1]
                K.mm(banks[2][:, :], wlo[0:64, hp * 128:(hp + 1) * 128], twxa[0:64, :], [wlo, twxa], [banks[2]])
                K.mm(banks[3][:, :], wlo[64:128, hp * 128:(hp + 1) * 128], twxa[64:128, :], [wlo, twxa], [banks[3]])
                K.act(lwp[:], banks[2][:, :], AF.Sigmoid, [banks[2], v4], [lwp], bias=w0c)
                K.act(av[:], banks[3][:, :], AF.Sigmoid, [banks[3], v4], [av], bias=a0c)
                if own:
                    K.mm(banks[0][:, :], wgb[:, hp * 128:(hp + 1) * 128], sgb[:], [wgb, sgb], [banks[0]])
                    K.cp(gT[:], banks[0][:, :], [banks[0]], [gT], e="act")
                proj_shift(8 + hp, shv)
                K.mm(banks[2][:, :], hselT[:, hp, :], rn8[:], [hselT, rn8], [banks[2]])
                K.stt(kkn[:], shk[:], kkc, banks[2][:, :], ALU.mult, ALU.mult, [shk, v4, banks[2]], [kkn])
                K.ts(tmpa[:], av[:], kac, omka[:, hp:hp + 1], ALU.mult, ALU.add, [av, v4, omka], [tmpa])
                K.tt(kmod[:], shk[:], tmpa[:], ALU.mult, [shk, tmpa], [kmod])
                K.tt(bb[:], kkn[:], av[:], ALU.mult, [kkn, av], [bb], e="pool")
                K.op("dve", lambda E: E.tensor_tensor_scan(out=cum[:], data0=restart[:], data1=lwp[:], initial=0.0, op0=ALU.mult, op1=ALU.add),
                     [restart, lwp], [cum])
                K.tt(tmpb[:], cum[:], lwp[:], ALU.subtract, [cum, lwp], [tmpb], e="pool")
                K.act(ep[:], cum[:], AF.Exp, [cum], [ep], scale=-C0)
                K.act(em[:], cum[:], AF.Exp, [cum], [em], scale=C0)
                K.act(eq[:], tmpb[:], AF.Exp, [tmpb], [eq], scale=-C0)
                epv = ep[:].rearrange("p (c t) -> p c t", t=64)
                K.cp(pc[:], epv[:, :, 63], [ep], [pc])
                pcb = epv[:, :, 63:64].broadcast_to([128, 8, 64])
                v3 = lambda t_: t_[:].rearrange("p (c t) -> p c t", t=64)
                K.tt(tmpa[:], bb[:], em[:], ALU.mult, [bb, em], [tmpa])
                K.cp(btb[:], tmpa[:], [tmpa], [btb], e="pool")
                K.tt(v3(bhb), v3(tmpa), pcb, ALU.mult, [tmpa, ep], [bhb])
                K.tt(tmpb[:], kmod[:], em[:], ALU.mult, [kmod, em], [tmpb])
                K.cp(ktb[:], tmpb[:], [tmpb], [ktb], e="pool")
                K.tt(v3(khb), v3(tmpb), pcb, ALU.mult, [tmpb, ep], [khb])
                K.stt(atb[:], kkn[:], -1.0, eq[:], ALU.mult, ALU.mult, [kkn, eq], [atb])
                K.cp(vbb[:], shv[:], [shv], [vbb], e="act")
                if own:
                    proj_shift(hp, shr)
                    K.tt(rtf[:], shr[:], ep[:], ALU.mult, [shr, ep], [rtf])
                    K.cp(rtb[:], rtf[:], [rtf], [rtb], e="pool")
                    K.tt(rkb[:], shr[:], kmod[:], ALU.mult, [shr, kmod], [rkb])
                    K.mm(banks[3][:, :], lrk[:, hp, :], rkb[:], [lrk, rkb], [banks[3]])
                    K.tt(bv[:], banks[3][:, :], shv[:], ALU.mult, [banks[3], shv], [bv])
                if dbg_el is not None and g == NGRP - 1 and hp == 1:
                    for i, t_ in enumerate((lwp, av, kkn, kmod, bb, cum, shv, gT, shr, bv)):
                        K.dma("sp", dbg_el[i], t_[:], [t_], ())

                def cp_gen(cp_):
                    p_ = cp_ % 2
                    B1, B2, B3, B4 = banks[4 * p_:4 * p_ + 4]
                    ptm_p = B3[:, 0:256].bitcast(BF16)
                    tok = slice(cp_ * 128, (cp_ + 1) * 128)
                    TMt = TM[p_]; ZBt = ZB[p_]; XQp = XQ2[p_]; YSp = YS2[p_]; UWp = UW2[p_]; MTp = MT2[p_]
                    for i, src in enumerate((atb, bhb, khb, vbb)):
                        K.tr(ptm_p[:, i * 128:(i + 1) * 128], src[:, tok], identb[:], [src, identb], [B3], inc=(i == 3))
                    K.cp(TMt[:].rearrange("p a b -> p (a b)"), ptm_p[:, 128:512], [B3], [TMt])
                    K.cp(ZBt[:, :, 1, :], ptm_p[:, 0:128].rearrange("p (h v) -> p h v", h=2), [B3], [ZBt], e="act")
                    yield
                    for h in range(2):
                        hb = slice(h * 64, (h + 1) * 64)
                        bXh = B1 if h == 0 else B2
                        for q in range(2):
                            qp = slice(q * 64, (q + 1) * 64)
                            tq = slice(cp_ * 128 + q * 64, cp_ * 128 + (q + 1) * 64)
                            K.mm(bXh[qp, q * 64:(q + 1) * 64], atb[hb, tq], btb[hb, tq], [btb, atb], [bXh], inc=False)
                            K.mm(bXh[qp, 128 + q * 64:128 + (q + 1) * 64], btb[hb, tq], atb[hb, tq], [btb, atb], [bXh], inc=False)
                            K.mm(bXh[qp, 384 + q * 64:384 + (q + 1) * 64], ktb[hb, tq], atb[hb, tq], [ktb, atb], [bXh], inc=(q == 1))
                    yield
                    for h in range(2):
                        bXh = B1 if h == 0 else B2
                        K.tt(XQp[h][0][:, 0:2, :].rearrange("p a t -> p (a t)"), bXh[:, 0:256], maskX[:, 0:256], ALU.mult, [bXh, maskX], [XQp[h][0]])
                        K.cp(XQp[h][0][:, 2, :], identb[:], [identb], [XQp[h][0]], e="pool")
                        K.tt(YSp[:, h, 0:128], bXh[:, 384:512], maskY[:, 0:128], ALU.mult, [bXh, maskY], [YSp])
                    for h in range(2):
                        K.mm(B3[:, 256 + h * 64:256 + (h + 1) * 64], YSp[:, h, 0:128], TMt[:, 2, h * 64:(h + 1) * 64], [YSp, TMt], [B3], inc=(h == 1))
                    K.cp(ZBt[:, :, 0, :], B3[:, 256:384].rearrange("p (h v) -> p h v", h=2), [B3], [ZBt], e="act")
                    yield
                    bN = [B1, B2]
                    for it in range(6):
                        for h in range(2):
                            Xc = XQp[h][it % 2]
                            if it < 5:
                                K.mm(bN[h][:, 128:384], Xc[:, 0, :], Xc[:, 1:3, :].rearrange("p a t -> p (a t)"), [Xc], [bN[h]], inc=False)
                                K.mm(bN[h][:, 0:128], Xc[:, 1, :], Xc[:, 0, :], [Xc], [bN[h]])
                            else:
                                K.mm(bN[h][:, 256:384], Xc[:, 0, :], Xc[:, 2, :], [Xc], [bN[h]])
                        yield
                        for h in range(2):
                            Xc = XQp[h][it % 2]; Xn = XQp[h][1 - it % 2]
                            if it < 5:
                                K.cp(Xn[:, 0:2, :].rearrange("p a t -> p (a t)"), bN[h][:, 0:256], [bN[h]], [Xn], e="act")
                            K.tt(Xn[:, 2, :], bN[h][:, 256:384], Xc[:, 2, :], ALU.add, [bN[h], Xc], [Xn])
                        yield
                    for h in range(2):
                        K.mm(B4[:, h * 128:(h + 1) * 128], XQp[h][0][:, 2, :], ZBt[:, h, :, :].rearrange("p a v -> p (a v)"), [XQp[h][0], ZBt], [B4], inc=(h == 1))
                    K.cp(UWp[:].rearrange("p k h v -> p h k v"), B4[:, 0:256].rearrange("p (h k v) -> p h k v", h=2, k=2), [B4], [UWp])
                    yield
                    mtbank = [B3, B1]
                    for q in range(2):
                        qp = slice(q * 64, (q + 1) * 64)
                        for h in range(2):
                            hs_ = slice(h * 64, (h + 1) * 64)
                            K.mm(mtbank[q][hs_, 384 + h * 64:384 + (h + 1) * 64], UWp[qp, 1, h, :], TMt[qp, 0, hs_], [UWp, TMt], [mtbank[q]], inc=(h == 1))
                    yield
                    for q in range(2):
                        ck = cp_ * 2 + q
                        K.tt(mtmp[p_][q][:], mtbank[q][:, 384:512], blk[:], ALU.mult, [mtbank[q], blk], [mtmp[p_][q]])
                        K.stt(MTp[q][:], ident[:], pc[:, ck:ck + 1], mtmp[p_][q][:], ALU.mult, ALU.add, [ident, pc, mtmp[p_][q]], [MTp[q]])
                    hbank = [B4, B2]; hcol = [256, 384]
                    for q in range(2):
                        qp = slice(q * 64, (q + 1) * 64)
                        for h in range(2):
                            hs_ = slice(h * 64, (h + 1) * 64)
                            c0_ = hcol[q] + h * 64
                            K.mm(hbank[q][hs_, c0_:c0_ + 64], TMt[qp, 0, hs_], UWp[qp, 0, h, :], [TMt, UWp], [hbank[q]], start=True, stop=False, inc=False)
                            K.mm(hbank[q][hs_, c0_:c0_ + 64], TMt[qp, 1, hs_], TMt[qp, 2, hs_], [TMt], [hbank[q]], start=False, stop=False, inc=(h == 1))
                    yield
                    for q in range(2):
                        Hc = Hb[hp][hpar[hp]]; Hn = Hb[hp][1 - hpar[hp]]
                        hreg = hbank[q][:, hcol[q]:hcol[q] + 128]
                        K.mm(hreg, MTp[q][:], Hc[:], [MTp[q], Hc], [hbank[q]], start=False, stop=True)
                        K.cp(Hn[:], hreg, [hbank[q]], [Hn])
                        hpar[hp] = 1 - hpar[hp]
                    yield

                if not own:
                    gens = [cp_gen(c_) for c_ in range(4)]
                    active = []; steps = {}
                    nxt = 0
                    LAG = 4
                    while nxt < 4 or active:
                        if nxt < 4 and len(active) < 2 and (not active or steps[id(active[-1])] >= LAG):
                            active.append(gens[nxt]); steps[id(gens[nxt])] = 0; nxt += 1
                        for gen_ in list(active):
                            try:
                                next(gen_); steps[id(gen_)] += 1
                            except StopIteration:
                                active.remove(gen_)
                for cp_ in (range(4) if own else ()):
                    tok = slice(cp_ * 128, (cp_ + 1) * 128)
                    TMt = TM[cp_ % 2]; ZBt = ZB[cp_ % 2]
                    for i, src in enumerate((atb, bhb, khb, vbb)):
                        K.tr(ptm[:, i * 128:(i + 1) * 128], src[:, tok], identb[:], [src, identb], [bYg], inc=(i == 3))
                    K.cp(TMt[:].rearrange("p a b -> p (a b)"), ptm[:, 128:512], [bYg], [TMt])
                    K.cp(ZBt[:, :, 1, :], ptm[:, 0:128].rearrange("p (h v) -> p h v", h=2), [bYg], [ZBt], e="act")
                    for h in range(2):
                        hb = slice(h * 64, (h + 1) * 64)
                        bY = bY0 if h == 0 else bY1
                        bXh = bX if h == 0 else bA
                        for q in range(2):
                            qp = slice(q * 64, (q + 1) * 64)
                            tq = slice(cp_ * 128 + q * 64, cp_ * 128 + (q + 1) * 64)
                            K.mm(bXh[qp, h * 256 + q * 64: h * 256 + (q + 1) * 64], atb[hb, tq], btb[hb, tq], [btb, atb], [bXh], inc=False)
                            K.mm(bXh[qp, h * 256 + 128 + q * 64: h * 256 + 128 + (q + 1) * 64], btb[hb, tq], atb[hb, tq], [btb, atb], [bXh], inc=False)
                            K.mm(bY[qp, q * 64:(q + 1) * 64], ktb[hb, tq], atb[hb, tq], [ktb, atb], [bY], inc=((q == 1) and not own))
                            if own:
                                K.mm(bY[qp, 128 + q * 64:128 + (q + 1) * 64], ktb[hb, tq], rtb[hb, tq], [ktb, rtb], [bY], inc=False)
                                K.mm(bY[qp, 256 + q * 64:256 + (q + 1) * 64], btb[hb, tq], rtb[hb, tq], [btb, rtb], [bY], inc=(q == 1))
                    ny = 384 if own else 128
                    for h in range(2):
                        bXh = bX if h == 0 else bA
                        bY = bY0 if h == 0 else bY1
                        K.tt(XQ[h][0][:, 0:2, :].rearrange("p a t -> p (a t)"), bXh[:, h * 256:(h + 1) * 256], maskX[:, h * 256:(h + 1) * 256], ALU.mult,
                             [bXh, maskX], [XQ[h][0]])
                        K.cp(XQ[h][0][:, 2, :], identb[:], [identb], [XQ[h][0]], e="pool")
                        K.tt(YS[:, h, 0:ny], bY[:, 0:ny], maskY[:, 0:ny], ALU.mult, [bY, maskY], [YS])
                    for h in range(2):
                        K.mm(bR[:, 256 + h * 64:256 + (h + 1) * 64], YS[:, h, 0:128], TMt[:, 2, h * 64:(h + 1) * 64], [YS, TMt], [bR], inc=(h == 1))
                    K.cp(ZBt[:, :, 0, :], bR[:, 256:384].rearrange("p (h v) -> p h v", h=2), [bR], [ZBt], e="act")
                    bN = [bX, bA]
                    for it in range(6):
                        for h in range(2):
                            Xc = XQ[h][it % 2]
                            if it < 5:
                                K.mm(bN[h][:, 128:384], Xc[:, 0, :], Xc[:, 1:3, :].rearrange("p a t -> p (a t)"), [Xc], [bN[h]], inc=False)
                                K.mm(bN[h][:, 0:128], Xc[:, 1, :], Xc[:, 0, :], [Xc], [bN[h]])
                            else:
                                K.mm(bN[h][:, 256:384], Xc[:, 0, :], Xc[:, 2, :], [Xc], [bN[h]])
                        for h in range(2):
                            Xc = XQ[h][it % 2]; Xn = XQ[h][1 - it % 2]
                            if it < 5:
                                K.cp(Xn[:, 0:2, :].rearrange("p a t -> p (a t)"), bN[h][:, 0:256], [bN[h]], [Xn], e="act")
                            K.tt(Xn[:, 2, :], bN[h][:, 256:384], Xc[:, 2, :], ALU.add, [bN[h], Xc], [Xn])
                    for h in range(2):
                        K.mm(bE[:, h * 128:(h + 1) * 128], XQ[h][0][:, 2, :], ZBt[:, h, :, :].rearrange("p a v -> p (a v)"), [XQ[h][0], ZBt], [bE], inc=(h == 1))
                    K.cp(UW[:].rearrange("p k h v -> p h k v"), bE[:, 0:256].rearrange("p (h k v) -> p h k v", h=2, k=2), [bE], [UW])
                    mtreg = [bR[:, 384:512], bE[:, 384:512]]; mtbank = [bR, bE]
                    for q in range(2):
                        qp = slice(q * 64, (q + 1) * 64)
                        for h in range(2):
                            hs_ = slice(h * 64, (h + 1) * 64)
                            K.mm(mtbank[q][hs_, 384 + h * 64:384 + (h + 1) * 64], UW[qp, 1, h, :], TMt[qp, 0, hs_], [UW, TMt], [mtbank[q]], inc=(h == 1))
                    for q in range(2):
                        ck = cp_ * 2 + q
                        K.stt(MT[q][:], ident[:], pc[:, ck:ck + 1], mtreg[q], ALU.mult, ALU.add, [ident, pc, mtbank[q]], [MT[q]])
                    hbank = [bH, bX]
                    for q in range(2):
                        qp = slice(q * 64, (q + 1) * 64)
                        for h in range(2):
                            hs_ = slice(h * 64, (h + 1) * 64)
                            K.mm(hbank[q][hs_, h * 64:(h + 1) * 64], TMt[qp, 0, hs_], UW[qp, 0, h, :], [TMt, UW], [hbank[q]], start=True, stop=False, inc=False)
                            K.mm(hbank[q][hs_, h * 64:(h + 1) * 64], TMt[qp, 1, hs_], TMt[qp, 2, hs_], [TMt], [hbank[q]], start=False, stop=False, inc=(h == 1))
                    for q in range(2):
                        qp = slice(q * 64, (q + 1) * 64)
                        tq = slice(cp_ * 128 + q * 64, cp_ * 128 + (q + 1) * 64)
                        Hc = Hb[hp][hpar[hp]]; Hn = Hb[hp][1 - hpar[hp]]
                        hreg = hbank[q][:, 0:128]
                        if own:
                            RhTt = RhT[q]
                            yreg = bYg[:, 256 + q * 64:256 + (q + 1) * 64]
                            for h in range(2):
                                hs_ = slice(h * 64, (h + 1) * 64)
                                K.mm(bE[hs_, 256 + q * 64:256 + (q + 1) * 64], UW[qp, 1, h, :], YS[qp, h, 256 + q * 64:256 + (q + 1) * 64], [UW, YS], [bE], inc=(h == 1))
                            K.tt(RhTt[:], bE[:, 256 + q * 64:256 + (q + 1) * 64], rtf[:, tq], ALU.add, [bE, rtf], [RhTt])
                            for h in range(2):
                                hs_ = slice(h * 64, (h + 1) * 64)
                                K.mm(bYg[hs_, 256 + q * 64:256 + (q + 1) * 64], UW[qp, 0, h, :], YS[qp, h, 256 + q * 64:256 + (q + 1) * 64], [UW, YS], [bYg], start=True, stop=False, inc=False)
                                K.mm(bYg[hs_, 256 + q * 64:256 + (q + 1) * 64], TMt[qp, 2, hs_], YS[qp, h, 128 + q * 64:128 + (q + 1) * 64], [TMt, YS], [bYg], start=False, stop=False, inc=False)
                            K.mm(yreg, Hc[:], RhTt[:], [Hc, RhTt], [bYg], start=False, stop=True)
                            K.cp(YT[:, tq], yreg, [bYg], [YT], e="act")
                        K.mm(hreg, MT[q][:], Hc[:], [MT[q], Hc], [hbank[q]], start=False, stop=True)
                        K.cp(Hn[:], hreg, [hbank[q]], [Hn])
                        hpar[hp] = 1 - hpar[hp]
                if dbg_yt is not None and own:
                    K.dma("sp", dbg_yt[g - OWN_G0, hp], YT[:], [YT], ())
                if dbg and stage == 1 and hp == 0:
                    dump(f"Hg{g}", Hb[0][hpar[0]][:], [128, 128], [Hb[0][hpar[0]]])
                if own_extra and own:
                    lgc = v4[:, 4, hp:hp + 1]; lbc = v4[:, 5, hp:hp + 1]
                    K.mm(banks[2][:, :], blk64[:], YT[:], [blk64, YT], [banks[2]])
                    K.tt(tmpa[:], YT[:], banks[2][:, :], ALU.subtract, [YT, banks[2]], [tmpa])
                    K.act(tmpb[:], tmpa[:], AF.Square, [tmpa], [tmpb])
                    K.mm(banks[3][:, :], blk64[:], tmpb[:], [blk64, tmpb], [banks[3]])
                    K.act(tmpb[:], banks[3][:, :], AF.Ln, [banks[3], epsln], [tmpb], bias=epsln[:, 0:1])
                    K.act(tmpb[:], tmpb[:], AF.Exp, [tmpb], [tmpb], scale=-0.5)
                    K.tt(tmpa[:], tmpa[:], tmpb[:], ALU.mult, [tmpa, tmpb], [tmpa])
                    K.ts(tmpa[:], tmpa[:], lgc, lbc, ALU.mult, ALU.add, [tmpa, v4], [tmpa])
                    K.tt(tmpa[:], tmpa[:], bv[:], ALU.add, [tmpa, bv], [tmpa], e="pool")
                    K.tt(xy[:, hp * GRP:(hp + 1) * GRP], tmpa[:], gT[:], ALU.mult, [tmpa, gT], [xy])
            if own_extra and own:
                for i in range(4):
                    bk = banks[i % 2]
                    for kc in range(8):
                        K.mm(bk[:, :], winb[:, kc, 1792 + i * 128:1792 + (i + 1) * 128], hTt[:, kc, :], [winb, hTt], [bk], start=(kc == 0), stop=(kc == 7), inc=(kc == 7))
                    K.act(uT[:, i, :], bk[:, :], AF.Gelu_apprx_tanh, [bk], [uT])
                spb = [banks[2], banks[3], banks[6], banks[7]]
                for tt in range(4):
                    for kc in range(8):
                        K.mm(banks[0][:, :], hTt[:, kc, tt * 128:(tt + 1) * 128], winb[:, kc, 2304:2816], [winb, hTt], [banks[0]], start=(kc == 0), stop=(kc == 7), inc=(kc == 7))
                    K.act(vtm[:], banks[0][:, :], AF.Gelu_apprx_tanh, [banks[0]], [vtm])
                    K.op("dve", lambda E: E.bn_stats(out=bnst[:], in_=vtm[:]), [vtm], [bnst])
                    K.op("dve", lambda E: E.bn_aggr(out=bnag[:], in_=bnst[:]), [bnst], [bnag])
                    K.ts(lnr[:, 0:1], bnag[:, 1:2], 1e-5, None, ALU.add, None, [bnag], [lnr])
                    K.act(lnr[:, 0:1], lnr[:, 0:1], AF.Sqrt, [lnr], [lnr])
                    K.op("dve", lambda E: E.reciprocal(out=lnr[:, 1:2], in_=lnr[:, 0:1]), [lnr], [lnr])
                    K.ts(vtm[:], vtm[:], bnag[:, 0:1], lnr[:, 1:2], ALU.subtract, ALU.mult, [vtm, bnag, lnr], [vtm])
                    K.tt(vtm[:], vtm[:], lngr[:], ALU.mult, [vtm, lngr], [vtm], e="pool")
                    K.tt(vnb[:, tt, :], vtm[:], lnbr[:], ALU.add, [vtm, lnbr], [vnb], e="pool")
                    for h in range(8):
                        K.mm(spb[h // 2][(h % 2) * 64:(h % 2) * 64 + 64, tt * 128:(tt + 1) * 128], vnb[:, tt, h * 64:(h + 1) * 64], wsTb[:, h, :],
                             [vnb, wsTb], [spb[h // 2]], inc=(h % 2 == 1))
                for hp in range(4):
                    K.tt(tmpa[:].rearrange("p (r t) -> p r t", r=4), spb[hp][:, :].rearrange("p (r t) -> p r t", r=4),
                         bsp[:, hp:hp + 1, :].broadcast_to([128, 4, 128]), ALU.add, [spb[hp], bsp], [tmpa])
                    K.tt(xy[:, (4 + hp) * GRP:(5 + hp) * GRP], tmpa[:], uT[:, hp, :], ALU.mult, [tmpa, uT], [xy])
                if dbg_ya is not None:
                    for i in range(8):
                        K.cp(tmpa[:], xy[:, i * GRP:(i + 1) * GRP], [xy], [tmpa])
                        K.dma("sp", dbg_ya[g - OWN_G0, i], tmpa[:], [tmpa], ())
                for tt in range(4):
                    for half in range(2):
                        bk = banks[half]
                        for kc in range(8):
                            K.mm(bk[:, :], xy[:, kc * GRP + tt * 128: kc * GRP + (tt + 1) * 128], woutb[:, kc, half * 512:(half + 1) * 512], [xy, woutb], [bk],
                                 start=(kc == 0), stop=(kc == 7), inc=(kc == 7))
                        K.tt(x2t[:, half * 512:(half + 1) * 512], bk[:, :], xt[:, tt, half * 512:(half + 1) * 512], ALU.add, [bk, xt], [x2t])
                    r0 = (g - OWN_G0) * GRP + tt * 128
                    K.dma("sp", x2d[r0:r0 + 128, :], x2t[:], [x2t], [x2dbuf])
        if dbg_H is not None:
            for hp in range(4):
                K.cp(tmpa[:, 0:128], Hb[hp][hpar[hp]][:], [Hb[hp][hpar[hp]]], [tmpa])
                K.dma("sp", dbg_H[hp], tmpa[:, 0:128], [tmpa], ())
        if stage <= 2:
            if stage == 2:
                for i in range(16):
                    K.dma("sp", x2t[:], x2d[i * 128:(i + 1) * 128, :], [x2dbuf], [x2t])
                    K.dma("sp", out[i * 128:(i + 1) * 128, :], x2t[:], [x2t], ())
            else:
                K.memset(x2t[:], 0.0, [x2t])
                for i in range(16):
                    K.dma("sp", out[i * 128:(i + 1) * 128, :], x2t[:], [x2t], ())
            finish()
            es_scan.__exit__(None, None, None)
            return nc, dbg_out
        allb = list(K.all_bufs)
        for e in ("pe", "act", "dve", "pool", "sp"):
            K._deps(e, (), allb)
        es_scan.__exit__(None, None, None)
        K.es = es

        acc = T("acc", [128, 16, D])
        h2T = T("h2T", [128, 8, SEG], BF16)
        gates = T("gates", [128, 16, NE + 1])
        ebr = T("ebr", [128, NE]); nfr = T("nfr", [128, D]); wrt = T("wrt", [128, 8, NE])
        K.dma("sp", ebr[:], e_bias[0, :].partition_broadcast(128), (), [ebr])
        K.dma("sp", nfr[:], nfg[0, :].partition_broadcast(128), (), [nfr])
        K.dma("sp", wrt[:], w_rt.rearrange("(kc p) n -> p kc n", p=128), (), [wrt])
        xnf = T("xnf", [128, D]); h2f = T("h2f", [128, 8, 128])
        sc = T("sc", [128, NE]); bi = T("bi", [128, NE]); mk = T("mk", [128, NE]); m8 = T("m8", [128, 8, 8])
        gs = T("gs", [128, 8]); gs8 = T("gs8", [128, 8]); gm = T("gm", [128, 8]); pen = T("pen", [128, 8]); t8 = T("t8", [128, 8])
        ssq2 = T("ssq2", [128, 1]); rs2 = T("rs2", [128, 1]); rsum = T("rsum", [128, 1]); junk2 = T("junk2", [128, D], BF16)
        dbg_g = dbg_tensor("gates", [16, 128, NE + 1]) if dbg else None

        def rms_rstd(t):
            K.act(junk2[:], acc[:, t, :], AF.Square, [acc.sub(t)], [junk2, ssq2], accum_out=ssq2[:, 0:1])
            K.ts(rs2[:], ssq2[:], 1.0 / D, 1e-6, ALU.mult, ALU.add, [ssq2], [rs2])
            K.act(rs2[:], rs2[:], AF.Sqrt, [rs2], [rs2])
            K.op("dve", lambda E: E.reciprocal(out=rs2[:], in_=rs2[:]), [rs2], [rs2])

        for t in range(16):
            K.dma("sp", acc[:, t, :], x2d[t * 128:(t + 1) * 128, :], [x2dbuf], [acc.sub(t)])
            rms_rstd(t)
            K.act(xnf[:], acc[:, t, :], AF.Copy, [acc.sub(t), rs2], [xnf], scale=rs2[:, 0:1])
            for kc in range(8):
                bk = banks[kc // 4]
                K.tr(bk[:, (kc % 4) * 128:(kc % 4 + 1) * 128], xnf[:, kc * 128:(kc + 1) * 128], ident[:], [xnf, ident], [bk], inc=(kc % 4 == 3))
            for kc in range(8):
                bk = banks[kc // 4]; reg = bk[:, (kc % 4) * 128:(kc % 4 + 1) * 128]
                K.act(h2T[:, kc, t * 128:(t + 1) * 128], reg, AF.Identity, [bk, A2, modT], [h2T.sub(t)], scale=A2[:, kc:kc + 1], bias=modT[:, 24 + kc:25 + kc])
                K.ts(h2f[:, kc, :], reg, A2[:, kc:kc + 1], modT[:, 24 + kc:25 + kc], ALU.mult, ALU.add, [bk, A2, modT], [h2f])
            for kc in range(8):
                K.mm(banks[2][:, 0:NE], h2f[:, kc, :], wrt[:, kc, :], [h2f, wrt], [banks[2]], start=(kc == 0), stop=(kc == 7), inc=(kc == 7))
            K.act(sc[:], banks[2][:, 0:NE], AF.Sigmoid, [banks[2]], [sc])
            K.tt(bi[:], sc[:], ebr[:], ALU.add, [sc, ebr], [bi])
            for gi in range(8):
                K.op("dve", lambda E: E.max(out=m8[:, gi, :], in_=bi[:, gi * 8:(gi + 1) * 8]), [bi], [m8])
            K.tt(gs[:], m8[:, :, 0], m8[:, :, 1], ALU.add, [m8], [gs])
            K.op("dve", lambda E: E.max(out=gs8[:], in_=gs[:]), [gs], [gs8])
            K.ts(gm[:], gs[:], gs8[:, 3:4], None, ALU.is_ge, None, [gs, gs8], [gm])
            K.ts(pen[:], gm[:], 1e9, -1e9, ALU.mult, ALU.add, [gm], [pen])
            mkv = mk[:].rearrange("p (g e) -> p g e", e=8); biv = bi[:].rearrange("p (g e) -> p g e", e=8)
            K.tt(mkv, biv, gm[:].unsqueeze(2).broadcast_to([128, 8, 8]), ALU.mult, [bi, gm], [mk])
            K.tt(mkv, mkv, pen[:].unsqueeze(2).broadcast_to([128, 8, 8]), ALU.add, [mk, pen], [mk])
            K.op("dve", lambda E: E.max(out=t8[:], in_=mk[:]), [mk], [t8])
            K.ts(mk[:], mk[:], t8[:, 7:8], None, ALU.is_ge, None, [mk, t8], [mk])
            K.tt(mk[:], mk[:], sc[:], ALU.mult, [mk, sc], [mk])
            K.op("dve", lambda E: E.tensor_reduce(out=rsum[:], in_=mk[:], axis=AX.X, op=ALU.add), [mk], [rsum])
            K.op("dve", lambda E: E.reciprocal(out=rsum[:], in_=rsum[:]), [rsum], [rsum])
            K.ts(gates[:, t, 0:NE], mk[:], rsum[:, 0:1], 2.5, ALU.mult, ALU.mult, [mk, rsum], [gates])
            K.memset(gates[:, t, NE:NE + 1], 1.0, [gates])
            if dbg_g is not None:
                K.dma("sp", dbg_g[t], gates[:, t, :], [gates], ())

        w1b = [T(f"w1b{i}", [128, 8, DE], BF16) for i in range(2)]
        w3b = [T(f"w3b{i}", [128, 8, DE], BF16) for i in range(2)]
        w2b = [T(f"w2b{i}", [128, 2, D], BF16) for i in range(2)]
        actb = [T(f"actb{i}", [128, 2, GRP], BF16) for i in range(2)]
        sgt = [T(f"sgt{i}", [128, GRP]) for i in range(2)]
        n_exp = NE + 1
        for e in range(n_exp):
            i = e % 2
            for hf in range(2):
                K.dma("pool", w1b[i][:, hf * 4:(hf + 1) * 4, :], w1e[e, hf * 512:(hf + 1) * 512, :].rearrange("(kc p) n -> p kc n", p=128), (), [w1b[i]])
                K.dma("pool", w3b[i][:, hf * 4:(hf + 1) * 4, :], w3e[e, hf * 512:(hf + 1) * 512, :].rearrange("(kc p) n -> p kc n", p=128), (), [w3b[i]])
                K.dma("pool", w2b[i][:, hf, :], w2e[e, hf * 128:(hf + 1) * 128, :], (), [w2b[i]])
            for hf in range(2):
                K.tt(w2b[i][:, hf, :], w2b[i][:, hf, :], g2rep[:], ALU.mult, [w2b[i], g2rep], [w2b[i]], e="pool")
            for tg in range(4):
                ab = actb[tg % 2]
                h2r = [h2T.sub(4 * tg + j) for j in range(4)]
                for cc in range(2):
                    bG = banks[cc * 2]; bU = banks[cc * 2 + 1]
                    for kc in range(8):
                        K.mm(bG[:, :], w1b[i][:, kc, cc * 128:(cc + 1) * 128], h2T[:, kc, tg * GRP:(tg + 1) * GRP], [w1b[i]] + h2r, [bG],
                             start=(kc == 0), stop=(kc == 7), inc=(kc == 7))
                    for kc in range(8):
                        K.mm(bU[:, :], w3b[i][:, kc, cc * 128:(cc + 1) * 128], h2T[:, kc, tg * GRP:(tg + 1) * GRP], [w3b[i]] + h2r, [bU],
                             start=(kc == 0), stop=(kc == 7), inc=(kc == 7))
                    K.act(sgt[cc][:], bG[:, :], AF.Silu, [bG], [sgt[cc]])
                    K.tt(ab[:, cc, :], sgt[cc][:], bU[:, :], ALU.mult, [sgt[cc], bU], [ab])
                for tt in range(4):
                    t = tg * 4 + tt
                    for half in range(2):
                        bO = banks[4 + (tt % 2) * 2 + half]
                        for cc in range(2):
                            K.mm(bO[:, :], ab[:, cc, tt * 128:(tt + 1) * 128], w2b[i][:, cc, half * 512:(half + 1) * 512], [ab, w2b[i]], [bO],
                                 start=(cc == 0), stop=(cc == 1), inc=(cc == 1))
                        K.stt(acc[:, t, half * 512:(half + 1) * 512], bO[:, :], gates[:, t, e:e + 1], acc[:, t, half * 512:(half + 1) * 512],
                              ALU.mult, ALU.add, [bO, gates, acc.sub(t)], [acc.sub(t)])
        for t in range(16):
            rms_rstd(t)
            K.stt(xnf[:], acc[:, t, :], rs2[:, 0:1], nfr[:], ALU.mult, ALU.mult, [acc.sub(t), rs2, nfr], [xnf])
            K.dma("sp", out[t * 128:(t + 1) * 128, :], xnf[:], [xnf], ())
        finish()
    return nc, dbg_out


def _fm(v, n):
    return np.ascontiguousarray(np.asarray(v, np.float32).reshape(n, 128).T)


def make_in_maps(inputs, small=False):
    g = lambda k: np.asarray(inputs[k], np.float32)
    x = g("x"); c = g("c")
    consts = _consts()
    shared = {
        "w_ada": g("w_ada")[0], "b_ada": _fm(g("b_ada")[0], 48), "n1g": _fm(g("norm1_g")[0], 8),
        "w_in": g("w_in")[0], "mu": _fm(g("mu_shift")[0], 14),
        "vec4": np.ascontiguousarray(np.stack([_fm(g("w0")[0], 4), _fm(g("a0")[0], 4), _fm(g("k_k")[0], 4), _fm(g("k_a")[0], 4),
                                               _fm(g("lnx_g")[0], 4), _fm(g("lnx_b")[0], 4), _fm(g("r_k")[0].reshape(-1), 4)], axis=1)),
        "w_dec": g("w_decay_up")[0], "w_aup": g("w_a_up")[0], "w_gup": g("w_g_up")[0],
        "sgu_g": g("sgu_ln_g")[0][None, :], "sgu_b": g("sgu_ln_b")[0][None, :],
        "w_sp": g("w_spatial")[0], "b_sp": g("b_spatial")[0], "w_out": g("w_out")[0], "n2g": _fm(g("norm2_g")[0], 8),
        "w_rt": g("w_router")[0], "e_bias": g("e_bias")[0][None, :],
        "nfg": g("norm_f_g")[None, :],
    }
    if small:
        shared["w1e"] = np.zeros((1, D, DE), np.float32); shared["w3e"] = np.zeros((1, D, DE), np.float32); shared["w2e"] = np.zeros((1, DE, D), np.float32)
    else:
        shared["w1e"] = np.concatenate([g("w1_e")[0], g("w1_s")], axis=0)
        shared["w3e"] = np.concatenate([g("w3_e")[0], g("w3_s")], axis=0)
        shared["w2e"] = np.concatenate([g("w2_e")[0], g("w2_s")], axis=0)
    for k, v in consts.items():
        shared["c_" + k] = v
    maps = []
    for core in range(8):
        b, j = core // 4, core % 4
        win = np.zeros((NSEG * SEG, D), np.float32)
        lo = (j - 3) * SEG
        src0 = max(lo, 0)
        win[src0 - lo:] = x[b, src0:(j + 1) * SEG]
        fl = np.zeros((128, 5), np.float32)
        for s in range(5):
            seg_global = j - 4 + s
            fl[:, s] = 1.0 if seg_global >= 0 else 0.0
        m = dict(shared)
        m["xw"] = win
        m["flags"] = fl
        m["c_fm"] = _fm(c[b], 8)
        maps.append(m)
    return maps


_NC_CACHE = {}


def kernel(**inputs):
    if "nc" not in _NC_CACHE:
        _NC_CACHE["nc"] = build_nc()[0]
    nc = _NC_CACHE["nc"]
    maps = make_in_maps(inputs)
    res = run_bass_kernel_spmd(nc, maps, core_ids=list(range(8)))
    outp = np.zeros((2, 4 * SEG, D), np.float32)
    for core in range(8):
        b, j = core // 4, core % 4
        outp[b, j * SEG:(j + 1) * SEG] = res.results[core]["out"]
    return outp
```

```python
import numpy as np
import ml_dtypes
from contextlib import ExitStack

import concourse.bass as bass
import concourse.mybir as mybir
from concourse.bass_utils import run_bass_kernel_spmd

F32 = mybir.dt.float32
BF16 = mybir.dt.bfloat16
AF = mybir.ActivationFunctionType
ALU = mybir.AluOpType
AX = mybir.AxisListType

D = 1024
SEG = 2048
NSEG = 4
GRP = 512
NGRP = NSEG * SEG // GRP
OWN_G0 = (NSEG - 1) * SEG // GRP
C0 = 0.6065306597126334
NE = 64
DE = 256
SAME_SYNC = True


class Buf:
    def __init__(self, name):
        self.name = name
        self.psum = False
        self.w = None
        self.r = {}


class Tile(Buf):
    def __init__(self, ctx, name, shape, dtype, psum=False):
        super().__init__(name)
        self.psum = psum
        ctx.all_bufs.append(self)
        alloc = ctx.nc.psum_tensor if psum else ctx.nc.sbuf_tensor
        self.t = ctx.es.enter_context(alloc(name, list(shape), dtype))
        self.subs = {}

    def __getitem__(self, idx):
        return self.t[idx]

    def sub(self, key):
        if key not in self.subs:
            self.subs[key] = Buf(f"{self.name}.{key}")
        return self.subs[key]


class Ctx:
    NDS = 24

    def __init__(self, nc, es):
        self.nc = nc
        self.es = es
        self.eng = {"pe": nc.tensor, "act": nc.scalar, "dve": nc.vector, "pool": nc.gpsimd, "sp": nc.sync}
        self.sem = {k: es.enter_context(nc.semaphore("s_" + k)) for k in ("pe", "act", "dve", "pool")}
        self.cnt = {k: 0 for k in self.sem}
        self.seen = {k: {} for k in self.eng}
        self.dsem = [es.enter_context(nc.semaphore(f"s_dma{i}")) for i in range(self.NDS)]
        self.dcnt = [0] * self.NDS
        self.dnext = 0
        self.pending = {k: [] for k in self.sem}
        self.n_inst = 0
        self.all_bufs = []

    def _wait(self, e, h):
        kind, key, val = h
        if kind == "e" and key == e and (e == "pe" or not SAME_SYNC):
            return
        sk = (kind, key)
        if self.seen[e].get(sk, 0) >= val:
            return
        sem = self.sem[key] if kind == "e" else self.dsem[key]
        self.eng[e].wait_ge(sem, val)
        self.seen[e][sk] = val

    def _deps(self, e, reads, writes):
        hs = {}
        for b in reads:
            if b.w is not None:
                k = b.w[:2]
                hs[k] = max(hs.get(k, 0), b.w[2])
        for b in writes:
            if b.w is not None:
                k = b.w[:2]
                hs[k] = max(hs.get(k, 0), b.w[2])
            for k, v in b.r.items():
                hs[k] = max(hs.get(k, 0), v)
        for k, v in hs.items():
            self._wait(e, (k[0], k[1], v))

    def _mark(self, h, reads, writes):
        k = h[:2]
        for b in reads:
            b.r[k] = max(b.r.get(k, 0), h[2])
        for b in writes:
            b.w = h
            b.r = {}

    def op(self, e, fn, reads=(), writes=(), inc=True):
        pr = [b for b in reads if b.psum]
        if pr:
            reads = [b for b in reads if not b.psum]
            writes = list(writes) + [b for b in pr if b not in writes]
        self._deps(e, reads, writes)
        inst = fn(self.eng[e])
        self.n_inst += 1
        if inc:
            self.cnt[e] += 1
            inst.then_inc(self.sem[e], 1)
            h = ("e", e, self.cnt[e])
        else:
            h = ("e", e, self.cnt[e] + 1)
        self._mark(h, reads, writes)
        return h

    def dma(self, q, out, in_, reads=(), writes=(), **kw):
        i = self.dnext
        self.dnext = (self.dnext + 1) % self.NDS
        if self.dcnt[i]:
            self._wait(q, ("d", i, self.dcnt[i]))
        self._deps(q, reads, writes)
        inst = self.eng[q].dma_start(out=out, in_=in_, **kw)
        self.n_inst += 1
        self.dcnt[i] += 16
        inst.then_inc(self.dsem[i], 16)
        h = ("d", i, self.dcnt[i])
        self._mark(h, reads, writes)
        return h

    def wait_all(self, e, bufs):
        self._deps(e, (), bufs)

    def mm(self, out, lhsT, rhs, reads, writes, start=True, stop=True, inc=True):
        return self.op("pe", lambda E: E.matmul(out, lhsT, rhs, start=start, stop=stop), reads, writes, inc=inc)

    def tr(self, out, in_, ident, reads, writes, inc=True):
        return self.op("pe", lambda E: E.transpose(out, in_, ident), reads, writes, inc=inc)

    def act(self, out, in_, func, reads, writes, bias=None, scale=None, e="act", accum_out=None):
        kw = {}
        if bias is not None:
            kw["bias"] = bias
        if scale is not None:
            kw["scale"] = scale
        if accum_out is not None:
            kw["accum_out"] = accum_out
        return self.op(e, lambda E: E.activation(out=out, in_=in_, func=func, **kw), reads, writes)

    def tt(self, out, in0, in1, op, reads, writes, e="dve"):
        return self.op(e, lambda E: E.tensor_tensor(out=out, in0=in0, in1=in1, op=op), reads, writes)

    def ts(self, out, in0, s1, s2, op0, op1, reads, writes, e="dve"):
        if op1 is None:
            return self.op(e, lambda E: E.tensor_scalar(out=out, in0=in0, scalar1=s1, scalar2=None, op0=op0), reads, writes)
        return self.op(e, lambda E: E.tensor_scalar(out=out, in0=in0, scalar1=s1, scalar2=s2, op0=op0, op1=op1), reads, writes)

    def stt(self, out, in0, scalar, in1, op0, op1, reads, writes):
        return self.op("dve", lambda E: E.scalar_tensor_tensor(out=out, in0=in0, scalar=scalar, in1=in1, op0=op0, op1=op1), reads, writes)

    def cp(self, out, in_, reads, writes, e="dve"):
        if e == "act":
            return self.op("act", lambda E: E.copy(out=out, in_=in_), reads, writes)
        return self.op(e, lambda E: E.tensor_copy(out=out, in_=in_), reads, writes)

    def memset(self, ap, val, writes, e="dve"):
        return self.op(e, lambda E: E.memset(ap, val), (), writes)


def _consts():
    c = {}
    ident = np.eye(128, dtype=np.float32)
    c["ident"] = ident
    s = np.arange(128)[:, None] % 64
    t = np.arange(64)[None, :]
    su = (t > s).astype(np.float32)
    ui = (t >= s).astype(np.float32)
    sl = (t < s).astype(np.float32)
    half = (np.arange(128)[:, None] // 64 == np.arange(128)[None, :] // 64).astype(np.float32)
    def bd(m64):
        return np.concatenate([m64, m64], axis=1) * half
    c["maskX"] = np.concatenate([bd(sl), bd(su), bd(sl), bd(su)], axis=1)
    c["maskY"] = np.concatenate([bd(su), bd(ui), bd(ui)], axis=1)
    c["blk"] = half.copy()
    restart = np.ones((128, GRP), np.float32)
    restart[:, ::64] = 0.0
    c["restart"] = restart
    hsel = np.zeros((128, 4, 8), np.float32)
    for p_ in range(128):
        for hp in range(4):
            hsel[p_, hp, 2 * hp + p_ // 64] = 1.0
    c["hsel"] = hsel
    c["hselT"] = np.ascontiguousarray(hsel.transpose(2, 1, 0))
    return c


CONST_SHAPES = {"ident": [128, 128], "maskX": [128, 512], "maskY": [128, 384], "blk": [128, 128], "restart": [128, GRP], "hsel": [128, 4, 8], "hselT": [8, 4, 128]}


def build_nc(stage=99, dbg=False):
    nc = bass.Bass("TRN2", target_bir_lowering=False)

    def din(name, shape, dt=F32):
        return nc.dram_tensor(name, list(shape), dt, kind="ExternalInput").ap()

    xw = din("xw", [NSEG * SEG, D])
    flags = din("flags", [128, 5])
    c_fm = din("c_fm", [128, 8])
    w_ada = din("w_ada", [D, 6 * D])
    b_ada = din("b_ada", [128, 48])
    n1g = din("n1g", [128, 8])
    w_in = din("w_in", [D, 2816])
    mu = din("mu", [128, 14])
    vec4 = din("vec4", [128, 7, 4])
    w_dec = din("w_dec", [64, 512])
    w_aup = din("w_aup", [64, 512])
    w_gup = din("w_gup", [128, 512])
    sgu_g = din("sgu_g", [1, 512])
    sgu_b = din("sgu_b", [1, 512])
    w_sp = din("w_sp", [8, 128, 128])
    b_sp = din("b_sp", [8, 128])
    w_out = din("w_out", [D, D])
    n2g = din("n2g", [128, 8])
    w_rt = din("w_rt", [D, NE])
    e_bias = din("e_bias", [1, NE])
    nexp = NE + 1 if stage >= 3 else 1
    w1e = din("w1e", [nexp, D, DE])
    w3e = din("w3e", [nexp, D, DE])
    w2e = din("w2e", [nexp, DE, D])
    nfg = din("nfg", [1, D])
    cst = {k: din("c_" + k, shp) for k, shp in CONST_SHAPES.items()}
    out = nc.dram_tensor("out", [SEG, D], F32, kind="ExternalOutput").ap()
    x2d = nc.dram_tensor("x2_scratch", [SEG, D], F32, kind="Internal").ap()
    dbg_out = {}
    x2dbuf = Buf("x2dbuf")

    def dbg_tensor(name, shape):
        dbg_out[name] = nc.dram_tensor("dbg_" + name, list(shape), F32, kind="ExternalOutput").ap()
        return dbg_out[name]

    es = ExitStack()
    with es:
        K = Ctx(nc, es)
        T = lambda name, shape, dt=F32, psum=False: Tile(K, name, shape, dt, psum)
        def sb_used():
            return 0

        ident = T("ident", [128, 128])
        identb = T("identb", [128, 128], BF16)
        maskX = T("maskX", [128, 512])
        maskY = T("maskY", [128, 384])
        blk = T("blk", [128, 128])
        blkb = T("blkb", [128, 128], BF16)
        blk64 = T("blk64", [128, 128])
        restart = T("restart", [128, GRP])
        ones1 = T("ones1", [1, 128])
        flg = T("flg", [128, 5])
        K.dma("sp", ident[:], cst["ident"], (), [ident])
        K.dma("sp", maskX[:], cst["maskX"], (), [maskX])
        K.dma("sp", maskY[:], cst["maskY"], (), [maskY])
        K.dma("sp", blk[:], cst["blk"], (), [blk])
        K.dma("sp", restart[:], cst["restart"], (), [restart])
        K.dma("sp", flg[:], flags, (), [flg])
        K.cp(identb[:], ident[:], [ident], [identb])
        K.cp(blkb[:], blk[:], [blk], [blkb])
        K.ts(blk64[:], blk[:], 1.0 / 64.0, None, ALU.mult, None, [blk], [blk64])
        K.memset(ones1[:], 1.0, [ones1])

        banks = [T(f"bank{i}", [128, 512], F32, psum=True) for i in range(8)]

        n1g_t = T("n1g_t", [128, 8]); n2g_t = T("n2g_t", [128, 8]); mu_t = T("mu_t", [128, 14]); omm_t = T("omm_t", [128, 14])
        v4 = T("v4", [128, 7, 4]); cfm = T("cfm", [128, 8])
        K.dma("sp", n1g_t[:], n1g, (), [n1g_t]); K.dma("sp", n2g_t[:], n2g, (), [n2g_t])
        K.dma("sp", mu_t[:], mu, (), [mu_t]); K.dma("sp", v4[:], vec4, (), [v4]); K.dma("sp", cfm[:], c_fm, (), [cfm])
        K.ts(omm_t[:], mu_t[:], -1.0, 1.0, ALU.mult, ALU.add, [mu_t], [omm_t])
        omka = T("omka", [128, 4])
        K.ts(omka[:], v4[:, 3, :], -1.0, 1.0, ALU.mult, ALU.add, [v4], [omka])

        silc = T("silc", [128, 8])
        K.act(silc[:], cfm[:], AF.Silu, [cfm], [silc])
        modT = T("modT", [128, 48])
        K.dma("sp", modT[:], b_ada, (), [modT])
        g1rep = T("g1rep", [128, D]); g2rep = T("g2rep", [128, D])
        with ExitStack() as es2:
            K.es = es2
            wab = [T(f"wab{i}", [128, 3072]) for i in range(2)]
            dg = T("dg", [128, 128])
            it = 0
            for kc in range(8):
                for half in range(2):
                    wt = wab[it % 2]; it += 1
                    K.dma("sp", wt[:], w_ada[kc * 128:(kc + 1) * 128, half * 3072:(half + 1) * 3072], (), [wt])
                    for jc in range(24):
                        K.mm(banks[0][:, half * 24 + jc: half * 24 + jc + 1], wt[:, jc * 128:(jc + 1) * 128], silc[:, kc:kc + 1], [wt, silc], [banks[0]],
                             inc=(jc == 23))
                K.tt(modT[:], banks[0][:, 0:48], modT[:], ALU.add, [banks[0], modT], [modT])
            onesf = T("onesf", [128, 128])
            K.memset(onesf[:], 1.0, [onesf])
            for (idx, dst) in ((2, g1rep), (5, g2rep)):
                for kc in range(8):
                    K.ts(dg[:], ident[:], modT[:, idx * 8 + kc: idx * 8 + kc + 1], None, ALU.mult, None, [ident, modT], [dg])
                    K.mm(banks[1][:, 0:128], onesf[:], dg[:], [onesf, dg], [banks[1]])
                    K.cp(dst[:, kc * 128:(kc + 1) * 128], banks[1][:, 0:128], [banks[1]], [dst])
            K.wait_all("dve", wab + [dg, onesf]); K.wait_all("pe", wab + [dg, onesf]); K.wait_all("sp", wab)
            K.wait_all("act", wab + [dg, onesf]); K.wait_all("pool", wab + [dg, onesf])
        K.es = es
        A1 = T("A1", [128, 8]); A2 = T("A2", [128, 8])
        K.stt(A1[:], modT[:, 8:16], 1.0, n1g_t[:], ALU.add, ALU.mult, [modT, n1g_t], [A1])
        K.stt(A2[:], modT[:, 32:40], 1.0, n2g_t[:], ALU.add, ALU.mult, [modT, n2g_t], [A2])
        A1f = T("A1f", [128, 5, 8]); B1f = T("B1f", [128, 5, 8])
        for f in range(5):
            K.ts(A1f[:, f, :], A1[:], flg[:, f:f + 1], None, ALU.mult, None, [A1, flg], [A1f])
            K.ts(B1f[:, f, :], modT[:, 0:8], flg[:, f:f + 1], None, ALU.mult, None, [modT, flg], [B1f])

        def finish():
            for i in range(K.NDS):
                if K.dcnt[i]:
                    K._wait("sp", ("d", i, K.dcnt[i]))
            for e in ("pe", "act", "dve", "pool"):
                if K.cnt[e]:
                    K._wait("sp", ("e", e, K.cnt[e]))

        if dbg and stage == 0:
            d = dbg_tensor("modT", [128, 48])
            K.dma("sp", d, modT[:], [modT], ())
            d = dbg_tensor("A1", [128, 8])
            K.dma("sp", d, A1[:], [A1], ())
        if stage == 0:
            zt = T("zt", [128, D])
            K.memset(zt[:], 0.0, [zt])
            for i in range(16):
                K.dma("sp", out[i * 128:(i + 1) * 128, :], zt[:], [zt], ())
            finish()
            return nc, dbg_out

        es_scan = ExitStack()
        es_scan.__enter__()
        K.es = es_scan
        own_extra = stage >= 2
        winb = T("winb", [128, 8, 2816], BF16)
        for kc in range(8):
            for c0 in range(0, 2816, 1408):
                K.dma("pool", winb[:, kc, c0:c0 + 1408], w_in[kc * 128:(kc + 1) * 128, c0:c0 + 1408], (), [winb])
        wlo = T("wlo", [128, 512], BF16)
        wgb = T("wgb", [128, 512], BF16)
        K.dma("pool", wlo[0:64, :], w_dec, (), [wlo])
        K.dma("pool", wlo[64:128, :], w_aup, (), [wlo])
        K.dma("pool", wgb[:], w_gup, (), [wgb])
        hsel = T("hsel", [128, 4, 8], BF16); hselT = T("hselT", [8, 4, 128], BF16)
        K.dma("pool", hsel[:], cst["hsel"], (), [hsel]); K.dma("pool", hselT[:], cst["hselT"], (), [hselT])
        lrk = T("lrk", [128, 4, 128], BF16)
        for hp in range(4):
            K.ts(lrk[:, hp, :], blk[:], v4[:, 6, hp:hp + 1], None, ALU.mult, None, [blk, v4], [lrk])
        epsln = T("epsln", [128, 1])
        K.memset(epsln[:], 64e-5, [epsln])
        for bk in banks:
            K.memset(bk[:], 0.0, [bk])

        xt = T("xt", [128, 4, D])
        x2t = T("x2t", [128, D])
        if own_extra:
            woutb = T("woutb", [128, 8, D], BF16)
            for kc in range(8):
                K.dma("sp", x2t[:], w_out[kc * 128:(kc + 1) * 128, :], (), [x2t])
                K.tt(woutb[:, kc, :], x2t[:], g1rep[:], ALU.mult, [x2t, g1rep], [woutb])
            wsTb = T("wsTb", [128, 8, 128], BF16)
            for h in range(8):
                K.dma("sp", x2t[:, 0:128], w_sp[h], (), [x2t])
                K.tr(banks[0][:, 0:128], x2t[:, 0:128], ident[:], [x2t, ident], [banks[0]])
                K.cp(wsTb[:, h, :], banks[0][:, 0:128], [banks[0]], [wsTb])
                K.memset(wsTb[64:128, h, 0:64], 0.0, [wsTb])
            bsp = T("bsp", [128, 4, 128])
            for hp in range(4):
                for hh in range(2):
                    K.dma("sp", bsp[hh * 64:(hh + 1) * 64, hp, :], b_sp[2 * hp + hh, :].partition_broadcast(64), (), [bsp])
            lngr = T("lngr", [128, 512]); lnbr = T("lnbr", [128, 512])
            K.dma("sp", lngr[:], sgu_g[0, :].partition_broadcast(128), (), [lngr])
            K.dma("sp", lnbr[:], sgu_b[0, :].partition_broadcast(128), (), [lnbr])

        xy = T("xy", [128, 4096], BF16)
        hTt = T("hTt", [128, 8, GRP], BF16)
        uT = T("uT", [128, 4, GRP], BF16)
        ssq = T("ssq", [128, 4]); rstd = T("rstd", [128, 4])
        carry = T("carry", [128, 14])
        K.memset(carry[:], 0.0, [carry])
        shtmp = T("shtmp", [128, GRP + 1])
        sh12 = T("sh12", [128, GRP])
        twxa = T("twxa", [128, GRP], BF16); sgb = T("sgb", [128, GRP], BF16)
        shv = T("shv", [128, GRP])
        shk4 = [T(f"shk{i}", [128, GRP]) for i in range(4)]
        ss8 = T("ss8", [8, GRP]); rn8 = T("rn8", [8, GRP], BF16)
        lwp = T("lwp", [128, GRP]); av = T("av", [128, GRP]); gT = T("gT", [128, GRP])
        kkn = T("kkn", [128, GRP]); kmod = T("kmod", [128, GRP]); bb = T("bb", [128, GRP])
        cum = T("cum", [128, GRP])
        ep = T("ep", [128, GRP]); em = T("em", [128, GRP]); eq = T("eq", [128, GRP])
        tmpa = T("tmpa", [128, GRP]); tmpb = T("tmpb", [128, GRP])
        pc = T("pc", [128, 8])
        atb = T("atb", [128, GRP], BF16); rtb = T("rtb", [128, GRP], BF16); rtf = T("rtf", [128, GRP])
        btb = T("btb", [128, GRP], BF16); ktb = T("ktb", [128, GRP], BF16)
        bhb = T("bhb", [128, GRP], BF16); khb = T("khb", [128, GRP], BF16); vbb = T("vbb", [128, GRP], BF16)
        sqb = [atb, rtb, btb, ktb]
        rkb = T("rkb", [128, GRP], BF16); bv = T("bv", [128, GRP])
        TM = [T(f"TM{i}", [128, 3, 128], BF16) for i in range(2)]
        ZB = [T(f"ZB{i}", [128, 2, 2, 64], BF16) for i in range(2)]
        XQ = [[T(f"XQ{h}_{i}", [128, 3, 128], BF16) for i in range(2)] for h in range(2)]
        XQb = [[T(f"XQb{h}_{i}", [128, 3, 128], BF16) for i in range(2)] for h in range(2)]
        XQ2 = [XQ, XQb]
        mtmp = [[T(f"mtmp{p_}_{q}", [128, 128], BF16) for q in range(2)] for p_ in range(2)]
        YS = T("YS", [128, 2, 384], BF16)
        UW = T("UW", [128, 2, 2, 64], BF16)
        MT = [T(f"MT{i}", [128, 128], BF16) for i in range(2)]
        YSb = T("YSb", [128, 2, 128], BF16); UWb = T("UWb", [128, 2, 2, 64], BF16)
        MTb = [T(f"MTb{i}", [128, 128], BF16) for i in range(2)]
        YS2 = [YS, YSb]; UW2 = [UW, UWb]; MT2 = [MT, MTb]
        RhT = [T(f"RhT{i}", [128, 64], BF16) for i in range(2)]
        Hb = [[T(f"H{hp}_{i}", [128, 128], BF16) for i in range(2)] for hp in range(4)]
        hpar = [0, 0, 0, 0]
        for hp in range(4):
            K.memset(Hb[hp][0][:], 0.0, [Hb[hp][0]])
        YT = T("YT", [128, GRP])
        sh13 = YT; shr = cum; vtm = tmpb
        if own_extra:
            vnb = T("vnb", [128, 4, 512], BF16)
            bnst = T("bnst", [128, 6]); bnag = T("bnag", [128, 2]); lnr = T("lnr", [128, 2])
        bX, bY0, bY1, bA, bR, bE, bYg, bH = banks
        ptb = [banks[5][:, 0:256].bitcast(BF16), banks[6][:, 0:256].bitcast(BF16)]
        ptm = banks[6][:, 0:256].bitcast(BF16)

        dbg_el = dbg_tensor("el", [10, 128, GRP]) if (dbg and stage == 1) else None
        dbg_yt = dbg_tensor("yt", [4, 4, 128, GRP]) if (dbg and stage in (1, 2)) else None
        dbg_H = dbg_tensor("H", [4, 128, 128]) if (dbg and stage in (1, 2)) else None
        dbg_ya = dbg_tensor("ya", [4, 8, 128, GRP]) if (dbg and stage == 2) else None
        pj_cnt = [0]
        dump_n = [0]

        def dump(name, ap, shape, deps):
            if not (dbg and stage == 1):
                return
            dt_ = dbg_tensor(name, shape)
            st_ = T(f"dump{dump_n[0]}", shape); dump_n[0] += 1
            K.cp(st_[:], ap, deps, [st_])
            K.dma("sp", dt_, st_[:], [st_], ())


        def proj_shift(cc, dst, e2="dve"):
            bk = banks[pj_cnt[0] % 2]; pj_cnt[0] += 1
            for kc in range(8):
                K.mm(bk[:, :], winb[:, kc, cc * 128:(cc + 1) * 128], hTt[:, kc, :], [winb, hTt], [bk], start=(kc == 0), stop=(kc == 7), inc=(kc == 7))
            K.cp(shtmp[:, 0:1], carry[:, cc:cc + 1], [carry], [shtmp], e="pool")
            K.act(shtmp[:, 1:GRP + 1], bk[:, :], AF.Copy, [bk, mu_t], [shtmp], scale=mu_t[:, cc:cc + 1])
            K.cp(carry[:, cc:cc + 1], shtmp[:, GRP:GRP + 1], [shtmp], [carry], e="pool")
            K.stt(dst[:], bk[:, :], omm_t[:, cc:cc + 1], shtmp[:, 0:GRP], ALU.mult, ALU.add, [bk, omm_t, shtmp], [dst])

        g_first = 0 if stage != 1 else 0
        for g in range(g_first, NGRP):
            seg = g // 4
            own = g >= OWN_G0
            for tt in range(4):
                r0 = g * GRP + tt * 128
                K.dma("sp", xt[:, tt, :], xw[r0:r0 + 128, :], (), [xt])
            for tt in range(4):
                K.act(uT[:, 0:2, :].rearrange("p a b -> p (a b)"), xt[:, tt, :], AF.Square, [xt], [uT, ssq], accum_out=ssq[:, tt:tt + 1])
            K.ts(rstd[:], ssq[:], 1.0 / D, 1e-6, ALU.mult, ALU.add, [ssq], [rstd])
            K.act(rstd[:], rstd[:], AF.Sqrt, [rstd], [rstd])
            K.op("dve", lambda E: E.reciprocal(out=rstd[:], in_=rstd[:]), [rstd], [rstd])
            for tt in range(4):
                K.act(xy[:, tt * D:(tt + 1) * D], xt[:, tt, :], AF.Copy, [xt, rstd], [xy], scale=rstd[:, tt:tt + 1])
            for kc in range(8):
                pbk = banks[5 + kc % 2]
                pz = ptb[kc % 2]
                for tt in range(4):
                    K.tr(pz[:, tt * 128:(tt + 1) * 128], xy[:, tt * D + kc * 128: tt * D + (kc + 1) * 128], identb[:], [xy, identb], [pbk], inc=(tt == 3))
                K.act(hTt[:, kc, :], pz, AF.Identity, [pbk, A1f, B1f], [hTt],
                      scale=A1f[:, seg + 1, kc:kc + 1], bias=B1f[:, seg + 1, kc:kc + 1])

            if g == OWN_G0 - 1:
                for cc_ in (0, 1, 2, 3, 13):
                    proj_shift(cc_, sh13)
            proj_shift(12, sh12)
            K.act(twxa[0:64, :], sh12[0:64, :], AF.Tanh, [sh12], [twxa])
            K.cp(twxa[64:128, :], sh12[64:128, :], [sh12], [twxa], e="pool")
            if own:
                proj_shift(13, sh13)
                K.act(sgb[:], sh13[:], AF.Sigmoid, [sh13], [sgb])
            for hp in range(4):
                proj_shift(4 + hp, shk4[hp])
                K.act(sqb[hp][:], shk4[hp][:], AF.Square, [shk4[hp], v4], [sqb[hp]], scale=v4[:, 2, hp:hp + 1])
            for hp in range(4):
                K.mm(banks[2][0:8, :], hsel[:, hp, :], sqb[hp][:], [hsel, sqb[hp]], [banks[2]], start=(hp == 0), stop=(hp == 3), inc=(hp == 3))
            K.ts(ss8[:], banks[2][0:8, :], 1e-18, None, ALU.max, None, [banks[2]], [ss8])
            K.act(ss8[:], ss8[:], AF.Ln, [ss8], [ss8])
            K.act(rn8[:], ss8[:], AF.Exp, [ss8], [rn8], scale=-0.5)

            for hp in range(4):
                shk = shk4[hp]
                w0c = v4[:, 0, hp:hp + 1]; a0c = v4[:, 1, hp:hp + 1]; kkc = v4[:, 2, hp:hp + 1]; kac = v4[:, 3, hp:hp + 1]
                K.mm(banks[2][:, :], wlo[0:64, hp * 128:(hp + 1) * 128], twxa[0:64, :], [wlo, twxa], [banks[2]])
                K.mm(banks[3][:, :], wlo[64:128, hp * 128:(hp + 1) * 128], twxa[64:128, :], [wlo, twxa], [banks[3]])
                K.act(lwp[:], banks[2][:, :], AF.Sigmoid, [banks[2], v4], [lwp], bias=w0c)
                K.act(av[:], banks[3][:, :], AF.Sigmoid, [banks[3], v4], [av], bias=a0c)
                if own:
                    K.mm(banks[0][:, :], wgb[:, hp * 128:(hp + 1) * 128], sgb[:], [wgb, sgb], [banks[0]])
                    K.cp(gT[:], banks[0][:, :], [banks[0]], [gT], e="act")
                proj_shift(8 + hp, shv)
                K.mm(banks[2][:, :], hselT[:, hp, :], rn8[:], [hselT, rn8], [banks[2]])
                K.stt(kkn[:], shk[:], kkc, banks[2][:, :], ALU.mult, ALU.mult, [shk, v4, banks[2]], [kkn])
                K.ts(tmpa[:], av[:], kac, omka[:, hp:hp + 1], ALU.mult, ALU.add, [av, v4, omka], [tmpa])
                K.tt(kmod[:], shk[:], tmpa[:], ALU.mult, [shk, tmpa], [kmod])
                K.tt(bb[:], kkn[:], av[:], ALU.mult, [kkn, av], [bb], e="pool")
                K.op("dve", lambda E: E.tensor_tensor_scan(out=cum[:], data0=restart[:], data1=lwp[:], initial=0.0, op0=ALU.mult, op1=ALU.add),
                     [restart, lwp], [cum])
                K.tt(tmpb[:], cum[:], lwp[:], ALU.subtract, [cum, lwp], [tmpb], e="pool")
                K.act(ep[:], cum[:], AF.Exp, [cum], [ep], scale=-C0)
                K.act(em[:], cum[:], AF.Exp, [cum], [em], scale=C0)
                K.act(eq[:], tmpb[:], AF.Exp, [tmpb], [eq], scale=-C0)
                epv = ep[:].rearrange("p (c t) -> p c t", t=64)
                K.cp(pc[:], epv[:, :, 63], [ep], [pc])
                pcb = epv[:, :, 63:64].broadcast_to([128, 8, 64])
                v3 = lambda t_: t_[:].rearrange("p (c t) -> p c t", t=64)
                K.tt(tmpa[:], bb[:], em[:], ALU.mult, [bb, em], [tmpa])
                K.cp(btb[:], tmpa[:], [tmpa], [btb], e="pool")
                K.tt(v3(bhb), v3(tmpa), pcb, ALU.mult, [tmpa, ep], [bhb])
                K.tt(tmpb[:], kmod[:], em[:], ALU.mult, [kmod, em], [tmpb])
                K.cp(ktb[:], tmpb[:], [tmpb], [ktb], e="pool")
                K.tt(v3(khb), v3(tmpb), pcb, ALU.mult, [tmpb, ep], [khb])
                K.stt(atb[:], kkn[:], -1.0, eq[:], ALU.mult, ALU.mult, [kkn, eq], [atb])
                K.cp(vbb[:], shv[:], [shv], [vbb], e="act")
                if own:
                    proj_shift(hp, shr)
                    K.tt(rtf[:], shr[:], ep[:], ALU.mult, [shr, ep], [rtf])
                    K.cp(rtb[:], rtf[:], [rtf], [rtb], e="pool")
                    K.tt(rkb[:], shr[:], kmod[:], ALU.mult, [shr, kmod], [rkb])
                    K.mm(banks[3][:, :], lrk[:, hp, :], rkb[:], [lrk, rkb], [banks[3]])
                    K.tt(bv[:], banks[3][:, :], shv[:], ALU.mult, [banks[3], shv], [bv])
                if dbg_el is not None and g == NGRP - 1 and hp == 1:
                    for i, t_ in enumerate((lwp, av, kkn, kmod, bb, cum, shv, gT, shr, bv)):
                        K.dma("sp", dbg_el[i], t_[:], [t_], ())

                def cp_gen(cp_):
                    p_ = cp_ % 2
                    B1, B2, B3, B4 = banks[4 * p_:4 * p_ + 4]
                    ptm_p = B3[:, 0:256].bitcast(BF16)
                    tok = slice(cp_ * 128, (cp_ + 1) * 128)
                    TMt = TM[p_]; ZBt = ZB[p_]; XQp = XQ2[p_]; YSp = YS2[p_]; UWp = UW2[p_]; MTp = MT2[p_]
                    for i, src in enumerate((atb, bhb, khb, vbb)):
                        K.tr(ptm_p[:, i * 128:(i + 1) * 128], src[:, tok], identb[:], [src, identb], [B3], inc=(i == 3))
                    K.cp(TMt[:].rearrange("p a b -> p (a b)"), ptm_p[:, 128:512], [B3], [TMt])
                    K.cp(ZBt[:, :, 1, :], ptm_p[:, 0:128].rearrange("p (h v) -> p h v", h=2), [B3], [ZBt], e="act")
                    yield
                    for h in range(2):
                        hb = slice(h * 64, (h + 1) * 64)
                        bXh = B1 if h == 0 else B2
                        for q in range(2):
                            qp = slice(q * 64, (q + 1) * 64)
                            tq = slice(cp_ * 128 + q * 64, cp_ * 128 + (q + 1) * 64)
                            K.mm(bXh[qp, q * 64:(q + 1) * 64], atb[hb, tq], btb[hb, tq], [btb, atb], [bXh], inc=False)
                            K.mm(bXh[qp, 128 + q * 64:128 + (q + 1) * 64], btb[hb, tq], atb[hb, tq], [btb, atb], [bXh], inc=False)
                            K.mm(bXh[qp, 384 + q * 64:384 + (q + 1) * 64], ktb[hb, tq], atb[hb, tq], [ktb, atb], [bXh], inc=(q == 1))
                    yield
                    for h in range(2):
                        bXh = B1 if h == 0 else B2
                        K.tt(XQp[h][0][:, 0:2, :].rearrange("p a t -> p (a t)"), bXh[:, 0:256], maskX[:, 0:256], ALU.mult, [bXh, maskX], [XQp[h][0]])
                        K.cp(XQp[h][0][:, 2, :], identb[:], [identb], [XQp[h][0]], e="pool")
                        K.tt(YSp[:, h, 0:128], bXh[:, 384:512], maskY[:, 0:128], ALU.mult, [bXh, maskY], [YSp])
                    for h in range(2):
                        K.mm(B3[:, 256 + h * 64:256 + (h + 1) * 64], YSp[:, h, 0:128], TMt[:, 2, h * 64:(h + 1) * 64], [YSp, TMt], [B3], inc=(h == 1))
                    K.cp(ZBt[:, :, 0, :], B3[:, 256:384].rearrange("p (h v) -> p h v", h=2), [B3], [ZBt], e="act")
                    yield
                    bN = [B1, B2]
                    for it in range(6):
                        for h in range(2):
                            Xc = XQp[h][it % 2]
                            if it < 5:
                                K.mm(bN[h][:, 128:384], Xc[:, 0, :], Xc[:, 1:3, :].rearrange("p a t -> p (a t)"), [Xc], [bN[h]], inc=False)
                                K.mm(bN[h][:, 0:128], Xc[:, 1, :], Xc[:, 0, :], [Xc], [bN[h]])
                            else:
                                K.mm(bN[h][:, 256:384], Xc[:, 0, :], Xc[:, 2, :], [Xc], [bN[h]])
                        yield
                        for h in range(2):
                            Xc = XQp[h][it % 2]; Xn = XQp[h][1 - it % 2]
                            if it < 5:
                                K.cp(Xn[:, 0:2, :].rearrange("p a t -> p (a t)"), bN[h][:, 0:256], [bN[h]], [Xn], e="act")
                            K.tt(Xn[:, 2, :], bN[h][:, 256:384], Xc[:, 2, :], ALU.add, [bN[h], Xc], [Xn])
                        yield
                    for h in range(2):
                        K.mm(B4[:, h * 128:(h + 1) * 128], XQp[h][0][:, 2, :], ZBt[:, h, :, :].rearrange("p a v -> p (a v)"), [XQp[h][0], ZBt], [B4], inc=(h == 1))
                    K.cp(UWp[:].rearrange("p k h v -> p h k v"), B4[:, 0:256].rearrange("p (h k v) -> p h k v", h=2, k=2), [B4], [UWp])
                    yield
                    mtbank = [B3, B1]
                    for q in range(2):
                        qp = slice(q * 64, (q + 1) * 64)
                        for h in range(2):
                            hs_ = slice(h * 64, (h + 1) * 64)
                            K.mm(mtbank[q][hs_, 384 + h * 64:384 + (h + 1) * 64], UWp[qp, 1, h, :], TMt[qp, 0, hs_], [UWp, TMt], [mtbank[q]], inc=(h == 1))
                    yield
                    for q in range(2):
                        ck = cp_ * 2 + q
                        K.tt(mtmp[p_][q][:], mtbank[q][:, 384:512], blk[:], ALU.mult, [mtbank[q], blk], [mtmp[p_][q]])
                        K.stt(MTp[q][:], ident[:], pc[:, ck:ck + 1], mtmp[p_][q][:], ALU.mult, ALU.add, [ident, pc, mtmp[p_][q]], [MTp[q]])
                    hbank = [B4, B2]; hcol = [256, 384]
                    for q in range(2):
                        qp = slice(q * 64, (q + 1) * 64)
                        for h in range(2):
                            hs_ = slice(h * 64, (h + 1) * 64)
                            c0_ = hcol[q] + h * 64
                            K.mm(hbank[q][hs_, c0_:c0_ + 64], TMt[qp, 0, hs_], UWp[qp, 0, h, :], [TMt, UWp], [hbank[q]], start=True, stop=False, inc=False)
                            K.mm(hbank[q][hs_, c0_:c0_ + 64], TMt[qp, 1, hs_], TMt[qp, 2, hs_], [TMt], [hbank[q]], start=False, stop=False, inc=(h == 1))
                    yield
                    for q in range(2):
                        Hc = Hb[hp][hpar[hp]]; Hn = Hb[hp][1 - hpar[hp]]
                        hreg = hbank[q][:, hcol[q]:hcol[q] + 128]
                        K.mm(hreg, MTp[q][:], Hc[:], [MTp[q], Hc], [hbank[q]], start=False, stop=True)
                        K.cp(Hn[:], hreg, [hbank[q]], [Hn])
                        hpar[hp] = 1 - hpar[hp]
                    yield

                if not own:
                    gens = [cp_gen(c_) for c_ in range(4)]
                    active = []; steps = {}
                    nxt = 0
                    LAG = 2
                    while nxt < 4 or active:
                        if nxt < 4 and len(active) < 2 and (not active or steps[id(active[-1])] >= LAG):
                            active.append(gens[nxt]); steps[id(gens[nxt])] = 0; nxt += 1
                        for gen_ in list(active):
                            try:
                                next(gen_); steps[id(gen_)] += 1
                            except StopIteration:
                                active.remove(gen_)
                for cp_ in (range(4) if own else ()):
                    tok = slice(cp_ * 128, (cp_ + 1) * 128)
                    TMt = TM[cp_ % 2]; ZBt = ZB[cp_ % 2]
                    for i, src in enumerate((atb, bhb, khb, vbb)):
                        K.tr(ptm[:, i * 128:(i + 1) * 128], src[:, tok], identb[:], [src, identb], [bYg], inc=(i == 3))
                    K.cp(TMt[:].rearrange("p a b -> p (a b)"), ptm[:, 128:512], [bYg], [TMt])
                    K.cp(ZBt[:, :, 1, :], ptm[:, 0:128].rearrange("p (h v) -> p h v", h=2), [bYg], [ZBt], e="act")
                    for h in range(2):
                        hb = slice(h * 64, (h + 1) * 64)
                        bY = bY0 if h == 0 else bY1
                        bXh = bX if h == 0 else bA
                        for q in range(2):
                            qp = slice(q * 64, (q + 1) * 64)
                            tq = slice(cp_ * 128 + q * 64, cp_ * 128 + (q + 1) * 64)
                            K.mm(bXh[qp, h * 256 + q * 64: h * 256 + (q + 1) * 64], atb[hb, tq], btb[hb, tq], [btb, atb], [bXh], inc=False)
                            K.mm(bXh[qp, h * 256 + 128 + q * 64: h * 256 + 128 + (q + 1) * 64], btb[hb, tq], atb[hb, tq], [btb, atb], [bXh], inc=False)
                            K.mm(bY[qp, q * 64:(q + 1) * 64], ktb[hb, tq], atb[hb, tq], [ktb, atb], [bY], inc=((q == 1) and not own))
                            if own:
                                K.mm(bY[qp, 128 + q * 64:128 + (q + 1) * 64], ktb[hb, tq], rtb[hb, tq], [ktb, rtb], [bY], inc=False)
                                K.mm(bY[qp, 256 + q * 64:256 + (q + 1) * 64], btb[hb, tq], rtb[hb, tq], [btb, rtb], [bY], inc=(q == 1))
                    ny = 384 if own else 128
                    for h in range(2):
                        bXh = bX if h == 0 else bA
                        bY = bY0 if h == 0 else bY1
                        K.tt(XQ[h][0][:, 0:2, :].rearrange("p a t -> p (a t)"), bXh[:, h * 256:(h + 1) * 256], maskX[:, h * 256:(h + 1) * 256], ALU.mult,
                             [bXh, maskX], [XQ[h][0]])
                        K.cp(XQ[h][0][:, 2, :], identb[:], [identb], [XQ[h][0]], e="pool")
                        K.tt(YS[:, h, 0:ny], bY[:, 0:ny], maskY[:, 0:ny], ALU.mult, [bY, maskY], [YS])
                    for h in range(2):
                        K.mm(bR[:, 256 + h * 64:256 + (h + 1) * 64], YS[:, h, 0:128], TMt[:, 2, h * 64:(h + 1) * 64], [YS, TMt], [bR], inc=(h == 1))
                    K.cp(ZBt[:, :, 0, :], bR[:, 256:384].rearrange("p (h v) -> p h v", h=2), [bR], [ZBt], e="act")
                    bN = [bX, bA]
                    for it in range(6):
                        for h in range(2):
                            Xc = XQ[h][it % 2]
                            if it < 5:
                                K.mm(bN[h][:, 128:384], Xc[:, 0, :], Xc[:, 1:3, :].rearrange("p a t -> p (a t)"), [Xc], [bN[h]], inc=False)
                                K.mm(bN[h][:, 0:128], Xc[:, 1, :], Xc[:, 0, :], [Xc], [bN[h]])
                            else:
                                K.mm(bN[h][:, 256:384], Xc[:, 0, :], Xc[:, 2, :], [Xc], [bN[h]])
                        for h in range(2):
                            Xc = XQ[h][it % 2]; Xn = XQ[h][1 - it % 2]
                            if it < 5:
                                K.cp(Xn[:, 0:2, :].rearrange("p a t -> p (a t)"), bN[h][:, 0:256], [bN[h]], [Xn], e="act")
                            K.tt(Xn[:, 2, :], bN[h][:, 256:384], Xc[:, 2, :], ALU.add, [bN[h], Xc], [Xn])
                    for h in range(2):
                        K.mm(bE[:, h * 128:(h + 1) * 128], XQ[h][0][:, 2, :], ZBt[:, h, :, :].rearrange("p a v -> p (a v)"), [XQ[h][0], ZBt], [bE], inc=(h == 1))
                    K.cp(UW[:].rearrange("p k h v -> p h k v"), bE[:, 0:256].rearrange("p (h k v) -> p h k v", h=2, k=2), [bE], [UW])
                    mtreg = [bR[:, 384:512], bE[:, 384:512]]; mtbank = [bR, bE]
                    for q in range(2):
                        qp = slice(q * 64, (q + 1) * 64)
                        for h in range(2):
                            hs_ = slice(h * 64, (h + 1) * 64)
                            K.mm(mtbank[q][hs_, 384 + h * 64:384 + (h + 1) * 64], UW[qp, 1, h, :], TMt[qp, 0, hs_], [UW, TMt], [mtbank[q]], inc=(h == 1))
                    for q in range(2):
                        ck = cp_ * 2 + q
                        K.stt(MT[q][:], ident[:], pc[:, ck:ck + 1], mtreg[q], ALU.mult, ALU.add, [ident, pc, mtbank[q]], [MT[q]])
                    hbank = [bH, bX]
                    for q in range(2):
                        qp = slice(q * 64, (q + 1) * 64)
                        for h in range(2):
                            hs_ = slice(h * 64, (h + 1) * 64)
                            K.mm(hbank[q][hs_, h * 64:(h + 1) * 64], TMt[qp, 0, hs_], UW[qp, 0, h, :], [TMt, UW], [hbank[q]], start=True, stop=False, inc=False)
                            K.mm(hbank[q][hs_, h * 64:(h + 1) * 64], TMt[qp, 1, hs_], TMt[qp, 2, hs_], [TMt], [hbank[q]], start=False, stop=False, inc=(h == 1))
                    for q in range(2):
                        qp = slice(q * 64, (q + 1) * 64)
                        tq = slice(cp_ * 128 + q * 64, cp_ * 128 + (q + 1) * 64)
                        Hc = Hb[hp][hpar[hp]]; Hn = Hb[hp][1 - hpar[hp]]
                        hreg = hbank[q][:, 0:128]
                        if own:
                            RhTt = RhT[q]
                            yreg = bYg[:, 256 + q * 64:256 + (q + 1) * 64]
                            for h in range(2):
                                hs_ = slice(h * 64, (h + 1) * 64)
                                K.mm(bE[hs_, 256 + q * 64:256 + (q + 1) * 64], UW[qp, 1, h, :], YS[qp, h, 256 + q * 64:256 + (q + 1) * 64], [UW, YS], [bE], inc=(h == 1))
                            K.tt(RhTt[:], bE[:, 256 + q * 64:256 + (q + 1) * 64], rtf[:, tq], ALU.add, [bE, rtf], [RhTt])
                            for h in range(2):
                                hs_ = slice(h * 64, (h + 1) * 64)
                                K.mm(bYg[hs_, 256 + q * 64:256 + (q + 1) * 64], UW[qp, 0, h, :], YS[qp, h, 256 + q * 64:256 + (q + 1) * 64], [UW, YS], [bYg], start=True, stop=False, inc=False)
                                K.mm(bYg[hs_, 256 + q * 64:256 + (q + 1) * 64], TMt[qp, 2, hs_], YS[qp, h, 128 + q * 64:128 + (q + 1) * 64], [TMt, YS], [bYg], start=False, stop=False, inc=False)
                            K.mm(yreg, Hc[:], RhTt[:], [Hc, RhTt], [bYg], start=False, stop=True)
                            K.cp(YT[:, tq], yreg, [bYg], [YT], e="act")
                        K.mm(hreg, MT[q][:], Hc[:], [MT[q], Hc], [hbank[q]], start=False, stop=True)
                        K.cp(Hn[:], hreg, [hbank[q]], [Hn])
                        hpar[hp] = 1 - hpar[hp]
                if dbg_yt is not None and own:
                    K.dma("sp", dbg_yt[g - OWN_G0, hp], YT[:], [YT], ())
                if dbg and stage == 1 and hp == 0:
                    dump(f"Hg{g}", Hb[0][hpar[0]][:], [128, 128], [Hb[0][hpar[0]]])
                if own_extra and own:
                    lgc = v4[:, 4, hp:hp + 1]; lbc = v4[:, 5, hp:hp + 1]
                    K.mm(banks[2][:, :], blk64[:], YT[:], [blk64, YT], [banks[2]])
                    K.tt(tmpa[:], YT[:], banks[2][:, :], ALU.subtract, [YT, banks[2]], [tmpa])
                    K.act(tmpb[:], tmpa[:], AF.Square, [tmpa], [tmpb])
                    K.mm(banks[3][:, :], blk64[:], tmpb[:], [blk64, tmpb], [banks[3]])
                    K.act(tmpb[:], banks[3][:, :], AF.Ln, [banks[3], epsln], [tmpb], bias=epsln[:, 0:1])
                    K.act(tmpb[:], tmpb[:], AF.Exp, [tmpb], [tmpb], scale=-0.5)
                    K.tt(tmpa[:], tmpa[:], tmpb[:], ALU.mult, [tmpa, tmpb], [tmpa])
                    K.ts(tmpa[:], tmpa[:], lgc, lbc, ALU.mult, ALU.add, [tmpa, v4], [tmpa])
                    K.tt(tmpa[:], tmpa[:], bv[:], ALU.add, [tmpa, bv], [tmpa], e="pool")
                    K.tt(xy[:, hp * GRP:(hp + 1) * GRP], tmpa[:], gT[:], ALU.mult, [tmpa, gT], [xy])
            if own_extra and own:
                for i in range(4):
                    bk = banks[i % 2]
                    for kc in range(8):
                        K.mm(bk[:, :], winb[:, kc, 1792 + i * 128:1792 + (i + 1) * 128], hTt[:, kc, :], [winb, hTt], [bk], start=(kc == 0), stop=(kc == 7), inc=(kc == 7))
                    K.act(uT[:, i, :], bk[:, :], AF.Gelu_apprx_tanh, [bk], [uT])
                spb = [banks[2], banks[3], banks[6], banks[7]]
                for tt in range(4):
                    for kc in range(8):
                        K.mm(banks[0][:, :], hTt[:, kc, tt * 128:(tt + 1) * 128], winb[:, kc, 2304:2816], [winb, hTt], [banks[0]], start=(kc == 0), stop=(kc == 7), inc=(kc == 7))
                    K.act(vtm[:], banks[0][:, :], AF.Gelu_apprx_tanh, [banks[0]], [vtm])
                    K.op("dve", lambda E: E.bn_stats(out=bnst[:], in_=vtm[:]), [vtm], [bnst])
                    K.op("dve", lambda E: E.bn_aggr(out=bnag[:], in_=bnst[:]), [bnst], [bnag])
                    K.ts(lnr[:, 0:1], bnag[:, 1:2], 1e-5, None, ALU.add, None, [bnag], [lnr])
                    K.act(lnr[:, 0:1], lnr[:, 0:1], AF.Sqrt, [lnr], [lnr])
                    K.op("dve", lambda E: E.reciprocal(out=lnr[:, 1:2], in_=lnr[:, 0:1]), [lnr], [lnr])
                    K.ts(vtm[:], vtm[:], bnag[:, 0:1], lnr[:, 1:2], ALU.subtract, ALU.mult, [vtm, bnag, lnr], [vtm])
                    K.tt(vtm[:], vtm[:], lngr[:], ALU.mult, [vtm, lngr], [vtm], e="pool")
                    K.tt(vnb[:, tt, :], vtm[:], lnbr[:], ALU.add, [vtm, lnbr], [vnb], e="pool")
                    for h in range(8):
                        K.mm(spb[h // 2][(h % 2) * 64:(h % 2) * 64 + 64, tt * 128:(tt + 1) * 128], vnb[:, tt, h * 64:(h + 1) * 64], wsTb[:, h, :],
                             [vnb, wsTb], [spb[h // 2]], inc=(h % 2 == 1))
                for hp in range(4):
                    K.tt(tmpa[:].rearrange("p (r t) -> p r t", r=4), spb[hp][:, :].rearrange("p (r t) -> p r t", r=4),
                         bsp[:, hp:hp + 1, :].broadcast_to([128, 4, 128]), ALU.add, [spb[hp], bsp], [tmpa])
                    K.tt(xy[:, (4 + hp) * GRP:(5 + hp) * GRP], tmpa[:], uT[:, hp, :], ALU.mult, [tmpa, uT], [xy])
                if dbg_ya is not None:
                    for i in range(8):
                        K.cp(tmpa[:], xy[:, i * GRP:(i + 1) * GRP], [xy], [tmpa])
                        K.dma("sp", dbg_ya[g - OWN_G0, i], tmpa[:], [tmpa], ())
                for tt in range(4):
                    for half in range(2):
                        bk = banks[half]
                        for kc in range(8):
                            K.mm(bk[:, :], xy[:, kc * GRP + tt * 128: kc * GRP + (tt + 1) * 128], woutb[:, kc, half * 512:(half + 1) * 512], [xy, woutb], [bk],
                                 start=(kc == 0), stop=(kc == 7), inc=(kc == 7))
                        K.tt(x2t[:, half * 512:(half + 1) * 512], bk[:, :], xt[:, tt, half * 512:(half + 1) * 512], ALU.add, [bk, xt], [x2t])
                    r0 = (g - OWN_G0) * GRP + tt * 128
                    K.dma("sp", x2d[r0:r0 + 128, :], x2t[:], [x2t], [x2dbuf])
        if dbg_H is not None:
            for hp in range(4):
                K.cp(tmpa[:, 0:128], Hb[hp][hpar[hp]][:], [Hb[hp][hpar[hp]]], [tmpa])
                K.dma("sp", dbg_H[hp], tmpa[:, 0:128], [tmpa], ())
        if stage <= 2:
            if stage == 2:
                for i in range(16):
                    K.dma("sp", x2t[:], x2d[i * 128:(i + 1) * 128, :], [x2dbuf], [x2t])
                    K.dma("sp", out[i * 128:(i + 1) * 128, :], x2t[:], [x2t], ())
            else:
                K.memset(x2t[:], 0.0, [x2t])
                for i in range(16):
                    K.dma("sp", out[i * 128:(i + 1) * 128, :], x2t[:], [x2t], ())
            finish()
            es_scan.__exit__(None, None, None)
            return nc, dbg_out
        allb = list(K.all_bufs)
        for e in ("pe", "act", "dve", "pool", "sp"):
            K._deps(e, (), allb)
        es_scan.__exit__(None, None, None)
        K.es = es

        acc = T("acc", [128, 16, D])
        h2T = T("h2T", [128, 8, SEG], BF16)
        gates = T("gates", [128, 16, NE + 1])
        ebr = T("ebr", [128, NE]); nfr = T("nfr", [128, D]); wrt = T("wrt", [128, 8, NE])
        K.dma("sp", ebr[:], e_bias[0, :].partition_broadcast(128), (), [ebr])
        K.dma("sp", nfr[:], nfg[0, :].partition_broadcast(128), (), [nfr])
        K.dma("sp", wrt[:], w_rt.rearrange("(kc p) n -> p kc n", p=128), (), [wrt])
        xnf = T("xnf", [128, D]); h2f = T("h2f", [128, 8, 128])
        sc = T("sc", [128, NE]); bi = T("bi", [128, NE]); mk = T("mk", [128, NE]); m8 = T("m8", [128, 8, 8])
        gs = T("gs", [128, 8]); gs8 = T("gs8", [128, 8]); gm = T("gm", [128, 8]); pen = T("pen", [128, 8]); t8 = T("t8", [128, 8])
        ssq2 = T("ssq2", [128, 1]); rs2 = T("rs2", [128, 1]); rsum = T("rsum", [128, 1]); junk2 = T("junk2", [128, D], BF16)
        dbg_g = dbg_tensor("gates", [16, 128, NE + 1]) if dbg else None

        def rms_rstd(t):
            K.act(junk2[:], acc[:, t, :], AF.Square, [acc.sub(t)], [junk2, ssq2], accum_out=ssq2[:, 0:1])
            K.ts(rs2[:], ssq2[:], 1.0 / D, 1e-6, ALU.mult, ALU.add, [ssq2], [rs2])
            K.act(rs2[:], rs2[:], AF.Sqrt, [rs2], [rs2])
            K.op("dve", lambda E: E.reciprocal(out=rs2[:], in_=rs2[:]), [rs2], [rs2])

        for t in range(16):
            K.dma("sp", acc[:, t, :], x2d[t * 128:(t + 1) * 128, :], [x2dbuf], [acc.sub(t)])
            rms_rstd(t)
            K.act(xnf[:], acc[:, t, :], AF.Copy, [acc.sub(t), rs2], [xnf], scale=rs2[:, 0:1])
            for kc in range(8):
                bk = banks[kc // 4]
                K.tr(bk[:, (kc % 4) * 128:(kc % 4 + 1) * 128], xnf[:, kc * 128:(kc + 1) * 128], ident[:], [xnf, ident], [bk], inc=(kc % 4 == 3))
            for kc in range(8):
                bk = banks[kc // 4]; reg = bk[:, (kc % 4) * 128:(kc % 4 + 1) * 128]
                K.act(h2T[:, kc, t * 128:(t + 1) * 128], reg, AF.Identity, [bk, A2, modT], [h2T.sub(t)], scale=A2[:, kc:kc + 1], bias=modT[:, 24 + kc:25 + kc])
                K.ts(h2f[:, kc, :], reg, A2[:, kc:kc + 1], modT[:, 24 + kc:25 + kc], ALU.mult, ALU.add, [bk, A2, modT], [h2f])
            for kc in range(8):
                K.mm(banks[2][:, 0:NE], h2f[:, kc, :], wrt[:, kc, :], [h2f, wrt], [banks[2]], start=(kc == 0), stop=(kc == 7), inc=(kc == 7))
            K.act(sc[:], banks[2][:, 0:NE], AF.Sigmoid, [banks[2]], [sc])
            K.tt(bi[:], sc[:], ebr[:], ALU.add, [sc, ebr], [bi])
            for gi in range(8):
                K.op("dve", lambda E: E.max(out=m8[:, gi, :], in_=bi[:, gi * 8:(gi + 1) * 8]), [bi], [m8])
            K.tt(gs[:], m8[:, :, 0], m8[:, :, 1], ALU.add, [m8], [gs])
            K.op("dve", lambda E: E.max(out=gs8[:], in_=gs[:]), [gs], [gs8])
            K.ts(gm[:], gs[:], gs8[:, 3:4], None, ALU.is_ge, None, [gs, gs8], [gm])
            K.ts(pen[:], gm[:], 1e9, -1e9, ALU.mult, ALU.add, [gm], [pen])
            mkv = mk[:].rearrange("p (g e) -> p g e", e=8); biv = bi[:].rearrange("p (g e) -> p g e", e=8)
            K.tt(mkv, biv, gm[:].unsqueeze(2).broadcast_to([128, 8, 8]), ALU.mult, [bi, gm], [mk])
            K.tt(mkv, mkv, pen[:].unsqueeze(2).broadcast_to([128, 8, 8]), ALU.add, [mk, pen], [mk])
            K.op("dve", lambda E: E.max(out=t8[:], in_=mk[:]), [mk], [t8])
            K.ts(mk[:], mk[:], t8[:, 7:8], None, ALU.is_ge, None, [mk, t8], [mk])
            K.tt(mk[:], mk[:], sc[:], ALU.mult, [mk, sc], [mk])
            K.op("dve", lambda E: E.tensor_reduce(out=rsum[:], in_=mk[:], axis=AX.X, op=ALU.add), [mk], [rsum])
            K.op("dve", lambda E: E.reciprocal(out=rsum[:], in_=rsum[:]), [rsum], [rsum])
            K.ts(gates[:, t, 0:NE], mk[:], rsum[:, 0:1], 2.5, ALU.mult, ALU.mult, [mk, rsum], [gates])
            K.memset(gates[:, t, NE:NE + 1], 1.0, [gates])
            if dbg_g is not None:
                K.dma("sp", dbg_g[t], gates[:, t, :], [gates], ())

        w1b = [T(f"w1b{i}", [128, 8, DE], BF16) for i in range(2)]
        w3b = [T(f"w3b{i}", [128, 8, DE], BF16) for i in range(2)]
        w2b = [T(f"w2b{i}", [128, 2, D], BF16) for i in range(2)]
        actb = [T(f"actb{i}", [128, 2, GRP], BF16) for i in range(2)]
        sgt = [T(f"sgt{i}", [128, GRP]) for i in range(2)]
        n_exp = NE + 1
        for e in range(n_exp):
            i = e % 2
            for hf in range(2):
                K.dma("pool", w1b[i][:, hf * 4:(hf + 1) * 4, :], w1e[e, hf * 512:(hf + 1) * 512, :].rearrange("(kc p) n -> p kc n", p=128), (), [w1b[i]])
                K.dma("pool", w3b[i][:, hf * 4:(hf + 1) * 4, :], w3e[e, hf * 512:(hf + 1) * 512, :].rearrange("(kc p) n -> p kc n", p=128), (), [w3b[i]])
                K.dma("pool", w2b[i][:, hf, :], w2e[e, hf * 128:(hf + 1) * 128, :], (), [w2b[i]])
            for hf in range(2):
                K.tt(w2b[i][:, hf, :], w2b[i][:, hf, :], g2rep[:], ALU.mult, [w2b[i], g2rep], [w2b[i]], e="pool")
            for tg in range(4):
                ab = actb[tg % 2]
                h2r = [h2T.sub(4 * tg + j) for j in range(4)]
                for cc in range(2):
                    bG = banks[cc * 2]; bU = banks[cc * 2 + 1]
                    for kc in range(8):
                        K.mm(bG[:, :], w1b[i][:, kc, cc * 128:(cc + 1) * 128], h2T[:, kc, tg * GRP:(tg + 1) * GRP], [w1b[i]] + h2r, [bG],
                             start=(kc == 0), stop=(kc == 7), inc=(kc == 7))
                    for kc in range(8):
                        K.mm(bU[:, :], w3b[i][:, kc, cc * 128:(cc + 1) * 128], h2T[:, kc, tg * GRP:(tg + 1) * GRP], [w3b[i]] + h2r, [bU],
                             start=(kc == 0), stop=(kc == 7), inc=(kc == 7))
                    K.act(sgt[cc][:], bG[:, :], AF.Silu, [bG], [sgt[cc]])
                    K.tt(ab[:, cc, :], sgt[cc][:], bU[:, :], ALU.mult, [sgt[cc], bU], [ab])
                for tt in range(4):
                    t = tg * 4 + tt
                    for half in range(2):
                        bO = banks[4 + (tt % 2) * 2 + half]
                        for cc in range(2):
                            K.mm(bO[:, :], ab[:, cc, tt * 128:(tt + 1) * 128], w2b[i][:, cc, half * 512:(half + 1) * 512], [ab, w2b[i]], [bO],
                                 start=(cc == 0), stop=(cc == 1), inc=(cc == 1))
                        K.stt(acc[:, t, half * 512:(half + 1) * 512], bO[:, :], gates[:, t, e:e + 1], acc[:, t, half * 512:(half + 1) * 512],
                              ALU.mult, ALU.add, [bO, gates, acc.sub(t)], [acc.sub(t)])
        for t in range(16):
            rms_rstd(t)
            K.stt(xnf[:], acc[:, t, :], rs2[:, 0:1], nfr[:], ALU.mult, ALU.mult, [acc.sub(t), rs2, nfr], [xnf])
            K.dma("sp", out[t * 128:(t + 1) * 128, :], xnf[:], [xnf], ())
        finish()
    return nc, dbg_out


def _fm(v, n):
    return np.ascontiguousarray(np.asarray(v, np.float32).reshape(n, 128).T)


def make_in_maps(inputs, small=False):
    g = lambda k: np.asarray(inputs[k], np.float32)
    x = g("x"); c = g("c")
    consts = _consts()
    shared = {
        "w_ada": g("w_ada")[0], "b_ada": _fm(g("b_ada")[0], 48), "n1g": _fm(g("norm1_g")[0], 8),
        "w_in": g("w_in")[0], "mu": _fm(g("mu_shift")[0], 14),
        "vec4": np.ascontiguousarray(np.stack([_fm(g("w0")[0], 4), _fm(g("a0")[0], 4), _fm(g("k_k")[0], 4), _fm(g("k_a")[0], 4),
                                               _fm(g("lnx_g")[0], 4), _fm(g("lnx_b")[0], 4), _fm(g("r_k")[0].reshape(-1), 4)], axis=1)),
        "w_dec": g("w_decay_up")[0], "w_aup": g("w_a_up")[0], "w_gup": g("w_g_up")[0],
        "sgu_g": g("sgu_ln_g")[0][None, :], "sgu_b": g("sgu_ln_b")[0][None, :],
        "w_sp": g("w_spatial")[0], "b_sp": g("b_spatial")[0], "w_out": g("w_out")[0], "n2g": _fm(g("norm2_g")[0], 8),
        "w_rt": g("w_router")[0], "e_bias": g("e_bias")[0][None, :],
        "nfg": g("norm_f_g")[None, :],
    }
    if small:
        shared["w1e"] = np.zeros((1, D, DE), np.float32); shared["w3e"] = np.zeros((1, D, DE), np.float32); shared["w2e"] = np.zeros((1, DE, D), np.float32)
    else:
        shared["w1e"] = np.concatenate([g("w1_e")[0], g("w1_s")], axis=0)
        shared["w3e"] = np.concatenate([g("w3_e")[0], g("w3_s")], axis=0)
        shared["w2e"] = np.concatenate([g("w2_e")[0], g("w2_s")], axis=0)
    for k, v in consts.items():
        shared["c_" + k] = v
    maps = []
    for core in range(8):
        b, j = core // 4, core % 4
        win = np.zeros((NSEG * SEG, D), np.float32)
        lo = (j - 3) * SEG
        src0 = max(lo, 0)
        win[src0 - lo:] = x[b, src0:(j + 1) * SEG]
        fl = np.zeros((128, 5), np.float32)
        for s in range(5):
            seg_global = j - 4 + s
            fl[:, s] = 1.0 if seg_global >= 0 else 0.0
        m = dict(shared)
        m["xw"] = win
        m["flags"] = fl
        m["c_fm"] = _fm(c[b], 8)
        maps.append(m)
    return maps


_NC_CACHE = {}


def kernel(**inputs):
    if "nc" not in _NC_CACHE:
        _NC_CACHE["nc"] = build_nc()[0]
    nc = _NC_CACHE["nc"]
    maps = make_in_maps(inputs)
    res = run_bass_kernel_spmd(nc, maps, core_ids=list(range(8)))
    outp = np.zeros((2, 4 * SEG, D), np.float32)
    for core in range(8):
        b, j = core // 4, core % 4
        outp[b, j * SEG:(j + 1) * SEG] = res.results[core]["out"]
    return outp
```
